# Optimizing a Trainium2 kernel written in Bass

```python
import math
import jax
import jax.numpy as jnp
from jax import lax
import numpy as np

D_MODEL = 1024
BATCH = 8
SEQ = 4096
DEPTH = 2

GRID_W = 64
CTX_LEN = 256
ROPE_BASE = 10000.0
Q_BLOCK = 128
EPS = 1e-6

DIFF_HEADS = 4
DIFF_DH = 64
DIFF_VD = 2 * DIFF_DH
DIFF_QK = DIFF_HEADS * 2 * DIFF_DH
DIFF_V = DIFF_HEADS * DIFF_VD

SSD_HEADS = 8
SSD_P = 64
SSD_INNER = SSD_HEADS * SSD_P
SSD_GROUPS = 2
SSD_STATE = 128
SSD_CONV = 5
SSD_CHUNK = 128
SSD_CONV_CH = SSD_INNER + 2 * SSD_GROUPS * SSD_STATE

MLA_HEADS = 8
MLA_NOPE = 64
MLA_ROPE = 32
MLA_V = 64
MLA_Q_LORA = 384
MLA_KV_LORA = 256
MLA_QK = MLA_NOPE + MLA_ROPE

N_BRANCH = 3
BRANCH_W = DIFF_V

IN_SIZES = (DIFF_QK, DIFF_QK, DIFF_V, SSD_INNER, SSD_CONV_CH, 2 * SSD_HEADS,
            MLA_Q_LORA, MLA_KV_LORA, MLA_ROPE, N_BRANCH * D_MODEL)
D_IN = sum(IN_SIZES)

MOE_GROUPS = 4
MOE_PER_GROUP = 8
MOE_EXPERTS = MOE_GROUPS * MOE_PER_GROUP
MOE_TOP_K = 2
MOE_HIDDEN = 256
MOE_BLOCK = 128

kernel_name = 'hybrid_diffusion_trunk'

F32 = jnp.float32


def _split_points(sizes):
    pts, acc = [], 0
    for s in sizes[:-1]:
        acc += s
        pts.append(acc)
    return pts


def rms_norm(x, g):
    xf = x.astype(F32)
    y = xf * lax.rsqrt(jnp.mean(xf * xf, axis=-1, keepdims=True) + EPS)
    return y.astype(x.dtype) * g


def modulate(h, shift, scale):
    return h * (1.0 + scale) + shift


def axial_rope_tables(rows, dim):
    quarter = dim // 4
    inv_freq = ROPE_BASE ** (-jnp.arange(quarter, dtype=F32) / quarter)
    r = jnp.repeat(jnp.arange(rows, dtype=F32), GRID_W)
    cc = jnp.tile(jnp.arange(GRID_W, dtype=F32), rows)
    ar = r[:, None] * inv_freq
    ac = cc[:, None] * inv_freq
    ang = jnp.concatenate([ar, ar, ac, ac], axis=-1)
    return jnp.cos(ang), jnp.sin(ang)


def apply_axial_rope(x, cos, sin):
    x1, x2, x3, x4 = jnp.split(x, 4, axis=-1)
    rot = jnp.concatenate([-x2, x1, -x4, x3], axis=-1)
    shape = (1, cos.shape[0]) + (1,) * (x.ndim - 3) + (cos.shape[-1],)
    return x * cos.reshape(shape).astype(x.dtype) + rot * sin.reshape(shape).astype(x.dtype)


def sweep_query_blocks(fn, q):
    bsz, L = q.shape[:2]
    qb = jnp.moveaxis(q.reshape((bsz, L // Q_BLOCK, Q_BLOCK) + q.shape[2:]), 1, 0)
    out = jnp.moveaxis(lax.map(fn, qb), 0, 1)
    return out.reshape((bsz, L) + out.shape[3:])


def diff_attention(q, k, v, lam):
    s = jnp.einsum('bqhcd,bkhcd->bhcqk', q, k).astype(F32) * (DIFF_DH ** -0.5)
    a = jax.nn.softmax(s, axis=-1)
    w = a[:, :, 0] - lam * a[:, :, 1]
    return jnp.einsum('bhqk,bkhd->bqhd', w.astype(v.dtype), v)


def softmax_attention(q, k, v):
    s = jnp.einsum('bqhd,bkhd->bhqk', q, k).astype(F32) * (MLA_QK ** -0.5)
    a = jax.nn.softmax(s, axis=-1)
    return jnp.einsum('bhqk,bkhd->bqhd', a.astype(v.dtype), v)


def centred_depthwise_conv(u, w, b):
    k = w.shape[0]
    out = lax.conv_general_dilated(u, w[:, None, :].astype(u.dtype), window_strides=(1,),
                                   padding=[((k - 1) // 2, k // 2)],
                                   dimension_numbers=('NWC', 'WIO', 'NWC'),
                                   feature_group_count=u.shape[-1])
    return out + b


def ssd_chunked_scan(xs, dt, A, Bm, Cm, h0):
    bsz, L, H, P = xs.shape
    G, N = Bm.shape[2:]
    R = H // G
    nc = L // SSD_CHUNK
    x = xs.reshape(bsz, nc, SSD_CHUNK, G, R, P)
    Bc = Bm.reshape(bsz, nc, SSD_CHUNK, G, N)
    Cc = Cm.reshape(bsz, nc, SSD_CHUNK, G, N)
    dtc = dt.reshape(bsz, nc, SSD_CHUNK, G, R)
    a_cum = jnp.cumsum(dtc * A.reshape(G, R), axis=2)
    seg = a_cum[:, :, :, None] - a_cum[:, :, None, :]
    lower = jnp.tril(jnp.ones((SSD_CHUNK, SSD_CHUNK), bool))[:, :, None, None]
    decay = jnp.exp(jnp.where(lower, seg, -jnp.inf))
    cb = jnp.einsum('bcign,bcjgn->bcijg', Cc, Bc)
    mix = cb[..., None] * decay * dtc[:, :, None]
    y_diag = jnp.einsum('bcijgr,bcjgrp->bcigrp', mix, x)
    w_end = jnp.exp(a_cum[:, :, -1:] - a_cum) * dtc
    states = jnp.einsum('bcjgn,bcjgrp->bcgrpn', Bc, x * w_end[..., None])
    chunk_decay = jnp.exp(a_cum[:, :, -1])

    def step(h, inp):
        s_c, d_c = inp
        return h * d_c[..., None, None] + s_c, h

    h_last, h_in = lax.scan(step, h0, (jnp.moveaxis(states, 1, 0), jnp.moveaxis(chunk_decay, 1, 0)))
    h_in = jnp.moveaxis(h_in, 0, 1)
    y_off = jnp.einsum('bcign,bcgrpn->bcigrp', Cc, h_in) * jnp.exp(a_cum)[..., None]
    return (y_diag + y_off).reshape(bsz, L, H, P), h_last


def branch_inputs(h, p, rope_diff, rope_mla):
    bsz, L = h.shape[:2]
    dq, dk, dv, z, xbc, dt, cq, ckv, kr, gates = jnp.split(h @ p['w_in'], _split_points(IN_SIZES), axis=-1)
    dq = rms_norm(dq.reshape(bsz, L, DIFF_HEADS, 2, DIFF_DH), p['diff_q_g'])
    dk = rms_norm(dk.reshape(bsz, L, DIFF_HEADS, 2, DIFF_DH), p['diff_k_g'])
    dv = dv.reshape(bsz, L, DIFF_HEADS, DIFF_VD)
    q = (rms_norm(cq, p['mla_cq_g']) @ p['w_uq']).reshape(bsz, L, MLA_HEADS, MLA_QK)
    kv = (rms_norm(ckv, p['mla_ckv_g']) @ p['w_ukv']).reshape(bsz, L, MLA_HEADS, MLA_NOPE + MLA_V)
    gq, gk = p['mla_q_g'], p['mla_k_g']
    q_nope = rms_norm(q[..., :MLA_NOPE], gq[:MLA_NOPE])
    q_rope = rms_norm(q[..., MLA_NOPE:], gq[MLA_NOPE:])
    k_nope = rms_norm(kv[..., :MLA_NOPE], gk[:MLA_NOPE])
    k_rope = rms_norm(kr, gk[MLA_NOPE:])
    mv = kv[..., MLA_NOPE:]
    if rope_diff is not None:
        dq = apply_axial_rope(dq, *rope_diff)
        dk = apply_axial_rope(dk, *rope_diff)
        q_rope = apply_axial_rope(q_rope, *rope_mla)
        k_rope = apply_axial_rope(k_rope, *rope_mla)
    mq = jnp.concatenate([q_nope, q_rope], axis=-1)
    mk = jnp.concatenate([k_nope, jnp.broadcast_to(k_rope[:, :, None, :], (bsz, L, MLA_HEADS, MLA_ROPE))], axis=-1)
    return {'dq': dq, 'dk': dk, 'dv': dv, 'z': z, 'xbc': xbc, 'dt': dt,
            'mq': mq, 'mk': mk, 'mv': mv, 'gates': gates}


def ssd_mixer(lat, ctx, p, need_ctx):
    A = -jnp.exp(p['ssd_A_log'].astype(F32))
    d_skip = p['ssd_D'].astype(F32)[:, None]

    def prep(s):
        u = jax.nn.silu(centred_depthwise_conv(s['xbc'], p['ssd_conv_w'], p['ssd_conv_b']))
        bsz, L = u.shape[:2]
        xs, Bm, Cm = jnp.split(u, [SSD_INNER, SSD_INNER + SSD_GROUPS * SSD_STATE], axis=-1)
        xs = xs.reshape(bsz, L, SSD_HEADS, SSD_P)
        Bm = Bm.reshape(bsz, L, SSD_GROUPS, SSD_STATE)
        Cm = Cm.reshape(bsz, L, SSD_GROUPS, SSD_STATE)
        dt = jax.nn.softplus(s['dt'].astype(F32).reshape(bsz, L, 2, SSD_HEADS) + p['ssd_dt_bias'].astype(F32))
        return xs, dt, Bm, Cm

    def bidir(xs, dt, Bm, Cm, init_f, init_b):
        flip = lambda t: jnp.flip(t, axis=1)
        y_f, s_f = ssd_chunked_scan(xs, dt[:, :, 0], A[0], Bm, Cm, init_f)
        y_b, s_b = ssd_chunked_scan(flip(xs), flip(dt[:, :, 1]), A[1], flip(Bm), flip(Cm), init_b)
        return y_f + flip(y_b) + d_skip * xs, s_f, s_b

    def gated_out(y, z):
        y = y.reshape(z.shape).astype(z.dtype)
        return rms_norm(y * jax.nn.silu(z), p['ssd_norm_g'])

    xc = prep(ctx)
    zero = jnp.zeros((xc[0].shape[0], SSD_GROUPS, SSD_HEADS // SSD_GROUPS, SSD_P, SSD_STATE), F32)
    y_c, s_f, s_b = bidir(*xc, zero, zero)
    y_l, _, _ = bidir(*prep(lat), s_f, s_b)
    out_lat = gated_out(y_l, lat['z'])
    out_ctx = gated_out(y_c, ctx['z']) if need_ctx else None
    return out_lat, out_ctx


def merge_branches(outs, gate_logits, w_branch, w_out):
    gates = jax.nn.sigmoid(gate_logits)
    acc = gates[..., :D_MODEL] * (outs[0] @ w_branch[0])
    for k in range(1, N_BRANCH):
        acc = acc + gates[..., k * D_MODEL:(k + 1) * D_MODEL] * (outs[k] @ w_branch[k])
    return acc @ w_out


def token_mixer(h_lat, h_ctx, p, lam_init, rope_diff, rope_mla, need_ctx):
    lat = branch_inputs(h_lat, p, rope_diff, rope_mla)
    ctx = branch_inputs(h_ctx, p, None, None)
    lv = p['diff_lambda'].astype(F32)
    lam = jnp.exp(jnp.sum(lv[0] * lv[1])) - jnp.exp(jnp.sum(lv[2] * lv[3])) + lam_init

    def diff_post(o):
        o = rms_norm(o, p['diff_subln_g']) * (1.0 - lam_init)
        return o.reshape(o.shape[:2] + (DIFF_V,))

    def mla_post(o):
        return o.reshape(o.shape[:2] + (MLA_HEADS * MLA_V,))

    dk_all = jnp.concatenate([ctx['dk'], lat['dk']], axis=1)
    dv_all = jnp.concatenate([ctx['dv'], lat['dv']], axis=1)
    mk_all = jnp.concatenate([ctx['mk'], lat['mk']], axis=1)
    mv_all = jnp.concatenate([ctx['mv'], lat['mv']], axis=1)
    diff_lat = sweep_query_blocks(lambda qb: diff_attention(qb, dk_all, dv_all, lam), lat['dq'])
    mla_lat = sweep_query_blocks(lambda qb: softmax_attention(qb, mk_all, mv_all), lat['mq'])
    ssd_lat, ssd_ctx = ssd_mixer(lat, ctx, p, need_ctx)
    y_lat = merge_branches((diff_post(diff_lat), ssd_lat, mla_post(mla_lat)), lat['gates'], p['w_branch'], p['w_out'])
    if not need_ctx:
        return y_lat, None
    diff_ctx = diff_attention(ctx['dq'], ctx['dk'], ctx['dv'], lam)
    mla_ctx = softmax_attention(ctx['mq'], ctx['mk'], ctx['mv'])
    y_ctx = merge_branches((diff_post(diff_ctx), ssd_ctx, mla_post(mla_ctx)), ctx['gates'], p['w_branch'], p['w_out'])
    return y_lat, y_ctx


def routed_expert_ffn(t, e_idx, weights, w_gate, w_up, w_down):
    T, D = t.shape
    K = e_idx.shape[1]
    E = w_gate.shape[0]
    flat_e = e_idx.reshape(-1)
    flat_tok = jnp.repeat(jnp.arange(T, dtype=jnp.int32), K)
    flat_w = weights.reshape(-1)
    order = jnp.argsort(flat_e)
    e_sorted = flat_e[order]
    counts = jnp.bincount(flat_e, length=E)
    padded = (counts + MOE_BLOCK - 1) // MOE_BLOCK * MOE_BLOCK
    pad_end = jnp.cumsum(padded)
    pad_start = pad_end - padded
    start = jnp.cumsum(counts) - counts
    slot = pad_start[e_sorted] + jnp.arange(T * K, dtype=jnp.int32) - start[e_sorted]
    n_blocks = -(-(T * K) // MOE_BLOCK) + E
    P = n_blocks * MOE_BLOCK
    tok_buf = jnp.zeros((P,), jnp.int32).at[slot].set(flat_tok[order])
    w_buf = jnp.zeros((P,), t.dtype).at[slot].set(flat_w[order])
    blk_expert = jnp.minimum(jnp.searchsorted(pad_end, jnp.arange(n_blocks, dtype=jnp.int32) * MOE_BLOCK, side='right'), E - 1)
    xb = t[tok_buf].reshape(n_blocks, MOE_BLOCK, D)

    def expert_block(args):
        xblk, e = args
        hid = jax.nn.silu(xblk @ w_gate[e]) * (xblk @ w_up[e])
        return hid @ w_down[e]

    yb = lax.map(expert_block, (xb, blk_expert)).reshape(P, D)
    return jnp.zeros_like(t).at[tok_buf].add(yb * w_buf[:, None])


def hierarchical_moe(t, group_w, group_b, expert_w, expert_b, w_gate, w_up, w_down):
    T = t.shape[0]
    p_group = jax.nn.softmax((t @ group_w).astype(F32) + group_b.astype(F32), axis=-1)
    pg, g_idx = lax.top_k(p_group, 1)
    e_logits = ((t @ expert_w).astype(F32) + expert_b.astype(F32)).reshape(T, MOE_GROUPS, MOE_PER_GROUP)
    in_group = e_logits[jnp.arange(T), g_idx[:, 0]]
    pe, e_loc = lax.top_k(jax.nn.softmax(in_group, axis=-1), MOE_TOP_K)
    weights = pg * pe / jnp.sum(pe, axis=-1, keepdims=True)
    e_idx = g_idx * MOE_PER_GROUP + e_loc
    return routed_expert_ffn(t, e_idx, weights.astype(t.dtype), w_gate, w_up, w_down)


def setup_inputs(seed: int = 0) -> dict:
    key = jax.random.key(seed)
    ks = iter(jax.random.split(key, 48))
    L = DEPTH

    def nrm(shape, scale):
        return jax.random.normal(next(ks), shape, F32) * scale

    def gain(shape):
        return 1.0 + nrm(shape, 0.02)

    dt0 = jnp.exp(jax.random.uniform(next(ks), (L, 2, SSD_HEADS), F32, math.log(1e-3), math.log(1e-1)))
    dt_bias = dt0 + jnp.log(-jnp.expm1(-dt0))
    a_log = jnp.log(jax.random.uniform(next(ks), (L, 2, SSD_HEADS), F32, 1.0, 16.0))
    return {
        'x': nrm((BATCH, SEQ, D_MODEL), 1.0),
        'c': nrm((BATCH, D_MODEL), 1.0),
        'ctx': nrm((BATCH, CTX_LEN, D_MODEL), 1.0),
        'c_ctx': nrm((D_MODEL,), 1.0),
        'ada_w': nrm((L, D_MODEL, 6 * D_MODEL), 0.5 * D_MODEL ** -0.5),
        'ada_b': nrm((L, 6 * D_MODEL), 0.02),
        'norm1_g': gain((L, D_MODEL)),
        'norm2_g': gain((L, D_MODEL)),
        'w_in': nrm((L, D_MODEL, D_IN), D_MODEL ** -0.5),
        'diff_q_g': gain((L, DIFF_DH)),
        'diff_k_g': gain((L, DIFF_DH)),
        'diff_lambda': nrm((L, 4, DIFF_DH), 0.1),
        'diff_subln_g': gain((L, DIFF_VD)),
        'ssd_conv_w': nrm((L, SSD_CONV, SSD_CONV_CH), SSD_CONV ** -0.5),
        'ssd_conv_b': nrm((L, SSD_CONV_CH), 0.02),
        'ssd_dt_bias': dt_bias,
        'ssd_A_log': a_log,
        'ssd_D': gain((L, SSD_HEADS)),
        'ssd_norm_g': gain((L, SSD_INNER)),
        'mla_cq_g': gain((L, MLA_Q_LORA)),
        'mla_ckv_g': gain((L, MLA_KV_LORA)),
        'w_uq': nrm((L, MLA_Q_LORA, MLA_HEADS * MLA_QK), MLA_Q_LORA ** -0.5),
        'w_ukv': nrm((L, MLA_KV_LORA, MLA_HEADS * (MLA_NOPE + MLA_V)), MLA_KV_LORA ** -0.5),
        'mla_q_g': gain((L, MLA_QK)),
        'mla_k_g': gain((L, MLA_QK)),
        'w_branch': nrm((L, N_BRANCH, BRANCH_W, D_MODEL), BRANCH_W ** -0.5),
        'w_out': nrm((L, D_MODEL, D_MODEL), D_MODEL ** -0.5),
        'moe_group_w': nrm((L, D_MODEL, MOE_GROUPS), D_MODEL ** -0.5),
        'moe_group_b': nrm((L, MOE_GROUPS), 0.01),
        'moe_expert_w': nrm((L, D_MODEL, MOE_EXPERTS), D_MODEL ** -0.5),
        'moe_expert_b': nrm((L, MOE_EXPERTS), 0.01),
        'moe_w_gate': nrm((L, MOE_EXPERTS, D_MODEL, MOE_HIDDEN), D_MODEL ** -0.5),
        'moe_w_up': nrm((L, MOE_EXPERTS, D_MODEL, MOE_HIDDEN), D_MODEL ** -0.5),
        'moe_w_down': nrm((L, MOE_EXPERTS, MOE_HIDDEN, D_MODEL), MOE_HIDDEN ** -0.5),
    }


def reference(x, c, ctx, c_ctx, ada_w, ada_b, norm1_g, norm2_g, w_in, diff_q_g, diff_k_g, diff_lambda,
              diff_subln_g, ssd_conv_w, ssd_conv_b, ssd_dt_bias, ssd_A_log, ssd_D, ssd_norm_g, mla_cq_g,
              mla_ckv_g, w_uq, w_ukv, mla_q_g, mla_k_g, w_branch, w_out, moe_group_w, moe_group_b,
              moe_expert_w, moe_expert_b, moe_w_gate, moe_w_up, moe_w_down):
    bsz, S, D = x.shape
    ROWS = S // GRID_W
    rope_diff = axial_rope_tables(ROWS, DIFF_DH)
    rope_mla = axial_rope_tables(ROWS, MLA_ROPE)
    x_ctx = ctx
    for l in range(DEPTH):
        last = l == DEPTH - 1
        lam_init = 0.8 - 0.6 * math.exp(-0.3 * l)
        p = {'w_in': w_in[l], 'diff_q_g': diff_q_g[l], 'diff_k_g': diff_k_g[l], 'diff_lambda': diff_lambda[l],
             'diff_subln_g': diff_subln_g[l], 'ssd_conv_w': ssd_conv_w[l], 'ssd_conv_b': ssd_conv_b[l],
             'ssd_dt_bias': ssd_dt_bias[l], 'ssd_A_log': ssd_A_log[l], 'ssd_D': ssd_D[l],
             'ssd_norm_g': ssd_norm_g[l], 'mla_cq_g': mla_cq_g[l], 'mla_ckv_g': mla_ckv_g[l],
             'w_uq': w_uq[l], 'w_ukv': w_ukv[l], 'mla_q_g': mla_q_g[l], 'mla_k_g': mla_k_g[l],
             'w_branch': w_branch[l], 'w_out': w_out[l]}
        mod_lat = (jax.nn.silu(c) @ ada_w[l] + ada_b[l])[:, None, :]
        mod_ctx = jax.nn.silu(c_ctx) @ ada_w[l] + ada_b[l]
        sh1, sc1, g1, sh2, sc2, g2 = jnp.split(mod_lat, 6, axis=-1)
        csh1, csc1, cg1, csh2, csc2, cg2 = jnp.split(mod_ctx, 6, axis=-1)

        h_lat = modulate(rms_norm(x, norm1_g[l]), sh1, sc1)
        h_ctx = modulate(rms_norm(x_ctx, norm1_g[l]), csh1, csc1)
        y_lat, y_ctx = token_mixer(h_lat, h_ctx, p, lam_init, rope_diff, rope_mla, not last)
        x = x + g1 * y_lat
        h2 = modulate(rms_norm(x, norm2_g[l]), sh2, sc2).reshape(-1, D)
        moe_args = (moe_group_w[l], moe_group_b[l], moe_expert_w[l], moe_expert_b[l],
                    moe_w_gate[l], moe_w_up[l], moe_w_down[l])
        if last:
            x = x + g2 * hierarchical_moe(h2, *moe_args).reshape(bsz, S, D)
        else:
            x_ctx = x_ctx + cg1 * y_ctx
            h2c = modulate(rms_norm(x_ctx, norm2_g[l]), csh2, csc2).reshape(-1, D)
            f = hierarchical_moe(jnp.concatenate([h2, h2c], axis=0), *moe_args)
            x = x + g2 * f[:bsz * S].reshape(bsz, S, D)
            x_ctx = x_ctx + cg2 * f[bsz * S:].reshape(x_ctx.shape)
    return x
```

```python
import math
from contextlib import ExitStack
import numpy as np
import concourse.bass as bass
import concourse.mybir as mybir
from concourse.bass_utils import run_bass_kernel_spmd

F32 = mybir.dt.float32
BF16 = mybir.dt.bfloat16
I32 = mybir.dt.int32
U32 = mybir.dt.uint32
AF = mybir.ActivationFunctionType
ALU = mybir.AluOpType
AX = mybir.AxisListType

EPOCH = 20000
NDMASEM = 8
D = 1024
CTX = 256
EPS = 1e-6
NEG = -30000.0
BLK_CUT = 9
WT_RING = 3
NBLK_DBG = None
NBLK_START = 0
XLOAD = True


class Prog:
    CE = ("pe", "dve", "act", "pool")
    QE = ("sp", "pool", "act")

    def __init__(self, nc):
        self.nc = nc
        self.ops = {e: [] for e in ("pe", "dve", "act", "pool", "sp")}
        self.cnt = {e: 0 for e in self.CE}
        self.dcnt = {q: 0 for q in self.QE}
        self.seen = {e: {} for e in self.ops}
        self.last_w = {}
        self.readers = {}
        self.latest = {}
        self.nsem = set()
        self.sems = {}

    def _tok_compute(self, eng):
        k = self.cnt[eng]
        self.cnt[eng] += 1
        return (("c", eng, k // EPOCH), (k % EPOCH) + 1)

    def _tok_dma(self, q):
        k = self.dcnt[q]
        self.dcnt[q] += 1
        return (("d", q, k % NDMASEM), 16 * (k // NDMASEM + 1))

    def _filter(self, eng, need):
        out = []
        seen = self.seen[eng]
        for sk, v in need.items():
            if sk[0] == "c":
                hi = max([s[2] for s in seen if s[0] == "c" and s[1] == sk[1]] + [-1])
                if hi > sk[2]:
                    continue
            if seen.get(sk, 0) >= v:
                continue
            seen[sk] = v
            out.append((sk, v))
        return out

    def _deps(self, eng, reads, writes):
        need = {}

        def add(tok):
            if tok is None:
                return
            sk, v = tok
            if sk[0] == "c" and sk[1] == eng and eng == "pe":
                return
            if need.get(sk, 0) < v:
                need[sk] = v

        for r in reads:
            add(self.last_w.get(r))
        for w in writes:
            add(self.last_w.get(w))
            for sk, v in self.readers.get(w, {}).items():
                add((sk, v))
        return self._filter(eng, need)

    def _commit(self, tok, reads, writes):
        sk, v = tok
        self.latest[sk] = max(self.latest.get(sk, 0), v)
        self.nsem.add(sk)
        for r in reads:
            d = self.readers.setdefault(r, {})
            if d.get(sk, 0) < v:
                d[sk] = v
        for w in writes:
            self.last_w[w] = tok
            self.readers[w] = {}

    def op(self, eng, fn, reads=(), writes=()):
        waits = self._deps(eng, reads, writes)
        tok = self._tok_compute(eng)
        self.ops[eng].append((waits, fn, tok))
        self._commit(tok, reads, writes)

    def dma(self, q, out, in_, reads=(), writes=()):
        waits = self._deps(q, reads, writes)
        tok = self._tok_dma(q)
        self.ops[q].append((waits, (lambda e, o=out, i=in_: e.dma_start(out=o, in_=i)), tok))
        self._commit(tok, reads, writes)

    def idma(self, out, out_off, in_, in_off, reads=(), writes=()):
        waits = self._deps("pool", reads, writes)
        tok = self._tok_dma("pool")
        self.ops["pool"].append((waits, (lambda e: e.indirect_dma_start(out=out, out_offset=out_off, in_=in_, in_offset=in_off)), tok))
        self._commit(tok, reads, writes)

    def barrier(self):
        need = {}
        for sk, v in self.latest.items():
            if sk[0] == "c":
                hi = max(s[2] for s in self.latest if s[0] == "c" and s[1] == sk[1])
                if sk[2] < hi:
                    continue
            need[sk] = v
        for eng in self.ops:
            w = self._filter(eng, {sk: v for sk, v in need.items() if not (sk[0] == "c" and sk[1] == eng)})
            if w:
                self.ops[eng].append((w, None, None))
        self.last_w = {}
        self.readers = {}

    def emit(self, stack):
        nc = self.nc
        for sk in sorted(self.nsem, key=str):
            self.sems[sk] = stack.enter_context(nc.semaphore("s_" + "_".join(str(x) for x in sk)))
        block = stack.enter_context(nc.Block())
        sems = self.sems

        def run(eobj, lst):
            for waits, fn, tok in lst:
                for sk, v in waits:
                    eobj.wait_ge(sems[sk], v)
                if fn is not None:
                    fn(eobj).then_inc(sems[tok[0]], 16 if tok[0][0] == "d" else 1)

        ops = self.ops

        @block.tensor
        def _(e):
            run(e, ops["pe"])

        @block.vector
        def _(e):
            run(e, ops["dve"])

        @block.scalar
        def _(e):
            run(e, ops["act"])

        @block.gpsimd
        def _(e):
            run(e, ops["pool"])

        @block.sync
        def _(e):
            run(e, ops["sp"])


class Buf:
    def __init__(self, t, k):
        self.t = t
        self.k = k

    def __getitem__(self, idx):
        return self.t[idx]


class Job:
    def __init__(self, gen):
        self.gen = gen
        self.done = False


class Sched:
    cur = None


class Ring:
    def __init__(self, bufs, transient=False):
        self.bufs = bufs
        self.i = 0
        self.transient = transient

    def next(self):
        b = self.bufs[self.i % len(self.bufs)]
        self.i += 1
        if self.transient:
            return b
        own = getattr(b, "owner", None)
        if own is not None and not own.done and own is not Sched.cur:
            raise RuntimeError(f"ring too small: buffer {b.k} still owned by a live job")
        if not isinstance(b, int):
            b.owner = Sched.cur
        return b


def run_jobs(gens):
    active = []
    pending = list(gens)
    while pending or active:
        order = []
        if pending:
            order.append(Job(pending.pop(0)))
        order += list(reversed(active))
        for j in order:
            Sched.cur = j
            try:
                next(j.gen)
                if j not in active:
                    active.append(j)
            except StopIteration:
                j.done = True
                if j in active:
                    active.remove(j)
        Sched.cur = None


SM = {}
_c = 0
for _n, _w in [("n1g", 8), ("n2g", 8), ("adab", 48), ("dqg", 1), ("dkg", 1), ("subg", 1), ("convw", 40), ("convb", 8),
               ("cqg", 3), ("ckvg", 2), ("mqg", 1), ("mkg", 1), ("mkrg", 1)]:
    SM[_n] = (_c, _w)
    _c += _w
NSM = _c
RW = {}
_c = 0
for _n, _w in [("dtb", 16), ("alog", 16), ("ssdD", 8), ("ssdg", 512), ("rb", 36), ("lam", 256)]:
    RW[_n] = (_c, _w)
    _c += _w
NRW = _c
CM = {}
for _i, _n in enumerate(["ident", "rdT", "rmT", "triU", "negTriU", "strictL", "negmF", "triL", "negTriL", "strictU", "negmB"]):
    CM[_n] = _i
NCM = len(CM)
NMC = 140


def _rot_matrix(dim):
    q = dim // 4
    R = np.zeros((dim, dim), np.float32)
    for i in range(q):
        R[i, q + i] = -1.0
        R[q + i, i] = 1.0
        R[2 * q + i, 3 * q + i] = -1.0
        R[3 * q + i, 2 * q + i] = 1.0
    return R


def _rope_tables(S, dim):
    q = dim // 4
    inv = (10000.0 ** (-np.arange(q, dtype=np.float32) / q)).astype(np.float32)
    rows = S // 64
    r = np.repeat(np.arange(rows, dtype=np.float32), 64)
    cc = np.tile(np.arange(64, dtype=np.float32), rows)
    ar = r[:, None] * inv
    ac = cc[:, None] * inv
    ang = np.concatenate([ar, ar, ac, ac], axis=-1)
    return np.cos(ang).astype(np.float32).T, np.sin(ang).astype(np.float32).T


def host_consts(S):
    T = CTX + S
    cm = np.zeros((NCM, 128, 128), np.float32)
    cm[CM["ident"]] = np.eye(128, dtype=np.float32)
    R64 = _rot_matrix(64)
    rd = np.zeros((128, 128), np.float32)
    rd[:64, :64] = R64
    rd[64:, 64:] = R64
    cm[CM["rdT"]] = rd.T
    rm = np.zeros((128, 128), np.float32)
    rm[64:96, 64:96] = _rot_matrix(32)
    cm[CM["rmT"]] = rm.T
    t = np.arange(128)
    triU = (t[:, None] <= t[None, :]).astype(np.float32)
    triL = (t[:, None] >= t[None, :]).astype(np.float32)
    cm[CM["triU"]] = triU
    cm[CM["negTriU"]] = -triU
    cm[CM["strictL"]] = (t[:, None] > t[None, :]).astype(np.float32)
    cm[CM["negmF"]] = NEG * (t[:, None] > t[None, :])
    cm[CM["triL"]] = triL
    cm[CM["negTriL"]] = -triL
    cm[CM["strictU"]] = (t[:, None] < t[None, :]).astype(np.float32)
    cm[CM["negmB"]] = NEG * (t[:, None] < t[None, :])
    cmat = np.ascontiguousarray(cm.transpose(1, 0, 2))
    rope = np.zeros((6, 128, T), np.float32)
    cd, sd = _rope_tables(S, 64)
    rope[0, :, :CTX] = 1.0
    rope[0, :64, CTX:] = cd
    rope[0, 64:, CTX:] = cd
    rope[1, :64, CTX:] = sd
    rope[1, 64:, CTX:] = sd
    cmm, smm = _rope_tables(S, 32)
    rope[2, :, :] = 1.0
    rope[2, 64:96, CTX:] = cmm
    rope[3, 64:96, CTX:] = smm
    rope[4, :, :CTX] = 1.0
    rope[4, :32, CTX:] = cmm
    rope[5, :32, CTX:] = smm
    sel = np.zeros((64, 32, 128), np.float32)
    for e in range(32):
        sel[e, e, :] = 1.0
        sel[32 + e, e, :] = 1.0
    mc = np.zeros((128, NMC), np.float32)
    mc[:, 0:34] = 128.0 * np.arange(34, dtype=np.float32)[None, :]
    mc[:, 34:34 + 104] = np.arange(104, dtype=np.float32)[None, :]
    mc[:, 138] = np.arange(128, dtype=np.float32)
    return cmat, rope, sel, mc


def pack_smalls(inp, l):
    s = np.zeros((128, NSM), np.float32)

    def put(name, arr):
        c0, w = SM[name]
        s[: arr.shape[0], c0:c0 + w] = arr.reshape(arr.shape[0], w)

    put("n1g", inp["norm1_g"][l].reshape(8, 128).T)
    put("n2g", inp["norm2_g"][l].reshape(8, 128).T)
    put("adab", inp["ada_b"][l].reshape(48, 128).T)
    put("dqg", np.tile(inp["diff_q_g"][l], 2)[:, None])
    put("dkg", np.tile(inp["diff_k_g"][l], 2)[:, None])
    put("subg", inp["diff_subln_g"][l][:, None])
    put("convw", inp["ssd_conv_w"][l].T.reshape(8, 128, 5).transpose(1, 0, 2).reshape(128, 40))
    put("convb", inp["ssd_conv_b"][l].reshape(8, 128).T)
    put("cqg", inp["mla_cq_g"][l].reshape(3, 128).T)
    put("ckvg", inp["mla_ckv_g"][l].reshape(2, 128).T)
    put("mqg", inp["mla_q_g"][l][:, None])
    put("mkg", inp["mla_k_g"][l][:64][:, None])
    put("mkrg", inp["mla_k_g"][l][64:][:, None])
    return s


def pack_rows(inp, l):
    r = np.zeros((NRW,), np.float32)

    def put(name, arr):
        c0, w = RW[name]
        r[c0:c0 + w] = arr.reshape(-1)

    put("dtb", inp["ssd_dt_bias"][l])
    put("alog", inp["ssd_A_log"][l])
    put("ssdD", inp["ssd_D"][l])
    put("ssdg", inp["ssd_norm_g"][l])
    put("rb", np.concatenate([inp["moe_group_b"][l], inp["moe_expert_b"][l]]))
    put("lam", inp["diff_lambda"][l])
    return r


def build_program(S, depth, dbg=(), stop_after=None):
    T = CTX + S
    NT = T // 128
    nc = bass.Bass("TRN2", target_bir_lowering=False)
    P = Prog(nc)
    dram_in = {}

    def din(name, shape, dt=F32):
        dram_in[name] = nc.dram_tensor(name, list(shape), dt, kind="ExternalInput").ap()
        return dram_in[name]

    xT = din("xT", [D, T])
    cmod_d = din("cmod", [128, 8, 2])
    ada_w = din("ada_w", [depth, D, 6 * D])
    w_in = din("w_in", [depth, D, 6832])
    w_uq = din("w_uq", [depth, 384, 768])
    w_ukv = din("w_ukv", [depth, 256, 1024])
    w_branch = din("w_branch", [depth, 3, 512, D])
    w_out = din("w_out", [depth, D, D])
    router_w = din("router_w", [depth, D, 36])
    moe_wg = din("moe_w_gate", [depth, 32, D, 256])
    moe_wu = din("moe_w_up", [depth, 32, D, 256])
    moe_wd = din("moe_w_down", [depth, 32, 256, D])
    smalls_d = din("smalls", [depth, 128, NSM])
    rows_d = din("rows", [depth, NRW])
    cmat_d = din("cmat", [128, NCM, 128])
    rope_d = din("rope", [6, 128, T])
    sel_d = din("sel", [64, 32, 128])
    mconst_d = din("mconst", [128, NMC])
    yT = nc.dram_tensor("yT", [D, S], F32, kind="ExternalOutput").ap()

    scr = {}

    def dscr(name, shape, dt):
        kind = "ExternalOutput" if name in dbg else "Internal"
        scr[name] = nc.dram_tensor("scr_" + name, list(shape), dt, kind=kind).ap()
        return scr[name]

    NBMAX = 2 * NT + 32
    WBs = dscr("WB", [32 * 128, 6144], BF16)
    XBs = dscr("XB", [NBMAX * 128, D], BF16)
    YBs = dscr("YB", [NBMAX * 128, D], F32)
    XM = dscr("XM", [D, T], F32)
    XR = dscr("XR", [D, T], F32)
    Hs = dscr("H", [D, T], BF16)
    DQ = dscr("DQ", [4, 128, T], BF16)
    DK = dscr("DK", [4, 128, T], BF16)
    DV = dscr("DV", [T, 512], BF16)
    Zs = dscr("Z", [T, 512], BF16)
    XBC = dscr("XBC", [D, T], BF16)
    DTs = dscr("DT", [T, 16], F32)
    MQ = dscr("MQ", [8, 96, T], BF16)
    MK = dscr("MK", [8, 96, T], BF16)
    MV = dscr("MV", [T, 512], BF16)
    DOs = dscr("DO", [512, T], BF16)
    MOs = dscr("MO", [512, T], BF16)
    SOs = dscr("SO", [512, T], BF16)
    YF = dscr("YF", [T, 512], F32)
    XSs = dscr("XS", [T, 512], BF16)
    BTs = dscr("BT", [T, 256], BF16)
    H2s = dscr("H2", [D, T], BF16)
    CWT = dscr("CWT", [64, T], BF16)
    H2TM = dscr("H2TM", [T, D], BF16)

    SB_LO, SB_HI = 16640, 229376 - 64
    st = {"off": SB_LO, "n": 0}

    def sb(name, shape, dt=F32):
        nbytes = int(np.prod(shape[1:])) * (2 if dt == BF16 else 4)
        nbytes = (nbytes + 63) // 64 * 64
        off = st["off"]
        assert off + nbytes <= st.get("hi", SB_HI), f"SBUF overflow allocating {name}: {off}+{nbytes}"
        st["off"] = off + nbytes
        st["n"] += 1
        t = nc.alloc_sbuf_tensor_at(f"{name}_{st['n']}", list(shape), dt, offset=off)
        return Buf(t, f"{name}_{st['n']}")

    META_BYTES = 5632
    SB_META = SB_HI - META_BYTES

    def meta_bufs(tag):
        off = SB_META
        out = []
        for nm, shape, dt in (("MK1", [128, NT, 32], BF16), ("MK2", [128, NT, 32], BF16), ("W12", [128, NT, 2], F32),
                              ("RK", [128, NT, 2], F32), ("Macc", [128, 32], F32)):
            nb = (int(np.prod(shape[1:])) * (2 if dt == BF16 else 4) + 63) // 64 * 64
            st["n"] += 1
            out.append(Buf(nc.alloc_sbuf_tensor_at(f"{nm}_{tag}_{st['n']}", list(shape), dt, offset=off), f"{nm}"))
            off += nb
        assert off <= SB_HI
        return out

    def sbring(name, n, shape, dt=F32):
        return Ring([sb(f"{name}{i}", shape, dt) for i in range(n)])

    pbanks = [Buf(nc.alloc_psum_tensor(f"pb{i}", [128, 512], F32), f"pb{i}") for i in range(8)]

    def MM(out, lhsT, rhs, start, stop, r, w):
        P.op("pe", lambda e: e.matmul(out, lhsT=lhsT, rhs=rhs, start=start, stop=stop), r, w)

    def TR(out, in_, ident, r, w):
        P.op("pe", lambda e: e.transpose(out, in_, ident), r, w)

    def ACT(out, in_, func, r, w, **kw):
        P.op("act", lambda e: e.activation(out=out, in_=in_, func=func, **kw), r, w)

    def TT(eng, out, in0, in1, op, r, w):
        P.op(eng, lambda e: e.tensor_tensor(out=out, in0=in0, in1=in1, op=op), r, w)

    def TS(eng, out, in0, s1, s2, op0, op1, r, w):
        if s2 is None:
            P.op(eng, lambda e: e.tensor_scalar(out=out, in0=in0, scalar1=s1, scalar2=None, op0=op0), r, w)
        else:
            P.op(eng, lambda e: e.tensor_scalar(out=out, in0=in0, scalar1=s1, scalar2=s2, op0=op0, op1=op1), r, w)

    def STT(eng, out, in0, scalar, in1, op0, op1, r, w):
        P.op(eng, lambda e: e.scalar_tensor_tensor(out=out, in0=in0, scalar=scalar, in1=in1, op0=op0, op1=op1), r, w)

    def CP(eng, out, in_, r, w):
        if eng == "act":
            P.op("act", lambda e: e.copy(out=out, in_=in_), r, w)
        else:
            P.op(eng, lambda e: e.tensor_copy(out=out, in_=in_), r, w)

    def RECIP(out, in_, r, w):
        P.op("dve", lambda e: e.reciprocal(out=out, in_=in_), r, w)

    def MEMSET(eng, ap, val, w):
        P.op(eng, lambda e: e.memset(ap, val), (), w)

    cmat = sb("cmat", [128, NCM, 128])
    P.dma("sp", cmat[:], cmat_d, (), [cmat.k])
    cmatb = sb("cmatb", [128, 3, 128], BF16)
    P.dma("pool", cmatb[:], cmat_d[:, 0:3, :], (), [cmatb.k])
    ident_f = cmat[:, CM["ident"], :]
    ident_b = cmatb[:, 0, :]
    rdT_b = cmatb[:, 1, :]
    rmT_b = cmatb[:, 2, :]
    ones_b = sb("ones_b", [128, 128], BF16)
    MEMSET("dve", ones_b[:], 1.0, [ones_b.k])
    ones_f = sb("ones_f", [128, 128])
    MEMSET("dve", ones_f[:], 1.0, [ones_f.k])
    mean128_b = sb("mean128", [128, 128], BF16)
    MEMSET("dve", mean128_b[:], 1.0 / 128, [mean128_b.k])
    blk64_b = sb("blk64", [128, 128], BF16)
    MEMSET("dve", blk64_b[:], 0.0, [blk64_b.k])
    MEMSET("dve", blk64_b[0:64, 0:64], 1.0 / 64, [blk64_b.k])
    MEMSET("dve", blk64_b[64:128, 64:128], 1.0 / 64, [blk64_b.k])
    blk96_b = sb("blk96", [128, 128], BF16)
    MEMSET("dve", blk96_b[:], 0.0, [blk96_b.k])
    MEMSET("dve", blk96_b[0:64, 0:64], 1.0 / 64, [blk96_b.k])
    MEMSET("dve", blk96_b[64:96, 64:96], 1.0 / 32, [blk96_b.k])
    blk32_b = sb("blk32", [32, 32], BF16)
    MEMSET("dve", blk32_b[:], 1.0 / 32, [blk32_b.k])
    rkT_b = sb("rkT", [32, 32], BF16)
    P.dma("pool", rkT_b[:], cmat_d[64:96, CM["rmT"], 64:96], (), [rkT_b.k])
    sel65 = sb("sel65", [65, 64])
    MEMSET("dve", sel65[:], 0.0, [sel65.k])
    MEMSET("dve", sel65[64:65, :], 1.0, [sel65.k])
    cmod = sb("cmod", [128, 8, 2])
    P.dma("sp", cmod[:], cmod_d, (), [cmod.k])
    silu_c = sb("silu_c", [128, 8, 2])
    ACT(silu_c[:], cmod[:], AF.Silu, [cmod.k], [silu_c.k])
    modv = sb("modv", [128, 48, 2])
    gs1 = sb("gs1", [128, 8, 2])
    gs2 = sb("gs2", [128, 8, 2])
    smalls = sb("smalls", [128, NSM])
    rows = sb("rows", [128, NRW])
    neglam = sb("neglam", [128, 1])
    gsub = sb("gsub", [128, 1])
    lamt = sb("lamt", [128, 4])
    SB_PERSIST = st["off"]

    def smc(name, i=0, n=1, p0=0, p1=128):
        c0, _ = SM[name]
        return smalls[p0:p1, c0 + i:c0 + i + n]

    def blocks_for(include_ctx=True):
        b = [(0, CTX)] if include_ctx else []
        return b + [(CTX + 512 * i, 512) for i in range(S // 512)]

    PS = Ring(pbanks)

    def phase0(l):
        st["off"] = SB_PERSIST
        P.dma("sp", smalls[:], smalls_d[l], (), [smalls.k])
        P.dma("sp", rows[:], rows_d[l:l + 1, :].partition_broadcast(128), (), [rows.k])
        wa = sbring("wa", 2, [128, 8, 768])
        pb = PS.next()
        for j in range(8):
            w = wa.next()
            P.dma("sp", w[:], ada_w[l, :, j * 768:(j + 1) * 768].rearrange("(c p) n -> p c n", p=128), (), [w.k])
            for cc in range(6):
                q = j * 6 + cc
                for k in range(8):
                    MM(pb[:, 2 * q:2 * q + 2], w[:, k, cc * 128:(cc + 1) * 128], silu_c[:, k, :], k == 0, k == 7,
                       [w.k, silu_c.k], [pb.k])
        c0, _ = SM["adab"]
        TT("dve", modv[:], pb[:, 0:96].rearrange("p (q w) -> p q w", w=2),
           smalls[:, c0:c0 + 48].unsqueeze(2).to_broadcast([128, 48, 2]), ALU.add, [pb.k, smalls.k], [modv.k])
        for gs, nm, m in ((gs1, "n1g", 1), (gs2, "n2g", 4)):
            c0, _ = SM[nm]
            TS("dve", gs[:], modv[:, m * 8:(m + 1) * 8, :], 1.0, None, ALU.add, None, [modv.k], [gs.k])
            TT("dve", gs[:], gs[:], smalls[:, c0:c0 + 8].unsqueeze(2).to_broadcast([128, 8, 2]), ALU.mult,
               [gs.k, smalls.k], [gs.k])
        lam_init = 0.8 - 0.6 * math.exp(-0.3 * l)
        c0, _ = RW["lam"]
        prod = sb("lamprod", [128, 128])
        TT("dve", prod[:].rearrange("p (a d) -> p a d", a=2), rows[:, c0:c0 + 256].rearrange("p (a b d) -> p a b d", a=2, b=2)[:, :, 0, :],
           rows[:, c0:c0 + 256].rearrange("p (a b d) -> p a b d", a=2, b=2)[:, :, 1, :], ALU.mult, [rows.k], [prod.k])
        P.op("dve", lambda e: e.tensor_reduce(out=lamt[:, 0:2], in_=prod[:].rearrange("p (a d) -> p a d", a=2), axis=AX.X, op=ALU.add),
             [prod.k], [lamt.k])
        ACT(lamt[:, 2:4], lamt[:, 0:2], AF.Exp, [lamt.k], [lamt.k])
        TT("dve", neglam[:], lamt[:, 3:4], lamt[:, 2:3], ALU.subtract, [lamt.k], [neglam.k])
        TS("dve", neglam[:], neglam[:], -lam_init, None, ALU.add, None, [neglam.k], [neglam.k])
        c0, _ = SM["subg"]
        TS("dve", gsub[:], smalls[:, c0:c0 + 1], 1.0 - lam_init, None, ALU.mult, None, [smalls.k], [gsub.k])
        P.barrier()

    def rms_rstd(ss_ps_ap, n, scale, npart, ring_std, r, w_extra=()):
        sd = ring_std.next()
        if st.get("act_rstd"):
            ACT(sd[0:npart, 0:n], ss_ps_ap, AF.Ln, r, [sd.k], scale=scale, bias=EPS)
            ACT(sd[0:npart, 0:n], sd[0:npart, 0:n], AF.Exp, [sd.k], [sd.k], scale=-0.5)
            return sd
        ACT(sd[0:npart, 0:n], ss_ps_ap, AF.Sqrt, r, [sd.k], scale=scale, bias=EPS)
        RECIP(sd[0:npart, 0:n], sd[0:npart, 0:n], [sd.k], [sd.k])
        return sd

    def phaseA(l, Xin):
        st["off"] = SB_PERSIST
        NCA = 3760
        win = sb("win", [128, 8, NCA], BF16)
        for c in range(8):
            P.dma("pool", win[:, c, :], w_in[l, c * 128:(c + 1) * 128, 0:NCA], (), [(win.k, c)])
        wuq = sb("wuq", [128, 3, 768], BF16)
        P.dma("pool", wuq[:], w_uq[l].rearrange("(c p) n -> p c n", p=128), (), [wuq.k])
        wukv = sb("wukv", [128, 2, 1024], BF16)
        P.dma("pool", wukv[:], w_ukv[l].rearrange("(c p) n -> p c n", p=128), (), [wukv.k])
        xb_r = sbring("xb", 1, [128, 8, 512])
        sq_r = sbring("sq", 1, [128, 8, 512], BF16)
        hT_r = sbring("hT", 2, [128, 8, 512], BF16)
        std_r = sbring("std", 4, [128, 512])
        rope_r = sbring("rope", 2, [128, 6, 512])
        f32_r = sbring("tf", 4, [128, 512])
        xq_r = sbring("xq", 4, [128, 512])
        b16_r = sbring("tb", 10, [128, 512], BF16)
        cqf = sb("cqf", [128, 3, 512])
        cqn = sb("cqn", [128, 3, 512], BF16)
        ckvf = sb("ckvf", [128, 2, 512])
        ckvn = sb("ckvn", [128, 2, 512], BF16)
        sqcq = sb("sqcq", [128, 3, 512], BF16)
        sqckv = sb("sqckv", [128, 2, 512], BF16)
        PSA = Ring(pbanks, transient=True)
        blocks = blocks_for(True)
        B = {}

        def prep(bi):
            t0, n = blocks[bi]
            wh = 1 if t0 == 0 else 0
            xb, rp, hT, sq = xb_r.next(), rope_r.next(), hT_r.next(), sq_r.next()
            B[bi] = (hT, rp)
            P.dma("sp", xb[:, :, 0:n], Xin[:, t0:t0 + n].rearrange("(c p) n -> p c n", p=128), (), [xb.k])
            P.dma("sp", rp[:, :, 0:n], rope_d[:, :, t0:t0 + n].rearrange("a p n -> p a n"), (), [rp.k])
            ACT(sq[:, :, 0:n], xb[:, :, 0:n], AF.Square, [xb.k], [sq.k])
            pb = PSA.next()
            for c in range(8):
                MM(pb[:, 0:n], ones_b[:], sq[:, c, 0:n], c == 0, c == 7, [ones_b.k, sq.k], [pb.k])
            rs = rms_rstd(pb[:, 0:n], n, 1.0 / D, 128, std_r, [pb.k])
            yield
            TT("dve", xb[:, :, 0:n], xb[:, :, 0:n], rs[:, 0:n].unsqueeze(1).to_broadcast([128, 8, n]), ALU.mult,
               [xb.k, rs.k], [xb.k])
            for c in range(8):
                ACT(hT[:, c, 0:n], xb[:, c, 0:n], AF.Identity, [xb.k, gs1.k, modv.k], [(hT.k, c)],
                    scale=gs1[:, c, wh:wh + 1], bias=modv[:, 0 * 8 + c, wh:wh + 1])
            P.dma("sp", Hs[:, t0:t0 + n].rearrange("(c p) n -> p c n", p=128), hT[:, :, 0:n], [(hT.k, c) for c in range(8)], [("H", t0)])

        def hk(hT):
            return [(hT.k, c) for c in range(8)]

        def proj_fm(bi, col0, M, wt=None, nk=8, src=None, srck=None):
            t0, n = blocks[bi]
            hT, rp = B[bi]
            wt = win if wt is None else wt
            if src is None:
                src, srck = hT, hk(hT)
            pbx = PSA.next()
            for k in range(nk):
                MM(pbx[0:M, 0:n], wt[:, k, col0:col0 + M], src[:, k, 0:n], k == 0, k == nk - 1,
                   [(win.k, k) if wt is win else wt.k] + srck, [pbx.k])
            return pbx

        def norm_rope(bi, pbx, M, blk_ap, blk_k, gain_ap, rT_ap, rT_k, ci, si, dst, dkey):
            t0, n = blocks[bi]
            hT, rp = B[bi]
            s2 = b16_r.next()
            ACT(s2[0:M, 0:n], pbx[0:M, 0:n], AF.Square, [pbx.k], [s2.k])
            xq = xq_r.next()
            CP("act", xq[0:M, 0:n], pbx[0:M, 0:n], [pbx.k], [xq.k])
            yield
            pss = PSA.next()
            MM(pss[0:M, 0:n], blk_ap, s2[0:M, 0:n], True, True, [blk_k, s2.k], [pss.k])
            r2 = rms_rstd(pss[0:M, 0:n], n, 1.0, M, std_r, [pss.k])
            qn = b16_r.next()
            STT("dve", qn[0:M, 0:n], xq[0:M, 0:n], gain_ap, r2[0:M, 0:n], ALU.mult, ALU.mult,
                [xq.k, r2.k, smalls.k], [qn.k])
            if rT_ap is None:
                P.dma("sp", dst, qn[0:M, 0:n], [qn.k], [dkey])
                return
            yield
            prot = PSA.next()
            MM(prot[0:M, 0:n], rT_ap, qn[0:M, 0:n], True, True, [rT_k, qn.k], [prot.k])
            t1 = f32_r.next()
            TT("pool", t1[0:M, 0:n], qn[0:M, 0:n], rp[0:M, ci, 0:n], ALU.mult, [qn.k, rp.k], [t1.k])
            t2 = f32_r.next()
            TT("dve", t2[0:M, 0:n], prot[0:M, 0:n], rp[0:M, si, 0:n], ALU.mult, [prot.k, rp.k], [t2.k])
            o = b16_r.next()
            TT("pool", o[0:M, 0:n], t1[0:M, 0:n], t2[0:M, 0:n], ALU.add, [t1.k, t2.k], [o.k])
            for d_ in (dst if isinstance(dst, list) else [dst]):
                P.dma("sp", d_, o[0:M, 0:n], [o.k], [dkey])

        def job_dqk(bi, h, isk):
            t0, n = blocks[bi]
            pbx = proj_fm(bi, (512 if isk else 0) + h * 128, 128)
            dst = (DK if isk else DQ)[h, :, t0:t0 + n]
            yield from norm_rope(bi, pbx, 128, blk64_b[:], blk64_b.k, smc("dkg" if isk else "dqg"), rdT_b, cmatb.k, 0, 1,
                                 dst, ("DQK", isk, h, t0))

        def job_tm(bi, tt, col0, N, dst, dt_):
            t0, n = blocks[bi]
            hT, rp = B[bi]
            pbx = PSA.next()
            for k in range(8):
                MM(pbx[:, 0:N], hT[:, k, tt * 128:(tt + 1) * 128], win[:, k, col0:col0 + N], k == 0, k == 7,
                   [(hT.k, k), (win.k, k)], [pbx.k])
            o = (b16_r if dt_ == BF16 else f32_r).next()
            CP("act", o[:, 0:N], pbx[:, 0:N], [pbx.k], [o.k])
            P.dma("sp", dst[t0 + tt * 128:t0 + (tt + 1) * 128, :], o[:, 0:N], [o.k], [("tm", col0, t0, tt)])
            return
            yield

        def job_xbc(bi, c):
            t0, n = blocks[bi]
            pbx = proj_fm(bi, 2048 + c * 128, 128)
            o = b16_r.next()
            CP("act", o[:, 0:n], pbx[:, 0:n], [pbx.k], [o.k])
            P.dma("sp", XBC[c * 128:(c + 1) * 128, t0:t0 + n], o[:, 0:n], [o.k], [("XBC", c, t0)])
            return
            yield

        def job_lat(bi, col0, nch, xf, xnb, s3, gname, ndim):
            t0, n = blocks[bi]
            for c in range(nch):
                pbx = proj_fm(bi, col0 + c * 128, 128)
                CP("act", xf[:, c, 0:n], pbx[:, 0:n], [pbx.k], [(xf.k, c)])
                ACT(s3[:, c, 0:n], pbx[:, 0:n], AF.Square, [pbx.k], [(s3.k, c)])
            yield
            pss = PSA.next()
            for c in range(nch):
                MM(pss[:, 0:n], ones_b[:], s3[:, c, 0:n], c == 0, c == nch - 1, [ones_b.k, (s3.k, c)], [pss.k])
            r3 = rms_rstd(pss[:, 0:n], n, 1.0 / ndim, 128, std_r, [pss.k])
            for c in range(nch):
                STT("dve", xnb[:, c, 0:n], xf[:, c, 0:n], smc(gname, c), r3[:, 0:n], ALU.mult, ALU.mult,
                    [(xf.k, c), r3.k, smalls.k], [(xnb.k, c)])

        def job_mq(bi, h):
            t0, n = blocks[bi]
            pbx = proj_fm(bi, h * 96, 96, wt=wuq, nk=3, src=cqn, srck=[(cqn.k, c) for c in range(3)])
            yield from norm_rope(bi, pbx, 96, blk96_b[0:96, 0:96], blk96_b.k, smc("mqg", p1=96), rmT_b[0:96, 0:96], cmatb.k,
                                 2, 3, MQ[h, :, t0:t0 + n], ("MQ", h, t0))

        def job_mk(bi, h):
            t0, n = blocks[bi]
            pbx = proj_fm(bi, h * 128, 64, wt=wukv, nk=2, src=ckvn, srck=[(ckvn.k, c) for c in range(2)])
            yield from norm_rope(bi, pbx, 64, blk64_b[0:64, 0:64], blk64_b.k, smc("mkg", p1=64), None, None, None, None,
                                 MK[h, 0:64, t0:t0 + n], ("MKn", h, t0))

        def job_mv(bi, tt):
            t0, n = blocks[bi]
            pbx = PSA.next()
            for k in range(2):
                MM(pbx[:, 0:512].rearrange("p (h d) -> p h d", h=8), ckvn[:, k, tt * 128:(tt + 1) * 128],
                   wukv[:, k, :].rearrange("p (h d) -> p h d", h=8)[:, :, 64:128], k == 0, k == 1, [(ckvn.k, k), wukv.k], [pbx.k])
            o = b16_r.next()
            CP("act", o[:, 0:512], pbx[:, 0:512], [pbx.k], [o.k])
            P.dma("sp", MV[t0 + tt * 128:t0 + (tt + 1) * 128, :], o[:, 0:512], [o.k], [("MV", t0, tt)])
            return
            yield

        def job_kr(bi):
            t0, n = blocks[bi]
            pbx = proj_fm(bi, 3728, 32)
            yield from norm_rope(bi, pbx, 32, blk32_b[:], blk32_b.k, smc("mkrg", p1=32), rkT_b[:], rkT_b.k, 4, 5,
                                 [MK[h, 64:96, t0:t0 + n] for h in range(8)], ("MKr", t0))

        st["act_rstd"] = True
        run_jobs([prep(0)])
        gens = []
        for bi, (t0, n) in enumerate(blocks):
            jl = [job_lat(bi, 3088, 3, cqf, cqn, sqcq, "cqg", 384), job_lat(bi, 3472, 2, ckvf, ckvn, sqckv, "ckvg", 256)]
            jl += [job_dqk(bi, h, False) for h in range(4)]
            jl += [job_dqk(bi, h, True) for h in range(4)]
            for tt in range(n // 128):
                jl += [job_tm(bi, tt, 1024, 512, DV, BF16), job_tm(bi, tt, 1536, 512, Zs, BF16), job_tm(bi, tt, 3072, 16, DTs, F32)]
            jl += [job_xbc(bi, c) for c in range(8)]
            if bi + 1 < len(blocks):
                jl.append(prep(bi + 1))
            jl += [job_mq(bi, h) for h in range(8)]
            jl += [job_mk(bi, h) for h in range(8)]
            jl += [job_mv(bi, tt) for tt in range(n // 128)]
            jl.append(job_kr(bi))
            gens += jl
        run_jobs(gens)
        st["act_rstd"] = False
        P.barrier()

    def phaseB(l, need_ctx):
        st["off"] = SB_PERSIST
        kt_r = sbring("kt", 2, [128, 2, T], BF16)
        for b in kt_r.bufs:
            MEMSET("pool", b[:], 0.0, [b.k])
        qt_r = sbring("qt", 2, [128, T], BF16)
        vd_r = sbring("vd", 2, [128, NT, 128], BF16)
        vm_r = sbring("vm", 2, [128, NT, 65], BF16)
        for b in vm_r.bufs:
            MEMSET("pool", b[:, :, 64:65], 1.0, [b.k])
        pT_r = sbring("pT", 8, [128, 512], BF16)
        f32_r = sbring("bf", 6, [128, 512])
        b16_r = sbring("bb", 3, [128, 512], BF16)
        wst_r = sbring("wst", 3, [128, 6144], BF16)
        for e in range(32):
            w = wst_r.next()
            P.dma("pool", w[:, 0:2048].rearrange("p (c n) -> p c n", c=8), moe_wg[l, e].rearrange("(c p) n -> p c n", p=128), (), [(w.k, 0)])
            P.dma("pool", w[:, 2048:4096].rearrange("p (c n) -> p c n", c=8), moe_wu[l, e].rearrange("(c p) n -> p c n", p=128), (), [(w.k, 1)])
            P.dma("pool", w[:, 4096:6144].rearrange("p (c n) -> p c n", c=2), moe_wd[l, e].rearrange("(c p) n -> p c n", p=128), (), [(w.k, 2)])
            P.dma("pool", WBs[e * 128:(e + 1) * 128, :], w[:], [(w.k, 0), (w.k, 1), (w.k, 2)], ["WB", (w.k, 0), (w.k, 1), (w.k, 2)])
        SC = Ring(pbanks[0:4])
        OA = Ring(pbanks[4:6])
        SA = Ring(pbanks[6:8])
        qblocks = [(CTX + 512 * i, 512, NT) for i in range(S // 512)]
        if need_ctx:
            qblocks = [(0, CTX, CTX // 128)] + qblocks

        PD = 4

        def run_head(kt, qt, vv, is_diff, h):
            groups = []
            for (q0, nq, nkt) in qblocks:
                for comp in range(2 if is_diff else 1):
                    groups.append((q0, nq, nkt, comp))
            items = []
            for gi, (q0, nq, nkt, comp) in enumerate(groups):
                for ki in range(nkt):
                    items.append((gi, ki))
            gstate = {}
            pTs = {}
            oc_hold = {}

            def issue_sc(idx):
                gi, ki = items[idx]
                q0, nq, nkt, comp = groups[gi]
                sc = SC.next()
                scale = 0.125 if is_diff else mscale
                MM(sc[:, 0:nq], kt[:, comp, ki * 128:(ki + 1) * 128], qt[:, q0:q0 + nq], True, True, [kt.k, qt.k], [sc.k])
                pT = pT_r.next()
                ACT(pT[:, 0:nq], sc[:, 0:nq], AF.Exp, [sc.k], [pT.k], scale=scale)
                pTs[idx] = pT

            def issue_av(idx):
                gi, ki = items[idx]
                q0, nq, nkt, comp = groups[gi]
                if ki == 0:
                    gstate[gi] = (OA.next(), SA.next() if is_diff else None)
                O, Sm = gstate[gi]
                pT = pTs.pop(idx)
                if is_diff:
                    MM(O[:, 0:nq], vv[:, ki, :], pT[:, 0:nq], ki == 0, ki == nkt - 1, [vv.k, pT.k], [O.k])
                    MM(Sm[:, 0:nq], ones_b[:], pT[:, 0:nq], ki == 0, ki == nkt - 1, [ones_b.k, pT.k], [Sm.k])
                else:
                    MM(O[0:65, 0:nq], vv[:, ki, :], pT[:, 0:nq], ki == 0, ki == nkt - 1, [vv.k, pT.k], [O.k])
                if ki != nkt - 1:
                    return
                del gstate[gi]
                if is_diff:
                    rc = f32_r.next()
                    RECIP(rc[:, 0:nq], Sm[:, 0:nq], [Sm.k], [rc.k])
                    o_ = oc_r.next()
                    TT("dve", o_[:, 0:nq], O[:, 0:nq], rc[:, 0:nq], ALU.mult, [O.k, rc.k], [o_.k])
                    if comp == 0:
                        oc_hold[q0] = o_
                        return
                    o0 = oc_hold.pop(q0)
                    o = f32_r.next()
                    STT("dve", o[:, 0:nq], o_[:, 0:nq], neglam[:, 0:1], o0[:, 0:nq], ALU.mult, ALU.add,
                        [o0.k, o_.k, neglam.k], [o.k])
                    s2 = b16_r.next()
                    ACT(s2[:, 0:nq], o[:, 0:nq], AF.Square, [o.k], [s2.k])
                    pss = SC.next()
                    MM(pss[:, 0:nq], mean128_b[:], s2[:, 0:nq], True, True, [mean128_b.k, s2.k], [pss.k])
                    r2 = f32_r.next()
                    ACT(r2[:, 0:nq], pss[:, 0:nq], AF.Sqrt, [pss.k], [r2.k], scale=1.0, bias=EPS)
                    RECIP(r2[:, 0:nq], r2[:, 0:nq], [r2.k], [r2.k])
                    ob = b16_r.next()
                    STT("dve", ob[:, 0:nq], o[:, 0:nq], gsub[:, 0:1], r2[:, 0:nq], ALU.mult, ALU.mult, [o.k, gsub.k, r2.k], [ob.k])
                    P.dma("sp", DOs[h * 128:(h + 1) * 128, q0:q0 + nq], ob[:, 0:nq], [ob.k], [("DO", h, q0)])
                else:
                    oa = f32_r.next()
                    CP("act", oa[0:65, 0:nq], O[0:65, 0:nq], [O.k], [oa.k])
                    pbc = SC.next()
                    MM(pbc[0:64, 0:nq], sel65[:], oa[0:65, 0:nq], True, True, [sel65.k, oa.k], [pbc.k])
                    rc = f32_r.next()
                    RECIP(rc[0:64, 0:nq], pbc[0:64, 0:nq], [pbc.k], [rc.k])
                    ob = b16_r.next()
                    TT("dve", ob[0:64, 0:nq], oa[0:64, 0:nq], rc[0:64, 0:nq], ALU.mult, [oa.k, rc.k], [ob.k])
                    P.dma("sp", MOs[h * 64:(h + 1) * 64, q0:q0 + nq], ob[0:64, 0:nq], [ob.k], [("MO", h, q0)])

            N = len(items)
            for idx in range(N + PD):
                if idx < N:
                    issue_sc(idx)
                if idx >= PD:
                    issue_av(idx - PD)

        mscale = 96.0 ** -0.5
        oc_r = sbring("oc", 3, [128, 512])
        for h in range(4):
            kt, qt, vv = kt_r.next(), qt_r.next(), vd_r.next()
            P.dma("sp", kt[0:64, 0, :], DK[h, 0:64, :], (), [kt.k])
            P.dma("sp", kt[64:128, 1, :], DK[h, 64:128, :], (), [kt.k])
            P.dma("sp", qt[:], DQ[h], (), [qt.k])
            P.dma("sp", vv[:], DV[:, h * 128:(h + 1) * 128].rearrange("(t p) d -> p t d", p=128), (), [vv.k])
            run_head(kt, qt, vv, True, h)
        for h in range(8):
            kt, qt, vv = kt_r.next(), qt_r.next(), vm_r.next()
            P.dma("sp", kt[0:96, 0, :], MK[h], (), [kt.k])
            P.dma("sp", qt[0:96, :], MQ[h], (), [qt.k])
            P.dma("sp", vv[:, :, 0:64], MV[:, h * 64:(h + 1) * 64].rearrange("(t p) d -> p t d", p=128), (), [vv.k])
            run_head(kt, qt, vv, False, h)
        P.barrier()


    pbanks_b = [Buf(pb.t.bitcast(BF16), pb.k) for pb in pbanks]

    def phaseC(l, need_ctx):
        st["off"] = SB_PERSIST
        U = sb("U", [128, 8, T], BF16)
        base_after_U = st["off"]
        W = T + 8
        WO = W - 4
        uin_r = sbring("uin", 2, [128, W], BF16)
        for b in uin_r.bufs:
            MEMSET("pool", b[:, 0:2], 0.0, [b.k])
            MEMSET("pool", b[:, 258:262], 0.0, [b.k])
            MEMSET("pool", b[:, 262 + S:264 + S], 0.0, [b.k])
        cw0, _ = SM["convw"]
        cb0, _ = SM["convb"]
        dg = sb("dg", [128, 40, 128], BF16)
        for i in range(40):
            TS("dve" if i % 2 == 0 else "pool", dg[:, i, :], ident_f, smalls[:, cw0 + i:cw0 + i + 1], None, ALU.mult, None,
               [cmat.k, smalls.k], [(dg.k, i)])
        PSC = Ring(pbanks, transient=True)
        cblocks = [(0, CTX, 0)] + [(260 + 512 * i, 512, CTX + 512 * i) for i in range(S // 512)]
        for c in range(8):
            uin = uin_r.next()
            P.dma("sp", uin[:, 2:258], XBC[c * 128:(c + 1) * 128, 0:CTX], (), [uin.k])
            P.dma("sp", uin[:, 262:262 + S], XBC[c * 128:(c + 1) * 128, CTX:T], (), [uin.k])
            for (j0, w, u0) in cblocks:
                pc = PSC.next()
                for k in range(5):
                    MM(pc[:, 0:w], dg[:, c * 5 + k, :], uin[:, j0 + k:j0 + k + w], k == 0, k == 4, [(dg.k, c * 5 + k), uin.k], [pc.k])
                ACT(U[:, c, u0:u0 + w], pc[:, 0:w], AF.Silu, [pc.k, smalls.k], [U.k], bias=smalls[:, cb0 + c:cb0 + c + 1])
        P.barrier()
        st["off"] = base_after_U
        dtr = sb("dtr", [128, NT, 16])
        dtt = sb("dtt", [128, NT, 16])
        dtu = sb("dtu", [128, NT, 16])
        dtA = sb("dtA", [128, NT, 16])
        Arow = sb("Arow", [128, 16])
        Drow = sb("Drow", [128, 8])
        P.dma("sp", dtr[:], DTs.rearrange("(t p) d -> p t d", p=128), (), [dtr.k])
        c0, _ = RW["dtb"]
        TT("dve", dtr[:], dtr[:], rows[:, c0:c0 + 16].unsqueeze(1).to_broadcast([128, NT, 16]), ALU.add, [dtr.k, rows.k], [dtr.k])
        ACT(dtt[:], dtr[:], AF.Abs, [dtr.k], [dtt.k])
        ACT(dtt[:], dtt[:], AF.Exp, [dtt.k], [dtt.k], scale=-1.0)
        ACT(dtt[:], dtt[:], AF.Ln, [dtt.k], [dtt.k], bias=1.0)
        TS("dve", dtu[:], dtr[:], 0.0, None, ALU.max, None, [dtr.k], [dtu.k])
        TT("dve", dtu[:], dtu[:], dtt[:], ALU.add, [dtu.k, dtt.k], [dtu.k])
        c0, _ = RW["alog"]
        ACT(Arow[:], rows[:, c0:c0 + 16], AF.Exp, [rows.k], [Arow.k])
        TS("dve", Arow[:], Arow[:], -1.0, None, ALU.mult, None, [Arow.k], [Arow.k])
        TT("dve", dtA[:], dtu[:], Arow[:].unsqueeze(1).to_broadcast([128, NT, 16]), ALU.mult, [dtu.k, Arow.k], [dtA.k])
        c0, _ = RW["ssdD"]
        CP("dve", Drow[:], rows[:, c0:c0 + 8], [rows.k], [Drow.k])
        g0, _ = RW["ssdg"]

        hs = [sb(f"hs{d}", [128, 512]) for d in range(2)]
        hsb = [sb(f"hsb{d}", [128, 512], BF16) for d in range(2)]
        for d in range(2):
            MEMSET("dve", hs[d][:], 0.0, [hs[d].k])
            MEMSET("pool", hsb[d][:], 0.0, [hsb[d].k])
        xs_r = sbring("xs", 4, [128, 512], BF16)
        bt_r = sbring("bt", 4, [128, 256], BF16)
        cbT_r = sbring("cbT", 4, [128, 2, 128], BF16)
        ex_r = sbring("ex", 4, [128, 24])
        xdt_r = sbring("xdt", 4, [128, 512], BF16)
        xw_r = sbring("xw", 4, [128, 512], BF16)
        E_r = sbring("E", 4, [128, 128], BF16)
        E_r.transient = True
        mix_r = sbring("mix", 4, [128, 8, 128], BF16)
        y_r = sbring("y", 4, [128, 512])
        yf_r = sbring("yf", 4, [128, 512])
        z_r = sbring("z", 4, [128, 512], BF16)
        zg_r = sbring("zg", 4, [128, 512])
        ob_r = sbring("ob", 4, [128, 512], BF16)
        oT_r = sbring("oT", 4, [128, 4, 128], BF16)
        sm_r = sbring("sm", 6, [128, 2])
        tmp_r = sbring("tmp", 4, [128, 512])
        SEGR = Ring(pbanks[0:3], transient=True)
        MISC = Ring([3, 4])
        YD, YO, STP = pbanks[5], pbanks[6], pbanks[7]
        cmf = lambda nm: cmat[:, CM[nm], :]

        def chunk(ck, d, finalize):
            cs = slice(ck * 128, (ck + 1) * 128)
            hd = d * 8
            tri, ntri, strict, negm = (("triU", "negTriU", "strictL", "negmF") if d == 0 else ("triL", "negTriL", "strictU", "negmB"))
            mi = MISC.next()
            pt = pbanks_b[mi]
            for c in range(4):
                TR(pt[:, c * 128:(c + 1) * 128], U[:, c, cs], ident_b, [U.k, cmatb.k], [pt.k])
            xs = xs_r.next()
            CP("act", xs[:], pt[:, 0:512], [pt.k], [xs.k])
            mi = MISC.next()
            pt2 = pbanks_b[mi]
            for g in range(2):
                TR(pt2[:, g * 128:(g + 1) * 128], U[:, 4 + g, cs], ident_b, [U.k, cmatb.k], [pt2.k])
            bt = bt_r.next()
            CP("act", bt[:], pt2[:, 0:256], [pt2.k], [bt.k])
            mi = MISC.next()
            pc = pbanks[mi]
            for g in range(2):
                MM(pc[:, g * 128:(g + 1) * 128], U[:, 4 + g, cs], U[:, 6 + g, cs], True, True, [U.k], [pc.k])
            cbT = cbT_r.next()
            CP("dve", cbT[:], pc[:, 0:256].rearrange("p (g i) -> p g i", g=2), [pc.k], [cbT.k])
            mi = MISC.next()
            pa = pbanks[mi]
            dcol = dtA[:, ck, hd:hd + 8]
            MM(pa[:, 0:8], cmf(tri), dcol, True, True, [cmat.k, dtA.k], [pa.k])
            MM(pa[:, 8:16], cmf(strict), dcol, True, True, [cmat.k, dtA.k], [pa.k])
            MM(pa[:, 16:24], ones_f[:], dcol, True, True, [ones_f.k, dtA.k], [pa.k])
            ex = ex_r.next()
            ACT(ex[:], pa[:, 0:24], AF.Exp, [pa.k], [ex.k])
            xdt = xdt_r.next()
            TT("dve", xdt[:].rearrange("p (h q) -> p h q", h=8), xs[:].rearrange("p (h q) -> p h q", h=8),
               dtu[:, ck, hd:hd + 8].unsqueeze(2).to_broadcast([128, 8, 64]), ALU.mult, [xs.k, dtu.k], [xdt.k])
            xw = xw_r.next()
            TT("pool", xw[:].rearrange("p (h q) -> p h q", h=8), xdt[:].rearrange("p (h q) -> p h q", h=8),
               ex[:, 8:16].unsqueeze(2).to_broadcast([128, 8, 64]), ALU.mult, [xdt.k, ex.k], [xw.k])
            mixj = mix_r.next()
            for h in range(8):
                g = h // 4
                sg = SEGR.next()
                dbc = dtA[:, ck, hd + h:hd + h + 1].to_broadcast([128, 128])
                MM(sg[:, 0:128], dbc, cmf(tri), True, False, [dtA.k, cmat.k], [sg.k])
                MM(sg[:, 0:128], cmf(ntri), dbc, False, False, [dtA.k, cmat.k], [sg.k])
                MM(sg[:, 0:128], ident_f, cmf(negm), False, True, [cmat.k], [sg.k])
                E = E_r.next()
                ACT(E[:], sg[:, 0:128], AF.Exp, [sg.k], [E.k])
                TT("dve" if h % 2 == 0 else "pool", mixj[:, h, :], E[:], cbT[:, g, :], ALU.mult, [E.k, cbT.k], [(mixj.k, h)])
            yield
            for h in range(8):
                MM(YD[:, h * 64:(h + 1) * 64], mixj[:, h, :], xdt[:, h * 64:(h + 1) * 64], True, True, [(mixj.k, h), xdt.k], [YD.k])
            for g in range(2):
                MM(YO[:, g * 256:(g + 1) * 256], U[:, 6 + g, cs], hsb[d][:, g * 256:(g + 1) * 256], True, True, [U.k, hsb[d].k], [YO.k])
            y = y_r.next()
            TT("dve", y[:].rearrange("p (h q) -> p h q", h=8), YO[:, 0:512].rearrange("p (h q) -> p h q", h=8),
               ex[:, 0:8].unsqueeze(2).to_broadcast([128, 8, 64]), ALU.mult, [YO.k, ex.k], [y.k])
            TT("dve", y[:], y[:], YD[:, 0:512], ALU.add, [y.k, YD.k], [y.k])
            for g in range(2):
                MM(STP[:, g * 256:(g + 1) * 256], bt[:, g * 128:(g + 1) * 128], xw[:, g * 256:(g + 1) * 256], True, True, [bt.k, xw.k], [STP.k])
            TT("dve", hs[d][:].rearrange("p (h q) -> p h q", h=8), hs[d][:].rearrange("p (h q) -> p h q", h=8),
               ex[:, 16:24].unsqueeze(2).to_broadcast([128, 8, 64]), ALU.mult, [hs[d].k, ex.k], [hs[d].k])
            TT("dve", hs[d][:], hs[d][:], STP[:, 0:512], ALU.add, [hs[d].k, STP.k], [hs[d].k])
            CP("pool", hsb[d][:], hs[d][:], [hs[d].k], [hsb[d].k])
            rows_ck = slice(ck * 128, (ck + 1) * 128)
            if d == 0:
                tmp = tmp_r.next()
                TT("pool", tmp[:].rearrange("p (h q) -> p h q", h=8), xs[:].rearrange("p (h q) -> p h q", h=8),
                   Drow[:].unsqueeze(2).to_broadcast([128, 8, 64]), ALU.mult, [xs.k, Drow.k], [tmp.k])
                TT("pool", y[:], y[:], tmp[:], ALU.add, [y.k, tmp.k], [y.k])
                P.dma("sp", YF[rows_ck, :], y[:], [y.k], [("YF", ck)])
                return
            if not finalize:
                return
            yf = yf_r.next()
            P.dma("sp", yf[:], YF[rows_ck, :], [("YF", ck)], [yf.k])
            z = z_r.next()
            P.dma("sp", z[:], Zs[rows_ck, :], (), [z.k])
            zg = zg_r.next()
            ACT(zg[:], z[:], AF.Silu, [z.k], [zg.k])
            TT("pool", y[:], y[:], yf[:], ALU.add, [y.k, yf.k], [y.k])
            TT("dve", y[:], y[:], zg[:], ALU.mult, [y.k, zg.k], [y.k])
            sm = sm_r.next()
            MEMSET("dve", sm[:], 0.0, [sm.k])
            ACT(zg[:], y[:], AF.Square, [y.k, zg.k], [zg.k, sm.k], accum_out=sm[:, 0:1])
            ACT(sm[:, 1:2], sm[:, 0:1], AF.Sqrt, [sm.k], [sm.k], scale=1.0 / 512, bias=EPS)
            RECIP(sm[:, 1:2], sm[:, 1:2], [sm.k], [sm.k])
            ob = ob_r.next()
            STT("dve", ob[:], y[:], sm[:, 1:2], rows[:, g0:g0 + 512], ALU.mult, ALU.mult, [y.k, sm.k, rows.k], [ob.k])
            yield
            mi = MISC.next()
            po = pbanks_b[mi]
            for c in range(4):
                TR(po[:, c * 128:(c + 1) * 128], ob[:, c * 128:(c + 1) * 128], ident_b, [ob.k, cmatb.k], [po.k])
            oT = oT_r.next()
            CP("act", oT[:], po[:, 0:512].rearrange("p (c i) -> p c i", c=4), [po.k], [oT.k])
            P.dma("sp", SOs[:, rows_ck].rearrange("(c p) i -> p c i", p=128), oT[:], [oT.k], [("SO", ck)])

        nctx = CTX // 128
        order = list(range(nctx - 1, -1, -1)) + list(range(NT - 1, nctx - 1, -1))
        run_jobs([chunk(ck, 0, False) for ck in range(NT)])
        run_jobs([chunk(ck, 1, need_ctx or ck >= nctx) for ck in order])
        P.barrier()

    def phaseD(l, Xin, need_ctx):
        st["off"] = SB_PERSIST
        st["hi"] = SB_META
        wg = sb("wg", [128, 8, 3072], BF16)
        for c in range(8):
            P.dma("pool", wg[:, c, :], w_in[l, c * 128:(c + 1) * 128, 3760:6832], (), [(wg.k, c)])
        wb = sb("wb", [128, 3, 4, 1024], BF16)
        for k in range(3):
            P.dma("pool", wb[:, k], w_branch[l, k].rearrange("(c p) n -> p c n", p=128), (), [(wb.k, k)])
        wo = sb("wo", [128, 8, 1024], BF16)
        P.dma("pool", wo[:], w_out[l].rearrange("(c p) n -> p c n", p=128), (), [wo.k])
        wr = sb("wr", [128, 8, 36])
        P.dma("sp", wr[:], router_w[l].rearrange("(c p) n -> p c n", p=128), (), [wr.k])
        hT_r = sbring("dhT", 1, [128, 8, 512], BF16)
        o3_r = sbring("o3", 1, [128, 3, 4, 512], BF16)
        xb_r = sbring("dxb", 1, [128, 8, 512])
        acc = sb("dacc", [128, 8, 512])
        accb = sb("daccb", [128, 8, 512], BF16)
        h2f = sb("dh2f", [128, 8, 512])
        h2b = sb("dh2b", [128, 8, 512], BF16)
        g_r = sbring("dg", 3, [128, 512], BF16)
        gm_r = sbring("dgm", 2, [128, 512])
        std_r = sbring("dstd", 1, [128, 512])
        lg_r = sbring("lg", 2, [128, 36])
        sc_r = sbring("rsc", 2, [128, 16])
        m_r = sbring("rm", 2, [128, 4, 32])
        htm_r = sbring("htm", 2, [128, D], BF16)
        htm_r.transient = True
        rt_r = sbring("rt", 2, [128, 2, 32])
        rt_r.transient = True
        MK1, MK2, W12, RK, Macc = meta_bufs(f"d{l}")
        MEMSET("dve", Macc[:], 0.0, [Macc.k])
        for r_ in (g_r, gm_r, std_r, lg_r, sc_r, m_r):
            r_.transient = True
        PSD = Ring(pbanks, transient=True)
        rb0, _ = RW["rb"]
        BIG = 30000.0
        blocks = blocks_for(need_ctx)
        BD = {}
        ack = [(acc.k, c) for c in range(8)]
        abk = [(accb.k, c) for c in range(8)]
        hfk = [(h2f.k, c) for c in range(8)]

        def load_ho(bi):
            t0, n = blocks[bi]
            hT, o3 = hT_r.next(), o3_r.next()
            BD[bi] = [hT, o3, None]
            P.dma("sp", hT[:, :, 0:n], Hs[:, t0:t0 + n].rearrange("(c p) n -> p c n", p=128), (), [hT.k])
            for k, src in enumerate((DOs, SOs, MOs)):
                P.dma("sp", o3[:, k, :, 0:n], src[:, t0:t0 + n].rearrange("(c p) n -> p c n", p=128), (), [(o3.k, k)])
            return
            yield

        def load_x(bi):
            t0, n = blocks[bi]
            xb = xb_r.next()
            BD[bi][2] = xb
            P.dma("sp", xb[:, :, 0:n], Xin[:, t0:t0 + n].rearrange("(c p) n -> p c n", p=128), (), [xb.k])
            return
            yield

        def gjob(bi, c, k):
            t0, n = blocks[bi]
            hT, o3, _ = BD[bi]
            pg = PSD.next()
            for kc in range(8):
                MM(pg[:, 0:n], wg[:, kc, k * 1024 + c * 128:k * 1024 + (c + 1) * 128], hT[:, kc, 0:n], kc == 0, kc == 7,
                   [(wg.k, kc), hT.k], [pg.k])
            g = g_r.next()
            ACT(g[:, 0:n], pg[:, 0:n], AF.Sigmoid, [pg.k], [g.k])
            pm = PSD.next()
            for kc in range(4):
                MM(pm[:, 0:n], wb[:, k, kc, c * 128:(c + 1) * 128], o3[:, k, kc, 0:n], kc == 0, kc == 3,
                   [(wb.k, k), (o3.k, k)], [pm.k])
            ak = (acc.k, c)
            if k == 0:
                TT("dve", acc[:, c, 0:n], pm[:, 0:n], g[:, 0:n], ALU.mult, [pm.k, g.k], [ak])
            else:
                gm = gm_r.next()
                TT("dve", gm[:, 0:n], pm[:, 0:n], g[:, 0:n], ALU.mult, [pm.k, g.k], [gm.k])
                if k == 1:
                    TT("pool", acc[:, c, 0:n], acc[:, c, 0:n], gm[:, 0:n], ALU.add, [ak, gm.k], [ak])
                else:
                    TT("pool", accb[:, c, 0:n], acc[:, c, 0:n], gm[:, 0:n], ALU.add, [ak, gm.k], [(accb.k, c)])
            return
            yield

        def tail(bi):
            t0, n = blocks[bi]
            wh = 1 if t0 == 0 else 0
            xb = BD[bi][2]
            for c in range(8):
                py = PSD.next()
                for kc in range(8):
                    MM(py[:, 0:n], wo[:, kc, c * 128:(c + 1) * 128], accb[:, kc, 0:n], kc == 0, kc == 7,
                       [wo.k, (accb.k, kc)], [py.k])
                STT("dve", xb[:, c, 0:n], py[:, 0:n], modv[:, 2 * 8 + c, wh:wh + 1], xb[:, c, 0:n], ALU.mult, ALU.add,
                    [py.k, modv.k, xb.k], [xb.k])
            P.dma("sp", XM[:, t0:t0 + n].rearrange("(c p) n -> p c n", p=128), xb[:, :, 0:n], [xb.k], [("XM", t0)])
            ACT(accb[:, :, 0:n], xb[:, :, 0:n], AF.Square, [xb.k], abk)
            pb = PSD.next()
            for c in range(8):
                MM(pb[:, 0:n], ones_b[:], accb[:, c, 0:n], c == 0, c == 7, [ones_b.k] + abk, [pb.k])
            rs = rms_rstd(pb[:, 0:n], n, 1.0 / D, 128, std_r, [pb.k])
            TT("dve", h2f[:, :, 0:n], xb[:, :, 0:n], rs[:, 0:n].unsqueeze(1).to_broadcast([128, 8, n]), ALU.mult,
               [xb.k, rs.k], hfk)
            for c in range(8):
                ACT(h2f[:, c, 0:n], h2f[:, c, 0:n], AF.Identity, [(h2f.k, c), gs2.k, modv.k], [(h2f.k, c)],
                    scale=gs2[:, c, wh:wh + 1], bias=modv[:, 3 * 8 + c, wh:wh + 1])
            CP("pool", h2b[:, :, 0:n], h2f[:, :, 0:n], hfk, [(h2b.k, c) for c in range(8)])
            for _ in range(5):
                yield
            for tt in range(n // 128):
                pti = PSD.next()
                pt = Buf(pti.t.bitcast(BF16), pti.k)
                for c in range(8):
                    TR(pt[:, c * 128:(c + 1) * 128], h2b[:, c, tt * 128:(tt + 1) * 128], ident_b, [(h2b.k, c), cmatb.k], [pt.k])
                htm = htm_r.next()
                CP("act", htm[:], pt[:, 0:D], [pt.k], [htm.k])
                P.dma("sp", H2TM[t0 + tt * 128:t0 + (tt + 1) * 128, :], htm[:], [htm.k], [("H2TM", t0, tt)])
            for tt in range(n // 128):
                pr = PSD.next()
                for kc in range(8):
                    MM(pr[:, 0:36], h2f[:, kc, tt * 128:(tt + 1) * 128], wr[:, kc, :], kc == 0, kc == 7, [(h2f.k, kc), wr.k], [pr.k])
                lg, sc, m = lg_r.next(), sc_r.next(), m_r.next()
                TT("dve", lg[:], pr[:, 0:36], rows[:, rb0:rb0 + 36], ALU.add, [pr.k, rows.k], [lg.k])
                MEMSET("dve", sc[:], 0.0, [sc.k])
                RED = lambda o, i, r, w: P.op("dve", lambda e: e.tensor_reduce(out=o, in_=i, axis=AX.X, op=ALU.max), r, w)
                RED(sc[:, 0:1], lg[:, 0:4], [lg.k], [sc.k])
                TS("dve", m[:, 0, 0:4], lg[:, 0:4], sc[:, 0:1], None, ALU.is_equal, None, [lg.k, sc.k], [m.k])
                TS("dve", sc[:, 1:2], sc[:, 0:1], -1.0, None, ALU.mult, None, [sc.k], [sc.k])
                ACT(m[:, 0, 8:12], lg[:, 0:4], AF.Exp, [lg.k, sc.k, m.k], [m.k, sc.k], bias=sc[:, 1:2], accum_out=sc[:, 2:3])
                RECIP(sc[:, 3:4], sc[:, 2:3], [sc.k], [sc.k])
                TS("dve", m[:, 0, 4:8], m[:, 0, 0:4], -1.0, BIG, ALU.add, ALU.mult, [m.k], [m.k])
                TT("dve", m[:, 1, :].rearrange("p (g e) -> p g e", g=4), lg[:, 4:36].rearrange("p (g e) -> p g e", g=4),
                   m[:, 0, 4:8].unsqueeze(2).to_broadcast([128, 4, 8]), ALU.add, [lg.k, m.k], [m.k])
                RED(sc[:, 4:5], m[:, 1, :], [m.k], [sc.k])
                TS("dve", m[:, 2, :], m[:, 1, :], sc[:, 4:5], None, ALU.is_equal, None, [m.k, sc.k], [m.k])
                STT("dve", m[:, 1, :], m[:, 2, :], -BIG, m[:, 1, :], ALU.mult, ALU.add, [m.k], [m.k])
                RED(sc[:, 5:6], m[:, 1, :], [m.k], [sc.k])
                TS("dve", m[:, 3, :], m[:, 1, :], sc[:, 5:6], None, ALU.is_equal, None, [m.k, sc.k], [m.k])
                TT("dve", sc[:, 6:7], sc[:, 4:5], sc[:, 5:6], ALU.subtract, [sc.k], [sc.k])
                ACT(sc[:, 7:8], sc[:, 6:7], AF.Sigmoid, [sc.k], [sc.k])
                TT("dve", sc[:, 8:9], sc[:, 7:8], sc[:, 3:4], ALU.mult, [sc.k], [sc.k])
                TT("dve", sc[:, 9:10], sc[:, 3:4], sc[:, 8:9], ALU.subtract, [sc.k], [sc.k])
                gt = (t0 + tt * 128) // 128
                CP("dve", MK1[:, gt, :], m[:, 2, :], [m.k], [(MK1.k, gt)])
                CP("pool", MK2[:, gt, :], m[:, 3, :], [m.k], [(MK2.k, gt)])
                CP("dve", W12[:, gt, :], sc[:, 8:10], [sc.k], [(W12.k, gt)])
                TT("dve", m[:, 0, :], m[:, 2, :], m[:, 3, :], ALU.add, [m.k], [m.k])
                pk = PSD.next()
                MM(pk[:, 0:32], cmat[:, CM["strictU"], :], m[:, 0, :], True, False, [cmat.k, m.k], [pk.k])
                MM(pk[:, 0:32], ones_f[:], Macc[:], False, True, [ones_f.k, Macc.k], [pk.k])
                rt = rt_r.next()
                TT("dve", rt[:, 0, :], pk[:, 0:32], m[:, 2, :], ALU.mult, [pk.k, m.k], [rt.k])
                TT("dve", rt[:, 1, :], pk[:, 0:32], m[:, 3, :], ALU.mult, [pk.k, m.k], [rt.k])
                P.op("dve", lambda e, o=RK[:, gt, :], i=rt[:]: e.tensor_reduce(out=o, in_=i, axis=AX.X, op=ALU.add), [rt.k], [(RK.k, gt)])
                TT("dve", Macc[:], Macc[:], m[:, 0, :], ALU.add, [Macc.k, m.k], [Macc.k])

        gens = []
        for bi in range(len(blocks)):
            gens.append(load_ho(bi))
            gj = [gjob(bi, c, k) for c in range(8) for k in range(3)]
            gens += gj[:4] + [load_x(bi)] + gj[4:]
            gens.append(tail(bi))
        run_jobs(gens)
        st["hi"] = SB_HI
        P.barrier()

    def phaseE(l, need_ctx, last):
        st["off"] = SB_PERSIST
        st["hi"] = SB_META
        MK1, MK2, W12, RK, Macc = meta_bufs(f"e{l}")
        gt0 = 0 if need_ctx else CTX // 128
        ntl = NT - gt0
        NB = 2 * ntl + 32
        PSE = Ring(pbanks, transient=True)
        mc = sb("mc", [128, NMC])
        P.dma("sp", mc[:], mconst_d, (), [mc.k])
        RED = lambda o, i, r, w_, op=ALU.add: P.op("dve", lambda e_: e_.tensor_reduce(out=o, in_=i, axis=AX.X, op=op), r, w_)
        mk1k = [(MK1.k, g) for g in range(gt0, NT)]
        mk2k = [(MK2.k, g) for g in range(gt0, NT)]
        rkk = [(RK.k, g) for g in range(gt0, NT)]
        w12k = [(W12.k, g) for g in range(gt0, NT)]
        pc = PSE.next()
        MM(pc[:, 0:32], ones_f[:], Macc[:], True, True, [ones_f.k, Macc.k], [pc.k])
        cnt = sb("cnt", [128, 32])
        CP("dve", cnt[:], pc[:, 0:32], [pc.k], [cnt.k])
        cmp3 = sb("cmp3", [128, 32, NT])
        TT("dve", cmp3[:], cnt[:].unsqueeze(2).to_broadcast([128, 32, NT]), mc[:, 0:NT].unsqueeze(1).to_broadcast([128, 32, NT]),
           ALU.is_gt, [cnt.k, mc.k], [cmp3.k])
        nblk = sb("nblk", [128, 32])
        RED(nblk[:], cmp3[:], [cmp3.k], [nblk.k])
        ptr = PSE.next()
        TR(ptr[0:32, 0:128], nblk[:, 0:32], ident_f, [nblk.k, cmat.k], [ptr.k])
        nbT = sb("nbT", [32, 128])
        CP("act", nbT[:], ptr[0:32, 0:128], [ptr.k], [nbT.k])
        pp = PSE.next()
        MM(pp[:, 0:32], nbT[0:32, :], cmat[0:32, CM["triU"], 0:32], True, True, [nbT.k, cmat.k], [pp.k])
        pend = sb("pend", [128, 32])
        CP("dve", pend[:], pp[:, 0:32], [pp.k], [pend.k])
        pstart = sb("pstart", [128, 32])
        TT("dve", pstart[:], pend[:], nblk[:], ALU.subtract, [pend.k, nblk.k], [pstart.k])
        tmp3 = sb("tmp3", [128, NT, 32])
        psk = sb("psk", [128, NT])
        slotf = sb("slotf", [128, NT, 2])
        SL = sb("SL", [128, NT, 2], I32)
        for k, (MK, mkk) in enumerate(((MK1, mk1k), (MK2, mk2k))):
            TT("dve", tmp3[:, gt0:NT, :], MK[:, gt0:NT, :], pstart[:].unsqueeze(1).to_broadcast([128, ntl, 32]), ALU.mult,
               mkk + [pstart.k, tmp3.k], [tmp3.k])
            RED(psk[:, gt0:NT], tmp3[:, gt0:NT, :], [tmp3.k], [psk.k])
            STT("dve", slotf[:, gt0:NT, k], psk[:, gt0:NT], 128.0, RK[:, gt0:NT, k], ALU.mult, ALU.add, [psk.k] + rkk, [(slotf.k, k)])
        CP("dve", SL[:, gt0:NT, :], slotf[:, gt0:NT, :], [(slotf.k, 0), (slotf.k, 1)], [SL.k])
        cmpb = sb("cmpb", [128, NB, 32])
        TT("dve", cmpb[:], pend[:].unsqueeze(1).to_broadcast([128, NB, 32]), mc[:, 34:34 + NB].unsqueeze(2).to_broadcast([128, NB, 32]),
           ALU.is_le, [pend.k, mc.k], [cmpb.k])
        ebf = sb("ebf", [128, NB])
        RED(ebf[:], cmpb[:], [cmpb.k], [ebf.k])
        TS("dve", ebf[:], ebf[:], 31.0, None, ALU.min, None, [ebf.k], [ebf.k])
        TS("dve", ebf[:], ebf[:], 128.0, mc[:, 138:139], ALU.mult, ALU.add, [ebf.k, mc.k], [ebf.k])
        WIDX = sb("WIDX", [128, NB], I32)
        CP("dve", WIDX[:], ebf[:], [ebf.k], [WIDX.k])
        if "META" in dbg:
            d1 = nc.dram_tensor(f"dbg_SL{l}", [128, NT, 2], I32, kind="ExternalOutput").ap()
            d2 = nc.dram_tensor(f"dbg_WIDX{l}", [128, NB], I32, kind="ExternalOutput").ap()
            d3 = nc.dram_tensor(f"dbg_cnt{l}", [128, 32], F32, kind="ExternalOutput").ap()
            d4 = nc.dram_tensor(f"dbg_pend{l}", [128, 32], F32, kind="ExternalOutput").ap()
            P.dma("sp", d1[:, gt0:NT, :], SL[:, gt0:NT, :], [SL.k], ["d1"])
            P.dma("sp", d2, WIDX[:], [WIDX.k], ["d2"])
            P.dma("sp", d3, cnt[:], [cnt.k], ["d3"])
            P.dma("sp", d4, pend[:], [pend.k], ["d4"])
        if stop_after == ("E1", l):
            st["hi"] = SB_HI
            P.barrier()
            return
        idx_r = sbring("idx", 12, [128, 1], I32)
        idx_r.transient = True

        def IDX(col_ap, rk):
            it = idx_r.next()
            CP("pool", it[:, 0:1], col_ap, rk, [it.k])
            return it

        hrow_r = sbring("hrow", 3, [128, D], BF16)
        for gt in range(gt0, NT):
            hr = hrow_r.next()
            P.dma("sp", hr[:], H2TM[gt * 128:(gt + 1) * 128, :], (), [hr.k])
            for k in range(2):
                it = IDX(SL[:, gt, k:k + 1], [SL.k])
                P.idma(XBs, bass.IndirectOffsetOnAxis(it[:, 0:1].bitcast(U32), 0), hr[:], None, [hr.k, it.k], [hr.k, ("XB", gt, k)])
        P.barrier()
        if stop_after == ("E2", l):
            st["hi"] = SB_HI
            return
        base_blocks = st["off"]
        wt_r = sbring("wt", WT_RING, [128, 6144], BF16)
        wt_r.transient = True
        xtm_r = sbring("xtm", 3, [128, D], BF16)
        xT_r = sbring("xT", 2, [128, 8, 128], BF16)
        sg_r = sbring("ssg", 2, [128, 256], BF16)
        hid_r = sbring("shid", 2, [128, 256], BF16)
        yo_r = sbring("yo", 2, [128, D])
        for r_ in (xT_r, sg_r, hid_r):
            r_.transient = True

        def blockjob(b):
            wt, xtm = wt_r.next(), xtm_r.next()
            it = IDX(WIDX[:, b:b + 1], [WIDX.k])
            P.idma(wt[:], None, WBs, bass.IndirectOffsetOnAxis(it[:, 0:1].bitcast(U32), 0), [it.k], [wt.k])
            if XLOAD:
                P.dma("sp", xtm[:], XBs[b * 128:(b + 1) * 128, :], (), [xtm.k])
            yield
            yield
            if BLK_CUT == 0:
                return
            pti = PSE.next()
            pt = Buf(pti.t.bitcast(BF16), pti.k)
            for c in range(8):
                TR(pt[:, c * 128:(c + 1) * 128], xtm[:, c * 128:(c + 1) * 128], ident_b, [xtm.k, cmatb.k], [pt.k])
            xT = xT_r.next()
            CP("act", xT[:], pt[:, 0:D].rearrange("p (c n) -> p c n", c=8), [pt.k], [xT.k])
            if BLK_CUT == 1:
                return
            pg, pu = PSE.next(), PSE.next()
            for (pb_, off) in ((pg, 0), (pu, 2048)):
                for hc in range(2):
                    for kc in range(8):
                        MM(pb_[:, hc * 128:(hc + 1) * 128], wt[:, off + kc * 256 + hc * 128:off + kc * 256 + (hc + 1) * 128], xT[:, kc, :],
                           kc == 0, kc == 7, [wt.k, xT.k], [pb_.k])
            sg = sg_r.next()
            ACT(sg[:], pg[:, 0:256], AF.Silu, [pg.k], [sg.k])
            hid = hid_r.next()
            TT("dve", hid[:], pu[:, 0:256], sg[:], ALU.mult, [pu.k, sg.k], [hid.k])
            if BLK_CUT == 2:
                return
            yo = yo_r.next()
            for half in range(2):
                py = PSE.next()
                for hc in range(2):
                    MM(py[:, 0:512], hid[:, hc * 128:(hc + 1) * 128], wt[:, 4096 + hc * 1024 + half * 512:4096 + hc * 1024 + (half + 1) * 512],
                       hc == 0, hc == 1, [hid.k, wt.k], [py.k])
                if half == 0:
                    CP("act", yo[:, 0:512], py[:, 0:512], [py.k], [(yo.k, 0)])
                else:
                    CP("dve", yo[:, 512:1024], py[:, 0:512], [py.k], [(yo.k, 1)])
            P.dma("sp", YBs[b * 128:(b + 1) * 128, :], yo[:], [(yo.k, 0), (yo.k, 1)], [("YB", b), (yo.k, 0), (yo.k, 1)])

        run_jobs([blockjob(b) for b in range(NBLK_START, NB if NBLK_DBG is None else NBLK_DBG)])
        P.barrier()
        if stop_after == ("E3", l):
            st["hi"] = SB_HI
            return
        st["off"] = base_blocks
        ya_r = sbring("ya", 4, [128, D])
        yb_r = sbring("yb", 4, [128, D])
        xm_r = sbring("xm", 2, [128, 8, 512])
        blocks = blocks_for(need_ctx)

        def cjob(bi):
            t0, n = blocks[bi]
            wh = 1 if t0 == 0 else 0
            xm = xm_r.next()
            P.dma("sp", xm[:, :, 0:n], XM[:, t0:t0 + n].rearrange("(c p) n -> p c n", p=128), (), [xm.k])
            gath = []
            for tt in range(n // 128):
                gt = (t0 + tt * 128) // 128
                ya, yb = ya_r.next(), yb_r.next()
                ita = IDX(SL[:, gt, 0:1], [SL.k])
                P.idma(ya[:], None, YBs, bass.IndirectOffsetOnAxis(ita[:, 0:1].bitcast(U32), 0), [ita.k], [ya.k])
                itb = IDX(SL[:, gt, 1:2], [SL.k])
                P.idma(yb[:], None, YBs, bass.IndirectOffsetOnAxis(itb[:, 0:1].bitcast(U32), 0), [itb.k], [yb.k])
                gath.append((gt, ya, yb))
            for tt, (gt, ya, yb) in enumerate(gath):
                TS("dve", ya[:], ya[:], W12[:, gt, 0:1], None, ALU.mult, None, [ya.k] + w12k, [ya.k])
                STT("dve", ya[:], yb[:], W12[:, gt, 1:2], ya[:], ALU.mult, ALU.add, [ya.k, yb.k] + w12k, [ya.k])
                for q in range(2):
                    ptq = PSE.next()
                    for c4 in range(4):
                        c = q * 4 + c4
                        TR(ptq[:, c4 * 128:(c4 + 1) * 128], ya[:, c * 128:(c + 1) * 128], ident_f, [ya.k, cmat.k], [ptq.k])
                    for c4 in range(4):
                        c = q * 4 + c4
                        STT("dve", xm[:, c, tt * 128:(tt + 1) * 128], ptq[:, c4 * 128:(c4 + 1) * 128], modv[:, 5 * 8 + c, wh:wh + 1],
                            xm[:, c, tt * 128:(tt + 1) * 128], ALU.mult, ALU.add, [ptq.k, modv.k, xm.k], [xm.k])
            if last:
                P.dma("sp", yT[:, t0 - CTX:t0 - CTX + n].rearrange("(c p) n -> p c n", p=128), xm[:, :, 0:n], [xm.k], [("yT", t0), xm.k])
            else:
                P.dma("sp", XR[:, t0:t0 + n].rearrange("(c p) n -> p c n", p=128), xm[:, :, 0:n], [xm.k], [("XR", t0), xm.k])

        for bi in range(len(blocks)):
            cjob(bi)
        st["hi"] = SB_HI
        P.barrier()

    def finish():
        P.barrier()

    layers = []
    for l in range(depth):
        last = l == depth - 1
        Xin = xT if l == 0 else XR
        phase0(l)
        phaseA(l, Xin)
        if stop_after == ("A", l):
            break
        phaseB(l, not last)
        if stop_after == ("B", l):
            break
        phaseC(l, not last)
        if stop_after == ("C", l):
            break
        phaseD(l, Xin, not last)
        if stop_after == ("D", l):
            break
        phaseE(l, not last, last)
        if stop_after in (("E", l), ("E1", l), ("E2", l), ("E3", l)):
            break
    finish()
    with ExitStack() as stack:
        P.emit(stack)
    return nc, scr


def make_in_maps(inp, S, depth):
    B = inp["x"].shape[0]
    cmat, rope, sel, mconst = host_consts(S)
    smalls = np.stack([pack_smalls(inp, l) for l in range(depth)])
    rows = np.stack([pack_rows(inp, l) for l in range(depth)])
    router_w = np.ascontiguousarray(np.concatenate([inp["moe_group_w"], inp["moe_expert_w"]], axis=-1))
    shared = {
        "ada_w": inp["ada_w"], "w_in": inp["w_in"], "w_uq": inp["w_uq"], "w_ukv": inp["w_ukv"],
        "w_branch": inp["w_branch"], "w_out": inp["w_out"], "router_w": router_w,
        "moe_w_gate": inp["moe_w_gate"], "moe_w_up": inp["moe_w_up"], "moe_w_down": inp["moe_w_down"],
        "smalls": smalls, "rows": rows, "cmat": cmat, "rope": rope, "sel": sel, "mconst": mconst,
    }
    shared = {k: np.ascontiguousarray(v, dtype=np.float32) for k, v in shared.items()}
    maps = []
    for b in range(B):
        xt = np.ascontiguousarray(np.concatenate([inp["ctx"][b], inp["x"][b]], axis=0).T)
        cm = np.stack([inp["c"][b].reshape(8, 128).T, inp["c_ctx"].reshape(8, 128).T], axis=-1)
        m = dict(shared)
        m["xT"] = xt.astype(np.float32)
        m["cmod"] = np.ascontiguousarray(cm, dtype=np.float32)
        maps.append(m)
    return maps


def kernel(**inputs):
    inp = {k: np.asarray(v) for k, v in inputs.items()}
    B, S, _ = inp["x"].shape
    depth = inp["w_in"].shape[0]
    nc, _ = build_program(S, depth)
    maps = make_in_maps(inp, S, depth)
    res = run_bass_kernel_spmd(nc, maps, core_ids=list(range(B)))
    out = np.stack([np.ascontiguousarray(res.results[b]["yT"].T) for b in range(B)])
    return out.astype(np.float32)
```

```python
import math
from contextlib import ExitStack
import numpy as np
import concourse.bass as bass
import concourse.mybir as mybir
from concourse.bass_utils import run_bass_kernel_spmd

F32 = mybir.dt.float32
BF16 = mybir.dt.bfloat16
I32 = mybir.dt.int32
U32 = mybir.dt.uint32
AF = mybir.ActivationFunctionType
ALU = mybir.AluOpType
AX = mybir.AxisListType

EPOCH = 20000
NDMASEM = 8
D = 1024
CTX = 256
EPS = 1e-6
NEG = -30000.0
BLK_CUT = 9
WT_RING = 3
NBLK_DBG = None
NBLK_START = 0
XLOAD = True


class Prog:
    CE = ("pe", "dve", "act", "pool")
    QE = ("sp", "pool", "act")

    def __init__(self, nc):
        self.nc = nc
        self.ops = {e: [] for e in ("pe", "dve", "act", "pool", "sp")}
        self.cnt = {e: 0 for e in self.CE}
        self.dcnt = {q: 0 for q in self.QE}
        self.seen = {e: {} for e in self.ops}
        self.last_w = {}
        self.readers = {}
        self.latest = {}
        self.nsem = set()
        self.sems = {}

    def _tok_compute(self, eng):
        k = self.cnt[eng]
        self.cnt[eng] += 1
        return (("c", eng, k // EPOCH), (k % EPOCH) + 1)

    def _tok_dma(self, q):
        k = self.dcnt[q]
        self.dcnt[q] += 1
        return (("d", q, k % NDMASEM), 16 * (k // NDMASEM + 1))

    def _filter(self, eng, need):
        out = []
        seen = self.seen[eng]
        for sk, v in need.items():
            if sk[0] == "c":
                hi = max([s[2] for s in seen if s[0] == "c" and s[1] == sk[1]] + [-1])
                if hi > sk[2]:
                    continue
            if seen.get(sk, 0) >= v:
                continue
            seen[sk] = v
            out.append((sk, v))
        return out

    def _deps(self, eng, reads, writes):
        need = {}

        def add(tok):
            if tok is None:
                return
            sk, v = tok
            if sk[0] == "c" and sk[1] == eng and eng == "pe":
                return
            if need.get(sk, 0) < v:
                need[sk] = v

        for r in reads:
            add(self.last_w.get(r))
        for w in writes:
            add(self.last_w.get(w))
            for sk, v in self.readers.get(w, {}).items():
                add((sk, v))
        return self._filter(eng, need)

    def _commit(self, tok, reads, writes):
        sk, v = tok
        self.latest[sk] = max(self.latest.get(sk, 0), v)
        self.nsem.add(sk)
        for r in reads:
            d = self.readers.setdefault(r, {})
            if d.get(sk, 0) < v:
                d[sk] = v
        for w in writes:
            self.last_w[w] = tok
            self.readers[w] = {}

    def op(self, eng, fn, reads=(), writes=()):
        waits = self._deps(eng, reads, writes)
        tok = self._tok_compute(eng)
        self.ops[eng].append((waits, fn, tok))
        self._commit(tok, reads, writes)

    def dma(self, q, out, in_, reads=(), writes=()):
        waits = self._deps(q, reads, writes)
        tok = self._tok_dma(q)
        self.ops[q].append((waits, (lambda e, o=out, i=in_: e.dma_start(out=o, in_=i)), tok))
        self._commit(tok, reads, writes)

    def idma(self, out, out_off, in_, in_off, reads=(), writes=()):
        waits = self._deps("pool", reads, writes)
        tok = self._tok_dma("pool")
        self.ops["pool"].append((waits, (lambda e: e.indirect_dma_start(out=out, out_offset=out_off, in_=in_, in_offset=in_off)), tok))
        self._commit(tok, reads, writes)

    def barrier(self):
        need = {}
        for sk, v in self.latest.items():
            if sk[0] == "c":
                hi = max(s[2] for s in self.latest if s[0] == "c" and s[1] == sk[1])
                if sk[2] < hi:
                    continue
            need[sk] = v
        for eng in self.ops:
            w = self._filter(eng, {sk: v for sk, v in need.items() if not (sk[0] == "c" and sk[1] == eng)})
            if w:
                self.ops[eng].append((w, None, None))
        self.last_w = {}
        self.readers = {}

    def emit(self, stack):
        nc = self.nc
        for sk in sorted(self.nsem, key=str):
            self.sems[sk] = stack.enter_context(nc.semaphore("s_" + "_".join(str(x) for x in sk)))
        block = stack.enter_context(nc.Block())
        sems = self.sems

        def run(eobj, lst):
            for waits, fn, tok in lst:
                for sk, v in waits:
                    eobj.wait_ge(sems[sk], v)
                if fn is not None:
                    fn(eobj).then_inc(sems[tok[0]], 16 if tok[0][0] == "d" else 1)

        ops = self.ops

        @block.tensor
        def _(e):
            run(e, ops["pe"])

        @block.vector
        def _(e):
            run(e, ops["dve"])

        @block.scalar
        def _(e):
            run(e, ops["act"])

        @block.gpsimd
        def _(e):
            run(e, ops["pool"])

        @block.sync
        def _(e):
            run(e, ops["sp"])


class Buf:
    def __init__(self, t, k):
        self.t = t
        self.k = k

    def __getitem__(self, idx):
        return self.t[idx]


class Job:
    def __init__(self, gen):
        self.gen = gen
        self.done = False


class Sched:
    cur = None


class Ring:
    def __init__(self, bufs, transient=False):
        self.bufs = bufs
        self.i = 0
        self.transient = transient

    def next(self):
        b = self.bufs[self.i % len(self.bufs)]
        self.i += 1
        if self.transient:
            return b
        own = getattr(b, "owner", None)
        if own is not None and not own.done and own is not Sched.cur:
            raise RuntimeError(f"ring too small: buffer {b.k} still owned by a live job")
        if not isinstance(b, int):
            b.owner = Sched.cur
        return b


def run_jobs(gens):
    active = []
    pending = list(gens)
    while pending or active:
        order = []
        if pending:
            order.append(Job(pending.pop(0)))
        order += list(reversed(active))
        for j in order:
            Sched.cur = j
            try:
                next(j.gen)
                if j not in active:
                    active.append(j)
            except StopIteration:
                j.done = True
                if j in active:
                    active.remove(j)
        Sched.cur = None


SM = {}
_c = 0
for _n, _w in [("n1g", 8), ("n2g", 8), ("adab", 48), ("dqg", 1), ("dkg", 1), ("subg", 1), ("convw", 40), ("convb", 8),
               ("cqg", 3), ("ckvg", 2), ("mqg", 1), ("mkg", 1), ("mkrg", 1)]:
    SM[_n] = (_c, _w)
    _c += _w
NSM = _c
RW = {}
_c = 0
for _n, _w in [("dtb", 16), ("alog", 16), ("ssdD", 8), ("ssdg", 512), ("rb", 36), ("lam", 256)]:
    RW[_n] = (_c, _w)
    _c += _w
NRW = _c
CM = {}
for _i, _n in enumerate(["ident", "rdT", "rmT", "triU", "negTriU", "strictL", "negmF", "triL", "negTriL", "strictU", "negmB"]):
    CM[_n] = _i
NCM = len(CM)
NMC = 140


def _rot_matrix(dim):
    q = dim // 4
    R = np.zeros((dim, dim), np.float32)
    for i in range(q):
        R[i, q + i] = -1.0
        R[q + i, i] = 1.0
        R[2 * q + i, 3 * q + i] = -1.0
        R[3 * q + i, 2 * q + i] = 1.0
    return R


def _rope_tables(S, dim):
    q = dim // 4
    inv = (10000.0 ** (-np.arange(q, dtype=np.float32) / q)).astype(np.float32)
    rows = S // 64
    r = np.repeat(np.arange(rows, dtype=np.float32), 64)
    cc = np.tile(np.arange(64, dtype=np.float32), rows)
    ar = r[:, None] * inv
    ac = cc[:, None] * inv
    ang = np.concatenate([ar, ar, ac, ac], axis=-1)
    return np.cos(ang).astype(np.float32).T, np.sin(ang).astype(np.float32).T


def host_consts(S):
    T = CTX + S
    cm = np.zeros((NCM, 128, 128), np.float32)
    cm[CM["ident"]] = np.eye(128, dtype=np.float32)
    R64 = _rot_matrix(64)
    rd = np.zeros((128, 128), np.float32)
    rd[:64, :64] = R64
    rd[64:, 64:] = R64
    cm[CM["rdT"]] = rd.T
    rm = np.zeros((128, 128), np.float32)
    rm[64:96, 64:96] = _rot_matrix(32)
    cm[CM["rmT"]] = rm.T
    t = np.arange(128)
    triU = (t[:, None] <= t[None, :]).astype(np.float32)
    triL = (t[:, None] >= t[None, :]).astype(np.float32)
    cm[CM["triU"]] = triU
    cm[CM["negTriU"]] = -triU
    cm[CM["strictL"]] = (t[:, None] > t[None, :]).astype(np.float32)
    cm[CM["negmF"]] = NEG * (t[:, None] > t[None, :])
    cm[CM["triL"]] = triL
    cm[CM["negTriL"]] = -triL
    cm[CM["strictU"]] = (t[:, None] < t[None, :]).astype(np.float32)
    cm[CM["negmB"]] = NEG * (t[:, None] < t[None, :])
    cmat = np.ascontiguousarray(cm.transpose(1, 0, 2))
    rope = np.zeros((6, 128, T), np.float32)
    cd, sd = _rope_tables(S, 64)
    rope[0, :, :CTX] = 1.0
    rope[0, :64, CTX:] = cd
    rope[0, 64:, CTX:] = cd
    rope[1, :64, CTX:] = sd
    rope[1, 64:, CTX:] = sd
    cmm, smm = _rope_tables(S, 32)
    rope[2, :, :] = 1.0
    rope[2, 64:96, CTX:] = cmm
    rope[3, 64:96, CTX:] = smm
    rope[4, :, :CTX] = 1.0
    rope[4, :32, CTX:] = cmm
    rope[5, :32, CTX:] = smm
    sel = np.zeros((64, 32, 128), np.float32)
    for e in range(32):
        sel[e, e, :] = 1.0
        sel[32 + e, e, :] = 1.0
    mc = np.zeros((128, NMC), np.float32)
    mc[:, 0:34] = 128.0 * np.arange(34, dtype=np.float32)[None, :]
    mc[:, 34:34 + 104] = np.arange(104, dtype=np.float32)[None, :]
    mc[:, 138] = np.arange(128, dtype=np.float32)
    return cmat, rope, sel, mc


def pack_smalls(inp, l):
    s = np.zeros((128, NSM), np.float32)

    def put(name, arr):
        c0, w = SM[name]
        s[: arr.shape[0], c0:c0 + w] = arr.reshape(arr.shape[0], w)

    put("n1g", inp["norm1_g"][l].reshape(8, 128).T)
    put("n2g", inp["norm2_g"][l].reshape(8, 128).T)
    put("adab", inp["ada_b"][l].reshape(48, 128).T)
    put("dqg", np.tile(inp["diff_q_g"][l], 2)[:, None])
    put("dkg", np.tile(inp["diff_k_g"][l], 2)[:, None])
    put("subg", inp["diff_subln_g"][l][:, None])
    put("convw", inp["ssd_conv_w"][l].T.reshape(8, 128, 5).transpose(1, 0, 2).reshape(128, 40))
    put("convb", inp["ssd_conv_b"][l].reshape(8, 128).T)
    put("cqg", inp["mla_cq_g"][l].reshape(3, 128).T)
    put("ckvg", inp["mla_ckv_g"][l].reshape(2, 128).T)
    put("mqg", inp["mla_q_g"][l][:, None])
    put("mkg", inp["mla_k_g"][l][:64][:, None])
    put("mkrg", inp["mla_k_g"][l][64:][:, None])
    return s


def pack_rows(inp, l):
    r = np.zeros((NRW,), np.float32)

    def put(name, arr):
        c0, w = RW[name]
        r[c0:c0 + w] = arr.reshape(-1)

    put("dtb", inp["ssd_dt_bias"][l])
    put("alog", inp["ssd_A_log"][l])
    put("ssdD", inp["ssd_D"][l])
    put("ssdg", inp["ssd_norm_g"][l])
    put("rb", np.concatenate([inp["moe_group_b"][l], inp["moe_expert_b"][l]]))
    put("lam", inp["diff_lambda"][l])
    return r


def build_program(S, depth, dbg=(), stop_after=None):
    T = CTX + S
    NT = T // 128
    nc = bass.Bass("TRN2", target_bir_lowering=False)
    P = Prog(nc)
    dram_in = {}

    def din(name, shape, dt=F32):
        dram_in[name] = nc.dram_tensor(name, list(shape), dt, kind="ExternalInput").ap()
        return dram_in[name]

    xT = din("xT", [D, T])
    cmod_d = din("cmod", [128, 8, 2])
    ada_w = din("ada_w", [depth, D, 6 * D])
    w_in = din("w_in", [depth, D, 6832])
    w_uq = din("w_uq", [depth, 384, 768])
    w_ukv = din("w_ukv", [depth, 256, 1024])
    w_branch = din("w_branch", [depth, 3, 512, D])
    w_out = din("w_out", [depth, D, D])
    router_w = din("router_w", [depth, D, 36])
    moe_wg = din("moe_w_gate", [depth, 32, D, 256])
    moe_wu = din("moe_w_up", [depth, 32, D, 256])
    moe_wd = din("moe_w_down", [depth, 32, 256, D])
    smalls_d = din("smalls", [depth, 128, NSM])
    rows_d = din("rows", [depth, NRW])
    cmat_d = din("cmat", [128, NCM, 128])
    rope_d = din("rope", [6, 128, T])
    sel_d = din("sel", [64, 32, 128])
    mconst_d = din("mconst", [128, NMC])
    yT = nc.dram_tensor("yT", [D, S], F32, kind="ExternalOutput").ap()

    scr = {}

    def dscr(name, shape, dt):
        kind = "ExternalOutput" if name in dbg else "Internal"
        scr[name] = nc.dram_tensor("scr_" + name, list(shape), dt, kind=kind).ap()
        return scr[name]

    NBMAX = 2 * NT + 32
    WBs = dscr("WB", [32 * 128, 6144], BF16)
    XBs = dscr("XB", [NBMAX * 128, D], BF16)
    YBs = dscr("YB", [NBMAX * 128, D], F32)
    XM = dscr("XM", [D, T], F32)
    XR = dscr("XR", [D, T], F32)
    Hs = dscr("H", [D, T], BF16)
    DQ = dscr("DQ", [4, 128, T], BF16)
    DK = dscr("DK", [4, 128, T], BF16)
    DV = dscr("DV", [T, 512], BF16)
    Zs = dscr("Z", [T, 512], BF16)
    XBC = dscr("XBC", [D, T], BF16)
    DTs = dscr("DT", [T, 16], F32)
    MQ = dscr("MQ", [8, 96, T], BF16)
    MK = dscr("MK", [8, 96, T], BF16)
    MV = dscr("MV", [T, 512], BF16)
    DOs = dscr("DO", [512, T], BF16)
    MOs = dscr("MO", [512, T], BF16)
    SOs = dscr("SO", [512, T], BF16)
    YF = dscr("YF", [T, 512], F32)
    XSs = dscr("XS", [T, 512], BF16)
    BTs = dscr("BT", [T, 256], BF16)
    H2s = dscr("H2", [D, T], BF16)
    CWT = dscr("CWT", [64, T], BF16)
    H2TM = dscr("H2TM", [T, D], BF16)

    SB_LO, SB_HI = 16640, 229376 - 64
    st = {"off": SB_LO, "n": 0}

    def sb(name, shape, dt=F32):
        nbytes = int(np.prod(shape[1:])) * (2 if dt == BF16 else 4)
        nbytes = (nbytes + 63) // 64 * 64
        off = st["off"]
        assert off + nbytes <= st.get("hi", SB_HI), f"SBUF overflow allocating {name}: {off}+{nbytes}"
        st["off"] = off + nbytes
        st["n"] += 1
        t = nc.alloc_sbuf_tensor_at(f"{name}_{st['n']}", list(shape), dt, offset=off)
        return Buf(t, f"{name}_{st['n']}")

    META_BYTES = 5632
    SB_META = SB_HI - META_BYTES

    def meta_bufs(tag):
        off = SB_META
        out = []
        for nm, shape, dt in (("MK1", [128, NT, 32], BF16), ("MK2", [128, NT, 32], BF16), ("W12", [128, NT, 2], F32),
                              ("RK", [128, NT, 2], F32), ("Macc", [128, 32], F32)):
            nb = (int(np.prod(shape[1:])) * (2 if dt == BF16 else 4) + 63) // 64 * 64
            st["n"] += 1
            out.append(Buf(nc.alloc_sbuf_tensor_at(f"{nm}_{tag}_{st['n']}", list(shape), dt, offset=off), f"{nm}"))
            off += nb
        assert off <= SB_HI
        return out

    def sbring(name, n, shape, dt=F32):
        return Ring([sb(f"{name}{i}", shape, dt) for i in range(n)])

    pbanks = [Buf(nc.alloc_psum_tensor(f"pb{i}", [128, 512], F32), f"pb{i}") for i in range(8)]

    def MM(out, lhsT, rhs, start, stop, r, w):
        P.op("pe", lambda e: e.matmul(out, lhsT=lhsT, rhs=rhs, start=start, stop=stop), r, w)

    def TR(out, in_, ident, r, w):
        P.op("pe", lambda e: e.transpose(out, in_, ident), r, w)

    def ACT(out, in_, func, r, w, **kw):
        P.op("act", lambda e: e.activation(out=out, in_=in_, func=func, **kw), r, w)

    def TT(eng, out, in0, in1, op, r, w):
        P.op(eng, lambda e: e.tensor_tensor(out=out, in0=in0, in1=in1, op=op), r, w)

    def TS(eng, out, in0, s1, s2, op0, op1, r, w):
        if s2 is None:
            P.op(eng, lambda e: e.tensor_scalar(out=out, in0=in0, scalar1=s1, scalar2=None, op0=op0), r, w)
        else:
            P.op(eng, lambda e: e.tensor_scalar(out=out, in0=in0, scalar1=s1, scalar2=s2, op0=op0, op1=op1), r, w)

    def STT(eng, out, in0, scalar, in1, op0, op1, r, w):
        P.op(eng, lambda e: e.scalar_tensor_tensor(out=out, in0=in0, scalar=scalar, in1=in1, op0=op0, op1=op1), r, w)

    def CP(eng, out, in_, r, w):
        if eng == "act":
            P.op("act", lambda e: e.copy(out=out, in_=in_), r, w)
        else:
            P.op(eng, lambda e: e.tensor_copy(out=out, in_=in_), r, w)

    def RECIP(out, in_, r, w):
        P.op("dve", lambda e: e.reciprocal(out=out, in_=in_), r, w)

    def MEMSET(eng, ap, val, w):
        P.op(eng, lambda e: e.memset(ap, val), (), w)

    cmat = sb("cmat", [128, NCM, 128])
    P.dma("sp", cmat[:], cmat_d, (), [cmat.k])
    cmatb = sb("cmatb", [128, 3, 128], BF16)
    P.dma("pool", cmatb[:], cmat_d[:, 0:3, :], (), [cmatb.k])
    ident_f = cmat[:, CM["ident"], :]
    ident_b = cmatb[:, 0, :]
    rdT_b = cmatb[:, 1, :]
    rmT_b = cmatb[:, 2, :]
    ones_b = sb("ones_b", [128, 128], BF16)
    MEMSET("dve", ones_b[:], 1.0, [ones_b.k])
    ones_f = sb("ones_f", [128, 128])
    MEMSET("dve", ones_f[:], 1.0, [ones_f.k])
    mean128_b = sb("mean128", [128, 128], BF16)
    MEMSET("dve", mean128_b[:], 1.0 / 128, [mean128_b.k])
    blk64_b = sb("blk64", [128, 128], BF16)
    MEMSET("dve", blk64_b[:], 0.0, [blk64_b.k])
    MEMSET("dve", blk64_b[0:64, 0:64], 1.0 / 64, [blk64_b.k])
    MEMSET("dve", blk64_b[64:128, 64:128], 1.0 / 64, [blk64_b.k])
    blk96_b = sb("blk96", [128, 128], BF16)
    MEMSET("dve", blk96_b[:], 0.0, [blk96_b.k])
    MEMSET("dve", blk96_b[0:64, 0:64], 1.0 / 64, [blk96_b.k])
    MEMSET("dve", blk96_b[64:96, 64:96], 1.0 / 32, [blk96_b.k])
    blk32_b = sb("blk32", [32, 32], BF16)
    MEMSET("dve", blk32_b[:], 1.0 / 32, [blk32_b.k])
    rkT_b = sb("rkT", [32, 32], BF16)
    P.dma("pool", rkT_b[:], cmat_d[64:96, CM["rmT"], 64:96], (), [rkT_b.k])
    sel65 = sb("sel65", [65, 64])
    MEMSET("dve", sel65[:], 0.0, [sel65.k])
    MEMSET("dve", sel65[64:65, :], 1.0, [sel65.k])
    cmod = sb("cmod", [128, 8, 2])
    P.dma("sp", cmod[:], cmod_d, (), [cmod.k])
    silu_c = sb("silu_c", [128, 8, 2])
    ACT(silu_c[:], cmod[:], AF.Silu, [cmod.k], [silu_c.k])
    modv = sb("modv", [128, 48, 2])
    gs1 = sb("gs1", [128, 8, 2])
    gs2 = sb("gs2", [128, 8, 2])
    smalls = sb("smalls", [128, NSM])
    rows = sb("rows", [128, NRW])
    neglam = sb("neglam", [128, 1])
    gsub = sb("gsub", [128, 1])
    lamt = sb("lamt", [128, 4])
    SB_PERSIST = st["off"]

    def smc(name, i=0, n=1, p0=0, p1=128):
        c0, _ = SM[name]
        return smalls[p0:p1, c0 + i:c0 + i + n]

    def blocks_for(include_ctx=True):
        b = [(0, CTX)] if include_ctx else []
        return b + [(CTX + 512 * i, 512) for i in range(S // 512)]

    PS = Ring(pbanks)

    def phase0(l):
        st["off"] = SB_PERSIST
        P.dma("sp", smalls[:], smalls_d[l], (), [smalls.k])
        P.dma("sp", rows[:], rows_d[l:l + 1, :].partition_broadcast(128), (), [rows.k])
        wa = sbring("wa", 2, [128, 8, 768])
        pb = PS.next()
        for j in range(8):
            w = wa.next()
            P.dma("sp", w[:], ada_w[l, :, j * 768:(j + 1) * 768].rearrange("(c p) n -> p c n", p=128), (), [w.k])
            for cc in range(6):
                q = j * 6 + cc
                for k in range(8):
                    MM(pb[:, 2 * q:2 * q + 2], w[:, k, cc * 128:(cc + 1) * 128], silu_c[:, k, :], k == 0, k == 7,
                       [w.k, silu_c.k], [pb.k])
        c0, _ = SM["adab"]
        TT("dve", modv[:], pb[:, 0:96].rearrange("p (q w) -> p q w", w=2),
           smalls[:, c0:c0 + 48].unsqueeze(2).to_broadcast([128, 48, 2]), ALU.add, [pb.k, smalls.k], [modv.k])
        for gs, nm, m in ((gs1, "n1g", 1), (gs2, "n2g", 4)):
            c0, _ = SM[nm]
            TS("dve", gs[:], modv[:, m * 8:(m + 1) * 8, :], 1.0, None, ALU.add, None, [modv.k], [gs.k])
            TT("dve", gs[:], gs[:], smalls[:, c0:c0 + 8].unsqueeze(2).to_broadcast([128, 8, 2]), ALU.mult,
               [gs.k, smalls.k], [gs.k])
        lam_init = 0.8 - 0.6 * math.exp(-0.3 * l)
        c0, _ = RW["lam"]
        prod = sb("lamprod", [128, 128])
        TT("dve", prod[:].rearrange("p (a d) -> p a d", a=2), rows[:, c0:c0 + 256].rearrange("p (a b d) -> p a b d", a=2, b=2)[:, :, 0, :],
           rows[:, c0:c0 + 256].rearrange("p (a b d) -> p a b d", a=2, b=2)[:, :, 1, :], ALU.mult, [rows.k], [prod.k])
        P.op("dve", lambda e: e.tensor_reduce(out=lamt[:, 0:2], in_=prod[:].rearrange("p (a d) -> p a d", a=2), axis=AX.X, op=ALU.add),
             [prod.k], [lamt.k])
        ACT(lamt[:, 2:4], lamt[:, 0:2], AF.Exp, [lamt.k], [lamt.k])
        TT("dve", neglam[:], lamt[:, 3:4], lamt[:, 2:3], ALU.subtract, [lamt.k], [neglam.k])
        TS("dve", neglam[:], neglam[:], -lam_init, None, ALU.add, None, [neglam.k], [neglam.k])
        c0, _ = SM["subg"]
        TS("dve", gsub[:], smalls[:, c0:c0 + 1], 1.0 - lam_init, None, ALU.mult, None, [smalls.k], [gsub.k])
        P.barrier()

    def rms_rstd(ss_ps_ap, n, scale, npart, ring_std, r, w_extra=()):
        sd = ring_std.next()
        if st.get("act_rstd"):
            ACT(sd[0:npart, 0:n], ss_ps_ap, AF.Ln, r, [sd.k], scale=scale, bias=EPS)
            ACT(sd[0:npart, 0:n], sd[0:npart, 0:n], AF.Exp, [sd.k], [sd.k], scale=-0.5)
            return sd
        ACT(sd[0:npart, 0:n], ss_ps_ap, AF.Sqrt, r, [sd.k], scale=scale, bias=EPS)
        RECIP(sd[0:npart, 0:n], sd[0:npart, 0:n], [sd.k], [sd.k])
        return sd

    def phaseA(l, Xin):
        st["off"] = SB_PERSIST
        NCA = 3760
        win = sb("win", [128, 8, NCA], BF16)
        for c in range(8):
            P.dma("pool", win[:, c, :], w_in[l, c * 128:(c + 1) * 128, 0:NCA], (), [(win.k, c)])
        wuq = sb("wuq", [128, 3, 768], BF16)
        P.dma("pool", wuq[:], w_uq[l].rearrange("(c p) n -> p c n", p=128), (), [wuq.k])
        wukv = sb("wukv", [128, 2, 1024], BF16)
        P.dma("pool", wukv[:], w_ukv[l].rearrange("(c p) n -> p c n", p=128), (), [wukv.k])
        xb_r = sbring("xb", 1, [128, 8, 512])
        sq_r = sbring("sq", 1, [128, 8, 512], BF16)
        hT_r = sbring("hT", 2, [128, 8, 512], BF16)
        std_r = sbring("std", 4, [128, 512])
        rope_r = sbring("rope", 2, [128, 6, 512])
        f32_r = sbring("tf", 4, [128, 512])
        xq_r = sbring("xq", 4, [128, 512])
        b16_r = sbring("tb", 13, [128, 512], BF16)
        cqf = sb("cqf", [128, 3, 512])
        cqn = sb("cqn", [128, 3, 512], BF16)
        ckvf = sb("ckvf", [128, 2, 512])
        ckvn = sb("ckvn", [128, 2, 512], BF16)
        sqcq = sb("sqcq", [128, 3, 512], BF16)
        sqckv = sb("sqckv", [128, 2, 512], BF16)
        PSA = Ring(pbanks, transient=True)
        blocks = blocks_for(True)
        B = {}

        def prep(bi):
            t0, n = blocks[bi]
            wh = 1 if t0 == 0 else 0
            xb, rp, hT, sq = xb_r.next(), rope_r.next(), hT_r.next(), sq_r.next()
            B[bi] = (hT, rp)
            P.dma("sp", xb[:, :, 0:n], Xin[:, t0:t0 + n].rearrange("(c p) n -> p c n", p=128), (), [xb.k])
            P.dma("sp", rp[:, :, 0:n], rope_d[:, :, t0:t0 + n].rearrange("a p n -> p a n"), (), [rp.k])
            ACT(sq[:, :, 0:n], xb[:, :, 0:n], AF.Square, [xb.k], [sq.k])
            pb = PSA.next()
            for c in range(8):
                MM(pb[:, 0:n], ones_b[:], sq[:, c, 0:n], c == 0, c == 7, [ones_b.k, sq.k], [pb.k])
            rs = rms_rstd(pb[:, 0:n], n, 1.0 / D, 128, std_r, [pb.k])
            yield
            TT("dve", xb[:, :, 0:n], xb[:, :, 0:n], rs[:, 0:n].unsqueeze(1).to_broadcast([128, 8, n]), ALU.mult,
               [xb.k, rs.k], [xb.k])
            for c in range(8):
                ACT(hT[:, c, 0:n], xb[:, c, 0:n], AF.Identity, [xb.k, gs1.k, modv.k], [(hT.k, c)],
                    scale=gs1[:, c, wh:wh + 1], bias=modv[:, 0 * 8 + c, wh:wh + 1])
            P.dma("sp", Hs[:, t0:t0 + n].rearrange("(c p) n -> p c n", p=128), hT[:, :, 0:n], [(hT.k, c) for c in range(8)], [("H", t0)])

        def hk(hT):
            return [(hT.k, c) for c in range(8)]

        def proj_fm(bi, col0, M, wt=None, nk=8, src=None, srck=None):
            t0, n = blocks[bi]
            hT, rp = B[bi]
            wt = win if wt is None else wt
            if src is None:
                src, srck = hT, hk(hT)
            pbx = PSA.next()
            for k in range(nk):
                MM(pbx[0:M, 0:n], wt[:, k, col0:col0 + M], src[:, k, 0:n], k == 0, k == nk - 1,
                   [(win.k, k) if wt is win else wt.k] + srck, [pbx.k])
            return pbx

        def norm_rope(bi, pbx, M, blk_ap, blk_k, gain_ap, rT_ap, rT_k, ci, si, dst, dkey):
            t0, n = blocks[bi]
            hT, rp = B[bi]
            s2 = b16_r.next()
            ACT(s2[0:M, 0:n], pbx[0:M, 0:n], AF.Square, [pbx.k], [s2.k])
            xq = xq_r.next()
            CP("act", xq[0:M, 0:n], pbx[0:M, 0:n], [pbx.k], [xq.k])
            yield
            pss = PSA.next()
            MM(pss[0:M, 0:n], blk_ap, s2[0:M, 0:n], True, True, [blk_k, s2.k], [pss.k])
            r2 = rms_rstd(pss[0:M, 0:n], n, 1.0, M, std_r, [pss.k])
            qn = b16_r.next()
            STT("dve", qn[0:M, 0:n], xq[0:M, 0:n], gain_ap, r2[0:M, 0:n], ALU.mult, ALU.mult,
                [xq.k, r2.k, smalls.k], [qn.k])
            if rT_ap is None:
                P.dma("sp", dst, qn[0:M, 0:n], [qn.k], [dkey])
                return
            yield
            yield
            prot = PSA.next()
            MM(prot[0:M, 0:n], rT_ap, qn[0:M, 0:n], True, True, [rT_k, qn.k], [prot.k])
            t1 = f32_r.next()
            TT("pool", t1[0:M, 0:n], qn[0:M, 0:n], rp[0:M, ci, 0:n], ALU.mult, [qn.k, rp.k], [t1.k])
            t2 = f32_r.next()
            TT("dve", t2[0:M, 0:n], prot[0:M, 0:n], rp[0:M, si, 0:n], ALU.mult, [prot.k, rp.k], [t2.k])
            o = b16_r.next()
            TT("pool", o[0:M, 0:n], t1[0:M, 0:n], t2[0:M, 0:n], ALU.add, [t1.k, t2.k], [o.k])
            for d_ in (dst if isinstance(dst, list) else [dst]):
                P.dma("sp", d_, o[0:M, 0:n], [o.k], [dkey])

        def job_dqk(bi, h, isk):
            t0, n = blocks[bi]
            pbx = proj_fm(bi, (512 if isk else 0) + h * 128, 128)
            dst = (DK if isk else DQ)[h, :, t0:t0 + n]
            yield from norm_rope(bi, pbx, 128, blk64_b[:], blk64_b.k, smc("dkg" if isk else "dqg"), rdT_b, cmatb.k, 0, 1,
                                 dst, ("DQK", isk, h, t0))

        def job_tm(bi, tt, col0, N, dst, dt_):
            t0, n = blocks[bi]
            hT, rp = B[bi]
            pbx = PSA.next()
            for k in range(8):
                MM(pbx[:, 0:N], hT[:, k, tt * 128:(tt + 1) * 128], win[:, k, col0:col0 + N], k == 0, k == 7,
                   [(hT.k, k), (win.k, k)], [pbx.k])
            o = (b16_r if dt_ == BF16 else f32_r).next()
            CP("act", o[:, 0:N], pbx[:, 0:N], [pbx.k], [o.k])
            P.dma("sp", dst[t0 + tt * 128:t0 + (tt + 1) * 128, :], o[:, 0:N], [o.k], [("tm", col0, t0, tt)])
            return
            yield

        def job_xbc(bi, c):
            t0, n = blocks[bi]
            pbx = proj_fm(bi, 2048 + c * 128, 128)
            o = b16_r.next()
            CP("act", o[:, 0:n], pbx[:, 0:n], [pbx.k], [o.k])
            P.dma("sp", XBC[c * 128:(c + 1) * 128, t0:t0 + n], o[:, 0:n], [o.k], [("XBC", c, t0)])
            return
            yield

        def job_lat(bi, col0, nch, xf, xnb, s3, gname, ndim):
            t0, n = blocks[bi]
            for c in range(nch):
                pbx = proj_fm(bi, col0 + c * 128, 128)
                CP("act", xf[:, c, 0:n], pbx[:, 0:n], [pbx.k], [(xf.k, c)])
                ACT(s3[:, c, 0:n], pbx[:, 0:n], AF.Square, [pbx.k], [(s3.k, c)])
            yield
            pss = PSA.next()
            for c in range(nch):
                MM(pss[:, 0:n], ones_b[:], s3[:, c, 0:n], c == 0, c == nch - 1, [ones_b.k, (s3.k, c)], [pss.k])
            r3 = rms_rstd(pss[:, 0:n], n, 1.0 / ndim, 128, std_r, [pss.k])
            for c in range(nch):
                STT("dve", xnb[:, c, 0:n], xf[:, c, 0:n], smc(gname, c), r3[:, 0:n], ALU.mult, ALU.mult,
                    [(xf.k, c), r3.k, smalls.k], [(xnb.k, c)])

        def job_mq(bi, h):
            t0, n = blocks[bi]
            pbx = proj_fm(bi, h * 96, 96, wt=wuq, nk=3, src=cqn, srck=[(cqn.k, c) for c in range(3)])
            yield from norm_rope(bi, pbx, 96, blk96_b[0:96, 0:96], blk96_b.k, smc("mqg", p1=96), rmT_b[0:96, 0:96], cmatb.k,
                                 2, 3, MQ[h, :, t0:t0 + n], ("MQ", h, t0))

        def job_mk(bi, h):
            t0, n = blocks[bi]
            pbx = proj_fm(bi, h * 128, 64, wt=wukv, nk=2, src=ckvn, srck=[(ckvn.k, c) for c in range(2)])
            yield from norm_rope(bi, pbx, 64, blk64_b[0:64, 0:64], blk64_b.k, smc("mkg", p1=64), None, None, None, None,
                                 MK[h, 0:64, t0:t0 + n], ("MKn", h, t0))

        def job_mv(bi, tt):
            t0, n = blocks[bi]
            pbx = PSA.next()
            for k in range(2):
                MM(pbx[:, 0:512].rearrange("p (h d) -> p h d", h=8), ckvn[:, k, tt * 128:(tt + 1) * 128],
                   wukv[:, k, :].rearrange("p (h d) -> p h d", h=8)[:, :, 64:128], k == 0, k == 1, [(ckvn.k, k), wukv.k], [pbx.k])
            o = b16_r.next()
            CP("act", o[:, 0:512], pbx[:, 0:512], [pbx.k], [o.k])
            P.dma("sp", MV[t0 + tt * 128:t0 + (tt + 1) * 128, :], o[:, 0:512], [o.k], [("MV", t0, tt)])
            return
            yield

        def job_kr(bi):
            t0, n = blocks[bi]
            pbx = proj_fm(bi, 3728, 32)
            yield from norm_rope(bi, pbx, 32, blk32_b[:], blk32_b.k, smc("mkrg", p1=32), rkT_b[:], rkT_b.k, 4, 5,
                                 [MK[h, 64:96, t0:t0 + n] for h in range(8)], ("MKr", t0))

        st["act_rstd"] = True
        run_jobs([prep(0)])
        gens = []
        for bi, (t0, n) in enumerate(blocks):
            jl = [job_lat(bi, 3088, 3, cqf, cqn, sqcq, "cqg", 384), job_lat(bi, 3472, 2, ckvf, ckvn, sqckv, "ckvg", 256)]
            jl += [job_dqk(bi, h, False) for h in range(4)]
            jl += [job_dqk(bi, h, True) for h in range(4)]
            for tt in range(n // 128):
                jl += [job_tm(bi, tt, 1024, 512, DV, BF16), job_tm(bi, tt, 1536, 512, Zs, BF16), job_tm(bi, tt, 3072, 16, DTs, F32)]
            jl += [job_xbc(bi, c) for c in range(8)]
            if bi + 1 < len(blocks):
                jl.append(prep(bi + 1))
            jl += [job_mq(bi, h) for h in range(8)]
            jl += [job_mk(bi, h) for h in range(8)]
            jl += [job_mv(bi, tt) for tt in range(n // 128)]
            jl.append(job_kr(bi))
            gens += jl
        run_jobs(gens)
        st["act_rstd"] = False
        P.barrier()

    def phaseB(l, need_ctx):
        st["off"] = SB_PERSIST
        kt_r = sbring("kt", 2, [128, 2, T], BF16)
        for b in kt_r.bufs:
            MEMSET("pool", b[:], 0.0, [b.k])
        qt_r = sbring("qt", 2, [128, T], BF16)
        vd_r = sbring("vd", 2, [128, NT, 128], BF16)
        vm_r = sbring("vm", 2, [128, NT, 65], BF16)
        for b in vm_r.bufs:
            MEMSET("pool", b[:, :, 64:65], 1.0, [b.k])
        pT_r = sbring("pT", 8, [128, 512], BF16)
        f32_r = sbring("bf", 6, [128, 512])
        b16_r = sbring("bb", 3, [128, 512], BF16)
        wst_r = sbring("wst", 3, [128, 6144], BF16)
        for e in range(32):
            w = wst_r.next()
            P.dma("pool", w[:, 0:2048].rearrange("p (c n) -> p c n", c=8), moe_wg[l, e].rearrange("(c p) n -> p c n", p=128), (), [(w.k, 0)])
            P.dma("pool", w[:, 2048:4096].rearrange("p (c n) -> p c n", c=8), moe_wu[l, e].rearrange("(c p) n -> p c n", p=128), (), [(w.k, 1)])
            P.dma("pool", w[:, 4096:6144].rearrange("p (c n) -> p c n", c=2), moe_wd[l, e].rearrange("(c p) n -> p c n", p=128), (), [(w.k, 2)])
            P.dma("pool", WBs[e * 128:(e + 1) * 128, :], w[:], [(w.k, 0), (w.k, 1), (w.k, 2)], ["WB", (w.k, 0), (w.k, 1), (w.k, 2)])
        SC = Ring(pbanks[0:4])
        OA = Ring(pbanks[4:6])
        SA = Ring(pbanks[6:8])
        qblocks = [(CTX + 512 * i, 512, NT) for i in range(S // 512)]
        if need_ctx:
            qblocks = [(0, CTX, CTX // 128)] + qblocks

        PD = 4

        def run_head(kt, qt, vv, is_diff, h):
            groups = []
            for (q0, nq, nkt) in qblocks:
                for comp in range(2 if is_diff else 1):
                    groups.append((q0, nq, nkt, comp))
            items = []
            for gi, (q0, nq, nkt, comp) in enumerate(groups):
                for ki in range(nkt):
                    items.append((gi, ki))
            gstate = {}
            pTs = {}
            oc_hold = {}

            def issue_sc(idx):
                gi, ki = items[idx]
                q0, nq, nkt, comp = groups[gi]
                sc = SC.next()
                scale = 0.125 if is_diff else mscale
                MM(sc[:, 0:nq], kt[:, comp, ki * 128:(ki + 1) * 128], qt[:, q0:q0 + nq], True, True, [kt.k, qt.k], [sc.k])
                pT = pT_r.next()
                ACT(pT[:, 0:nq], sc[:, 0:nq], AF.Exp, [sc.k], [pT.k], scale=scale)
                pTs[idx] = pT

            def issue_av(idx):
                gi, ki = items[idx]
                q0, nq, nkt, comp = groups[gi]
                if ki == 0:
                    gstate[gi] = (OA.next(), SA.next() if is_diff else None)
                O, Sm = gstate[gi]
                pT = pTs.pop(idx)
                if is_diff:
                    MM(O[:, 0:nq], vv[:, ki, :], pT[:, 0:nq], ki == 0, ki == nkt - 1, [vv.k, pT.k], [O.k])
                    MM(Sm[:, 0:nq], ones_b[:], pT[:, 0:nq], ki == 0, ki == nkt - 1, [ones_b.k, pT.k], [Sm.k])
                else:
                    MM(O[0:65, 0:nq], vv[:, ki, :], pT[:, 0:nq], ki == 0, ki == nkt - 1, [vv.k, pT.k], [O.k])
                if ki != nkt - 1:
                    return
                del gstate[gi]
                if is_diff:
                    rc = f32_r.next()
                    RECIP(rc[:, 0:nq], Sm[:, 0:nq], [Sm.k], [rc.k])
                    o_ = oc_r.next()
                    TT("dve", o_[:, 0:nq], O[:, 0:nq], rc[:, 0:nq], ALU.mult, [O.k, rc.k], [o_.k])
                    if comp == 0:
                        oc_hold[q0] = o_
                        return
                    o0 = oc_hold.pop(q0)
                    o = f32_r.next()
                    STT("dve", o[:, 0:nq], o_[:, 0:nq], neglam[:, 0:1], o0[:, 0:nq], ALU.mult, ALU.add,
                        [o0.k, o_.k, neglam.k], [o.k])
                    s2 = b16_r.next()
                    ACT(s2[:, 0:nq], o[:, 0:nq], AF.Square, [o.k], [s2.k])
                    pss = SC.next()
                    MM(pss[:, 0:nq], mean128_b[:], s2[:, 0:nq], True, True, [mean128_b.k, s2.k], [pss.k])
                    r2 = f32_r.next()
                    ACT(r2[:, 0:nq], pss[:, 0:nq], AF.Sqrt, [pss.k], [r2.k], scale=1.0, bias=EPS)
                    RECIP(r2[:, 0:nq], r2[:, 0:nq], [r2.k], [r2.k])
                    ob = b16_r.next()
                    STT("dve", ob[:, 0:nq], o[:, 0:nq], gsub[:, 0:1], r2[:, 0:nq], ALU.mult, ALU.mult, [o.k, gsub.k, r2.k], [ob.k])
                    P.dma("sp", DOs[h * 128:(h + 1) * 128, q0:q0 + nq], ob[:, 0:nq], [ob.k], [("DO", h, q0)])
                else:
                    oa = f32_r.next()
                    CP("act", oa[0:65, 0:nq], O[0:65, 0:nq], [O.k], [oa.k])
                    pbc = SC.next()
                    MM(pbc[0:64, 0:nq], sel65[:], oa[0:65, 0:nq], True, True, [sel65.k, oa.k], [pbc.k])
                    rc = f32_r.next()
                    RECIP(rc[0:64, 0:nq], pbc[0:64, 0:nq], [pbc.k], [rc.k])
                    ob = b16_r.next()
                    TT("dve", ob[0:64, 0:nq], oa[0:64, 0:nq], rc[0:64, 0:nq], ALU.mult, [oa.k, rc.k], [ob.k])
                    P.dma("sp", MOs[h * 64:(h + 1) * 64, q0:q0 + nq], ob[0:64, 0:nq], [ob.k], [("MO", h, q0)])

            N = len(items)
            for idx in range(N + PD):
                if idx < N:
                    issue_sc(idx)
                if idx >= PD:
                    issue_av(idx - PD)

        mscale = 96.0 ** -0.5
        oc_r = sbring("oc", 3, [128, 512])
        for h in range(4):
            kt, qt, vv = kt_r.next(), qt_r.next(), vd_r.next()
            P.dma("sp", kt[0:64, 0, :], DK[h, 0:64, :], (), [kt.k])
            P.dma("sp", kt[64:128, 1, :], DK[h, 64:128, :], (), [kt.k])
            P.dma("sp", qt[:], DQ[h], (), [qt.k])
            P.dma("sp", vv[:], DV[:, h * 128:(h + 1) * 128].rearrange("(t p) d -> p t d", p=128), (), [vv.k])
            run_head(kt, qt, vv, True, h)
        for h in range(8):
            kt, qt, vv = kt_r.next(), qt_r.next(), vm_r.next()
            P.dma("sp", kt[0:96, 0, :], MK[h], (), [kt.k])
            P.dma("sp", qt[0:96, :], MQ[h], (), [qt.k])
            P.dma("sp", vv[:, :, 0:64], MV[:, h * 64:(h + 1) * 64].rearrange("(t p) d -> p t d", p=128), (), [vv.k])
            run_head(kt, qt, vv, False, h)
        P.barrier()


    pbanks_b = [Buf(pb.t.bitcast(BF16), pb.k) for pb in pbanks]

    def phaseC(l, need_ctx):
        st["off"] = SB_PERSIST
        U = sb("U", [128, 8, T], BF16)
        base_after_U = st["off"]
        W = T + 8
        WO = W - 4
        uin_r = sbring("uin", 2, [128, W], BF16)
        for b in uin_r.bufs:
            MEMSET("pool", b[:, 0:2], 0.0, [b.k])
            MEMSET("pool", b[:, 258:262], 0.0, [b.k])
            MEMSET("pool", b[:, 262 + S:264 + S], 0.0, [b.k])
        cw0, _ = SM["convw"]
        cb0, _ = SM["convb"]
        dg = sb("dg", [128, 40, 128], BF16)
        for i in range(40):
            TS("dve" if i % 2 == 0 else "pool", dg[:, i, :], ident_f, smalls[:, cw0 + i:cw0 + i + 1], None, ALU.mult, None,
               [cmat.k, smalls.k], [(dg.k, i)])
        PSC = Ring(pbanks, transient=True)
        cblocks = [(0, CTX, 0)] + [(260 + 512 * i, 512, CTX + 512 * i) for i in range(S // 512)]
        for c in range(8):
            uin = uin_r.next()
            P.dma("sp", uin[:, 2:258], XBC[c * 128:(c + 1) * 128, 0:CTX], (), [uin.k])
            P.dma("sp", uin[:, 262:262 + S], XBC[c * 128:(c + 1) * 128, CTX:T], (), [uin.k])
            for (j0, w, u0) in cblocks:
                pc = PSC.next()
                for k in range(5):
                    MM(pc[:, 0:w], dg[:, c * 5 + k, :], uin[:, j0 + k:j0 + k + w], k == 0, k == 4, [(dg.k, c * 5 + k), uin.k], [pc.k])
                ACT(U[:, c, u0:u0 + w], pc[:, 0:w], AF.Silu, [pc.k, smalls.k], [U.k], bias=smalls[:, cb0 + c:cb0 + c + 1])
        P.barrier()
        st["off"] = base_after_U
        dtr = sb("dtr", [128, NT, 16])
        dtt = sb("dtt", [128, NT, 16])
        dtu = sb("dtu", [128, NT, 16])
        dtA = sb("dtA", [128, NT, 16])
        Arow = sb("Arow", [128, 16])
        Drow = sb("Drow", [128, 8])
        P.dma("sp", dtr[:], DTs.rearrange("(t p) d -> p t d", p=128), (), [dtr.k])
        c0, _ = RW["dtb"]
        TT("dve", dtr[:], dtr[:], rows[:, c0:c0 + 16].unsqueeze(1).to_broadcast([128, NT, 16]), ALU.add, [dtr.k, rows.k], [dtr.k])
        ACT(dtt[:], dtr[:], AF.Abs, [dtr.k], [dtt.k])
        ACT(dtt[:], dtt[:], AF.Exp, [dtt.k], [dtt.k], scale=-1.0)
        ACT(dtt[:], dtt[:], AF.Ln, [dtt.k], [dtt.k], bias=1.0)
        TS("dve", dtu[:], dtr[:], 0.0, None, ALU.max, None, [dtr.k], [dtu.k])
        TT("dve", dtu[:], dtu[:], dtt[:], ALU.add, [dtu.k, dtt.k], [dtu.k])
        c0, _ = RW["alog"]
        ACT(Arow[:], rows[:, c0:c0 + 16], AF.Exp, [rows.k], [Arow.k])
        TS("dve", Arow[:], Arow[:], -1.0, None, ALU.mult, None, [Arow.k], [Arow.k])
        TT("dve", dtA[:], dtu[:], Arow[:].unsqueeze(1).to_broadcast([128, NT, 16]), ALU.mult, [dtu.k, Arow.k], [dtA.k])
        c0, _ = RW["ssdD"]
        CP("dve", Drow[:], rows[:, c0:c0 + 8], [rows.k], [Drow.k])
        g0, _ = RW["ssdg"]

        hs = [sb(f"hs{d}", [128, 512]) for d in range(2)]
        hsb = [sb(f"hsb{d}", [128, 512], BF16) for d in range(2)]
        for d in range(2):
            MEMSET("dve", hs[d][:], 0.0, [hs[d].k])
            MEMSET("pool", hsb[d][:], 0.0, [hsb[d].k])
        xs_r = sbring("xs", 4, [128, 512], BF16)
        bt_r = sbring("bt", 4, [128, 256], BF16)
        cbT_r = sbring("cbT", 4, [128, 2, 128], BF16)
        ex_r = sbring("ex", 4, [128, 24])
        xdt_r = sbring("xdt", 4, [128, 512], BF16)
        xw_r = sbring("xw", 4, [128, 512], BF16)
        E_r = sbring("E", 4, [128, 128], BF16)
        E_r.transient = True
        mix_r = sbring("mix", 4, [128, 8, 128], BF16)
        y_r = sbring("y", 4, [128, 512])
        yf_r = sbring("yf", 4, [128, 512])
        z_r = sbring("z", 4, [128, 512], BF16)
        zg_r = sbring("zg", 4, [128, 512])
        ob_r = sbring("ob", 4, [128, 512], BF16)
        oT_r = sbring("oT", 4, [128, 4, 128], BF16)
        sm_r = sbring("sm", 6, [128, 2])
        tmp_r = sbring("tmp", 4, [128, 512])
        SEGR = Ring(pbanks[0:3], transient=True)
        MISC = Ring([3, 4])
        YD, YO, STP = pbanks[5], pbanks[6], pbanks[7]
        cmf = lambda nm: cmat[:, CM[nm], :]

        def chunk(ck, d, finalize):
            cs = slice(ck * 128, (ck + 1) * 128)
            hd = d * 8
            tri, ntri, strict, negm = (("triU", "negTriU", "strictL", "negmF") if d == 0 else ("triL", "negTriL", "strictU", "negmB"))
            mi = MISC.next()
            pt = pbanks_b[mi]
            for c in range(4):
                TR(pt[:, c * 128:(c + 1) * 128], U[:, c, cs], ident_b, [U.k, cmatb.k], [pt.k])
            xs = xs_r.next()
            CP("act", xs[:], pt[:, 0:512], [pt.k], [xs.k])
            mi = MISC.next()
            pt2 = pbanks_b[mi]
            for g in range(2):
                TR(pt2[:, g * 128:(g + 1) * 128], U[:, 4 + g, cs], ident_b, [U.k, cmatb.k], [pt2.k])
            bt = bt_r.next()
            CP("act", bt[:], pt2[:, 0:256], [pt2.k], [bt.k])
            mi = MISC.next()
            pc = pbanks[mi]
            for g in range(2):
                MM(pc[:, g * 128:(g + 1) * 128], U[:, 4 + g, cs], U[:, 6 + g, cs], True, True, [U.k], [pc.k])
            cbT = cbT_r.next()
            CP("dve", cbT[:], pc[:, 0:256].rearrange("p (g i) -> p g i", g=2), [pc.k], [cbT.k])
            mi = MISC.next()
            pa = pbanks[mi]
            dcol = dtA[:, ck, hd:hd + 8]
            MM(pa[:, 0:8], cmf(tri), dcol, True, True, [cmat.k, dtA.k], [pa.k])
            MM(pa[:, 8:16], cmf(strict), dcol, True, True, [cmat.k, dtA.k], [pa.k])
            MM(pa[:, 16:24], ones_f[:], dcol, True, True, [ones_f.k, dtA.k], [pa.k])
            ex = ex_r.next()
            ACT(ex[:], pa[:, 0:24], AF.Exp, [pa.k], [ex.k])
            xdt = xdt_r.next()
            TT("dve", xdt[:].rearrange("p (h q) -> p h q", h=8), xs[:].rearrange("p (h q) -> p h q", h=8),
               dtu[:, ck, hd:hd + 8].unsqueeze(2).to_broadcast([128, 8, 64]), ALU.mult, [xs.k, dtu.k], [xdt.k])
            xw = xw_r.next()
            TT("pool", xw[:].rearrange("p (h q) -> p h q", h=8), xdt[:].rearrange("p (h q) -> p h q", h=8),
               ex[:, 8:16].unsqueeze(2).to_broadcast([128, 8, 64]), ALU.mult, [xdt.k, ex.k], [xw.k])
            mixj = mix_r.next()
            for h in range(8):
                g = h // 4
                sg = SEGR.next()
                dbc = dtA[:, ck, hd + h:hd + h + 1].to_broadcast([128, 128])
                MM(sg[:, 0:128], dbc, cmf(tri), True, False, [dtA.k, cmat.k], [sg.k])
                MM(sg[:, 0:128], cmf(ntri), dbc, False, False, [dtA.k, cmat.k], [sg.k])
                MM(sg[:, 0:128], ident_f, cmf(negm), False, True, [cmat.k], [sg.k])
                E = E_r.next()
                ACT(E[:], sg[:, 0:128], AF.Exp, [sg.k], [E.k])
                TT("dve" if h % 2 == 0 else "pool", mixj[:, h, :], E[:], cbT[:, g, :], ALU.mult, [E.k, cbT.k], [(mixj.k, h)])
            yield
            for h in range(8):
                MM(YD[:, h * 64:(h + 1) * 64], mixj[:, h, :], xdt[:, h * 64:(h + 1) * 64], True, True, [(mixj.k, h), xdt.k], [YD.k])
            for g in range(2):
                MM(YO[:, g * 256:(g + 1) * 256], U[:, 6 + g, cs], hsb[d][:, g * 256:(g + 1) * 256], True, True, [U.k, hsb[d].k], [YO.k])
            y = y_r.next()
            TT("dve", y[:].rearrange("p (h q) -> p h q", h=8), YO[:, 0:512].rearrange("p (h q) -> p h q", h=8),
               ex[:, 0:8].unsqueeze(2).to_broadcast([128, 8, 64]), ALU.mult, [YO.k, ex.k], [y.k])
            TT("dve", y[:], y[:], YD[:, 0:512], ALU.add, [y.k, YD.k], [y.k])
            for g in range(2):
                MM(STP[:, g * 256:(g + 1) * 256], bt[:, g * 128:(g + 1) * 128], xw[:, g * 256:(g + 1) * 256], True, True, [bt.k, xw.k], [STP.k])
            TT("dve", hs[d][:].rearrange("p (h q) -> p h q", h=8), hs[d][:].rearrange("p (h q) -> p h q", h=8),
               ex[:, 16:24].unsqueeze(2).to_broadcast([128, 8, 64]), ALU.mult, [hs[d].k, ex.k], [hs[d].k])
            TT("dve", hs[d][:], hs[d][:], STP[:, 0:512], ALU.add, [hs[d].k, STP.k], [hs[d].k])
            CP("pool", hsb[d][:], hs[d][:], [hs[d].k], [hsb[d].k])
            rows_ck = slice(ck * 128, (ck + 1) * 128)
            if d == 0:
                tmp = tmp_r.next()
                TT("pool", tmp[:].rearrange("p (h q) -> p h q", h=8), xs[:].rearrange("p (h q) -> p h q", h=8),
                   Drow[:].unsqueeze(2).to_broadcast([128, 8, 64]), ALU.mult, [xs.k, Drow.k], [tmp.k])
                TT("pool", y[:], y[:], tmp[:], ALU.add, [y.k, tmp.k], [y.k])
                P.dma("sp", YF[rows_ck, :], y[:], [y.k], [("YF", ck)])
                return
            if not finalize:
                return
            yf = yf_r.next()
            P.dma("sp", yf[:], YF[rows_ck, :], [("YF", ck)], [yf.k])
            z = z_r.next()
            P.dma("sp", z[:], Zs[rows_ck, :], (), [z.k])
            zg = zg_r.next()
            ACT(zg[:], z[:], AF.Silu, [z.k], [zg.k])
            TT("pool", y[:], y[:], yf[:], ALU.add, [y.k, yf.k], [y.k])
            TT("dve", y[:], y[:], zg[:], ALU.mult, [y.k, zg.k], [y.k])
            sm = sm_r.next()
            MEMSET("dve", sm[:], 0.0, [sm.k])
            ACT(zg[:], y[:], AF.Square, [y.k, zg.k], [zg.k, sm.k], accum_out=sm[:, 0:1])
            ACT(sm[:, 1:2], sm[:, 0:1], AF.Sqrt, [sm.k], [sm.k], scale=1.0 / 512, bias=EPS)
            RECIP(sm[:, 1:2], sm[:, 1:2], [sm.k], [sm.k])
            ob = ob_r.next()
            STT("dve", ob[:], y[:], sm[:, 1:2], rows[:, g0:g0 + 512], ALU.mult, ALU.mult, [y.k, sm.k, rows.k], [ob.k])
            yield
            mi = MISC.next()
            po = pbanks_b[mi]
            for c in range(4):
                TR(po[:, c * 128:(c + 1) * 128], ob[:, c * 128:(c + 1) * 128], ident_b, [ob.k, cmatb.k], [po.k])
            oT = oT_r.next()
            CP("act", oT[:], po[:, 0:512].rearrange("p (c i) -> p c i", c=4), [po.k], [oT.k])
            P.dma("sp", SOs[:, rows_ck].rearrange("(c p) i -> p c i", p=128), oT[:], [oT.k], [("SO", ck)])

        nctx = CTX // 128
        order = list(range(nctx - 1, -1, -1)) + list(range(NT - 1, nctx - 1, -1))
        run_jobs([chunk(ck, 0, False) for ck in range(NT)])
        run_jobs([chunk(ck, 1, need_ctx or ck >= nctx) for ck in order])
        P.barrier()

    def phaseD(l, Xin, need_ctx):
        st["off"] = SB_PERSIST
        st["hi"] = SB_META
        wg = sb("wg", [128, 8, 3072], BF16)
        for c in range(8):
            P.dma("pool", wg[:, c, :], w_in[l, c * 128:(c + 1) * 128, 3760:6832], (), [(wg.k, c)])
        wb = sb("wb", [128, 3, 4, 1024], BF16)
        for k in range(3):
            P.dma("pool", wb[:, k], w_branch[l, k].rearrange("(c p) n -> p c n", p=128), (), [(wb.k, k)])
        wo = sb("wo", [128, 8, 1024], BF16)
        P.dma("pool", wo[:], w_out[l].rearrange("(c p) n -> p c n", p=128), (), [wo.k])
        wr = sb("wr", [128, 8, 36])
        P.dma("sp", wr[:], router_w[l].rearrange("(c p) n -> p c n", p=128), (), [wr.k])
        hT_r = sbring("dhT", 1, [128, 8, 512], BF16)
        o3_r = sbring("o3", 1, [128, 3, 4, 512], BF16)
        xb_r = sbring("dxb", 1, [128, 8, 512])
        acc = sb("dacc", [128, 8, 512])
        accb = sb("daccb", [128, 8, 512], BF16)
        h2f = sb("dh2f", [128, 8, 512])
        h2b = sb("dh2b", [128, 8, 512], BF16)
        g_r = sbring("dg", 3, [128, 512], BF16)
        gm_r = sbring("dgm", 2, [128, 512])
        std_r = sbring("dstd", 1, [128, 512])
        lg_r = sbring("lg", 2, [128, 36])
        sc_r = sbring("rsc", 2, [128, 16])
        m_r = sbring("rm", 2, [128, 4, 32])
        htm_r = sbring("htm", 2, [128, D], BF16)
        htm_r.transient = True
        rt_r = sbring("rt", 2, [128, 2, 32])
        rt_r.transient = True
        MK1, MK2, W12, RK, Macc = meta_bufs(f"d{l}")
        MEMSET("dve", Macc[:], 0.0, [Macc.k])
        for r_ in (g_r, gm_r, std_r, lg_r, sc_r, m_r):
            r_.transient = True
        PSD = Ring(pbanks, transient=True)
        rb0, _ = RW["rb"]
        BIG = 30000.0
        blocks = blocks_for(need_ctx)
        BD = {}
        ack = [(acc.k, c) for c in range(8)]
        abk = [(accb.k, c) for c in range(8)]
        hfk = [(h2f.k, c) for c in range(8)]

        def load_ho(bi):
            t0, n = blocks[bi]
            hT, o3 = hT_r.next(), o3_r.next()
            BD[bi] = [hT, o3, None]
            P.dma("sp", hT[:, :, 0:n], Hs[:, t0:t0 + n].rearrange("(c p) n -> p c n", p=128), (), [hT.k])
            for k, src in enumerate((DOs, SOs, MOs)):
                P.dma("sp", o3[:, k, :, 0:n], src[:, t0:t0 + n].rearrange("(c p) n -> p c n", p=128), (), [(o3.k, k)])
            return
            yield

        def load_x(bi):
            t0, n = blocks[bi]
            xb = xb_r.next()
            BD[bi][2] = xb
            P.dma("sp", xb[:, :, 0:n], Xin[:, t0:t0 + n].rearrange("(c p) n -> p c n", p=128), (), [xb.k])
            return
            yield

        def gjob(bi, c, k):
            t0, n = blocks[bi]
            hT, o3, _ = BD[bi]
            pg = PSD.next()
            for kc in range(8):
                MM(pg[:, 0:n], wg[:, kc, k * 1024 + c * 128:k * 1024 + (c + 1) * 128], hT[:, kc, 0:n], kc == 0, kc == 7,
                   [(wg.k, kc), hT.k], [pg.k])
            g = g_r.next()
            ACT(g[:, 0:n], pg[:, 0:n], AF.Sigmoid, [pg.k], [g.k])
            pm = PSD.next()
            for kc in range(4):
                MM(pm[:, 0:n], wb[:, k, kc, c * 128:(c + 1) * 128], o3[:, k, kc, 0:n], kc == 0, kc == 3,
                   [(wb.k, k), (o3.k, k)], [pm.k])
            ak = (acc.k, c)
            if k == 0:
                TT("dve", acc[:, c, 0:n], pm[:, 0:n], g[:, 0:n], ALU.mult, [pm.k, g.k], [ak])
            else:
                gm = gm_r.next()
                TT("dve", gm[:, 0:n], pm[:, 0:n], g[:, 0:n], ALU.mult, [pm.k, g.k], [gm.k])
                if k == 1:
                    TT("pool", acc[:, c, 0:n], acc[:, c, 0:n], gm[:, 0:n], ALU.add, [ak, gm.k], [ak])
                else:
                    TT("pool", accb[:, c, 0:n], acc[:, c, 0:n], gm[:, 0:n], ALU.add, [ak, gm.k], [(accb.k, c)])
            return
            yield

        def tail(bi):
            t0, n = blocks[bi]
            wh = 1 if t0 == 0 else 0
            xb = BD[bi][2]
            for c in range(8):
                py = PSD.next()
                for kc in range(8):
                    MM(py[:, 0:n], wo[:, kc, c * 128:(c + 1) * 128], accb[:, kc, 0:n], kc == 0, kc == 7,
                       [wo.k, (accb.k, kc)], [py.k])
                STT("dve", xb[:, c, 0:n], py[:, 0:n], modv[:, 2 * 8 + c, wh:wh + 1], xb[:, c, 0:n], ALU.mult, ALU.add,
                    [py.k, modv.k, xb.k], [xb.k])
            P.dma("sp", XM[:, t0:t0 + n].rearrange("(c p) n -> p c n", p=128), xb[:, :, 0:n], [xb.k], [("XM", t0)])
            ACT(accb[:, :, 0:n], xb[:, :, 0:n], AF.Square, [xb.k], abk)
            pb = PSD.next()
            for c in range(8):
                MM(pb[:, 0:n], ones_b[:], accb[:, c, 0:n], c == 0, c == 7, [ones_b.k] + abk, [pb.k])
            rs = rms_rstd(pb[:, 0:n], n, 1.0 / D, 128, std_r, [pb.k])
            TT("dve", h2f[:, :, 0:n], xb[:, :, 0:n], rs[:, 0:n].unsqueeze(1).to_broadcast([128, 8, n]), ALU.mult,
               [xb.k, rs.k], hfk)
            for c in range(8):
                ACT(h2f[:, c, 0:n], h2f[:, c, 0:n], AF.Identity, [(h2f.k, c), gs2.k, modv.k], [(h2f.k, c)],
                    scale=gs2[:, c, wh:wh + 1], bias=modv[:, 3 * 8 + c, wh:wh + 1])
            CP("pool", h2b[:, :, 0:n], h2f[:, :, 0:n], hfk, [(h2b.k, c) for c in range(8)])
            for _ in range(5):
                yield
            for tt in range(n // 128):
                pti = PSD.next()
                pt = Buf(pti.t.bitcast(BF16), pti.k)
                for c in range(8):
                    TR(pt[:, c * 128:(c + 1) * 128], h2b[:, c, tt * 128:(tt + 1) * 128], ident_b, [(h2b.k, c), cmatb.k], [pt.k])
                htm = htm_r.next()
                CP("act", htm[:], pt[:, 0:D], [pt.k], [htm.k])
                P.dma("sp", H2TM[t0 + tt * 128:t0 + (tt + 1) * 128, :], htm[:], [htm.k], [("H2TM", t0, tt)])
            for tt in range(n // 128):
                pr = PSD.next()
                for kc in range(8):
                    MM(pr[:, 0:36], h2f[:, kc, tt * 128:(tt + 1) * 128], wr[:, kc, :], kc == 0, kc == 7, [(h2f.k, kc), wr.k], [pr.k])
                lg, sc, m = lg_r.next(), sc_r.next(), m_r.next()
                TT("dve", lg[:], pr[:, 0:36], rows[:, rb0:rb0 + 36], ALU.add, [pr.k, rows.k], [lg.k])
                MEMSET("dve", sc[:], 0.0, [sc.k])
                RED = lambda o, i, r, w: P.op("dve", lambda e: e.tensor_reduce(out=o, in_=i, axis=AX.X, op=ALU.max), r, w)
                RED(sc[:, 0:1], lg[:, 0:4], [lg.k], [sc.k])
                TS("dve", m[:, 0, 0:4], lg[:, 0:4], sc[:, 0:1], None, ALU.is_equal, None, [lg.k, sc.k], [m.k])
                TS("dve", sc[:, 1:2], sc[:, 0:1], -1.0, None, ALU.mult, None, [sc.k], [sc.k])
                ACT(m[:, 0, 8:12], lg[:, 0:4], AF.Exp, [lg.k, sc.k, m.k], [m.k, sc.k], bias=sc[:, 1:2], accum_out=sc[:, 2:3])
                RECIP(sc[:, 3:4], sc[:, 2:3], [sc.k], [sc.k])
                TS("dve", m[:, 0, 4:8], m[:, 0, 0:4], -1.0, BIG, ALU.add, ALU.mult, [m.k], [m.k])
                TT("dve", m[:, 1, :].rearrange("p (g e) -> p g e", g=4), lg[:, 4:36].rearrange("p (g e) -> p g e", g=4),
                   m[:, 0, 4:8].unsqueeze(2).to_broadcast([128, 4, 8]), ALU.add, [lg.k, m.k], [m.k])
                RED(sc[:, 4:5], m[:, 1, :], [m.k], [sc.k])
                TS("dve", m[:, 2, :], m[:, 1, :], sc[:, 4:5], None, ALU.is_equal, None, [m.k, sc.k], [m.k])
                STT("dve", m[:, 1, :], m[:, 2, :], -BIG, m[:, 1, :], ALU.mult, ALU.add, [m.k], [m.k])
                RED(sc[:, 5:6], m[:, 1, :], [m.k], [sc.k])
                TS("dve", m[:, 3, :], m[:, 1, :], sc[:, 5:6], None, ALU.is_equal, None, [m.k, sc.k], [m.k])
                TT("dve", sc[:, 6:7], sc[:, 4:5], sc[:, 5:6], ALU.subtract, [sc.k], [sc.k])
                ACT(sc[:, 7:8], sc[:, 6:7], AF.Sigmoid, [sc.k], [sc.k])
                TT("dve", sc[:, 8:9], sc[:, 7:8], sc[:, 3:4], ALU.mult, [sc.k], [sc.k])
                TT("dve", sc[:, 9:10], sc[:, 3:4], sc[:, 8:9], ALU.subtract, [sc.k], [sc.k])
                gt = (t0 + tt * 128) // 128
                CP("dve", MK1[:, gt, :], m[:, 2, :], [m.k], [(MK1.k, gt)])
                CP("pool", MK2[:, gt, :], m[:, 3, :], [m.k], [(MK2.k, gt)])
                CP("dve", W12[:, gt, :], sc[:, 8:10], [sc.k], [(W12.k, gt)])
                TT("dve", m[:, 0, :], m[:, 2, :], m[:, 3, :], ALU.add, [m.k], [m.k])
                pk = PSD.next()
                MM(pk[:, 0:32], cmat[:, CM["strictU"], :], m[:, 0, :], True, False, [cmat.k, m.k], [pk.k])
                MM(pk[:, 0:32], ones_f[:], Macc[:], False, True, [ones_f.k, Macc.k], [pk.k])
                rt = rt_r.next()
                TT("dve", rt[:, 0, :], pk[:, 0:32], m[:, 2, :], ALU.mult, [pk.k, m.k], [rt.k])
                TT("dve", rt[:, 1, :], pk[:, 0:32], m[:, 3, :], ALU.mult, [pk.k, m.k], [rt.k])
                P.op("dve", lambda e, o=RK[:, gt, :], i=rt[:]: e.tensor_reduce(out=o, in_=i, axis=AX.X, op=ALU.add), [rt.k], [(RK.k, gt)])
                TT("dve", Macc[:], Macc[:], m[:, 0, :], ALU.add, [Macc.k, m.k], [Macc.k])

        gens = []
        for bi in range(len(blocks)):
            gens.append(load_ho(bi))
            gj = [gjob(bi, c, k) for c in range(8) for k in range(3)]
            gens += gj[:4] + [load_x(bi)] + gj[4:]
            gens.append(tail(bi))
        run_jobs(gens)
        st["hi"] = SB_HI
        P.barrier()

    def phaseE(l, need_ctx, last):
        st["off"] = SB_PERSIST
        st["hi"] = SB_META
        MK1, MK2, W12, RK, Macc = meta_bufs(f"e{l}")
        gt0 = 0 if need_ctx else CTX // 128
        ntl = NT - gt0
        NB = 2 * ntl + 32
        PSE = Ring(pbanks, transient=True)
        mc = sb("mc", [128, NMC])
        P.dma("sp", mc[:], mconst_d, (), [mc.k])
        RED = lambda o, i, r, w_, op=ALU.add: P.op("dve", lambda e_: e_.tensor_reduce(out=o, in_=i, axis=AX.X, op=op), r, w_)
        mk1k = [(MK1.k, g) for g in range(gt0, NT)]
        mk2k = [(MK2.k, g) for g in range(gt0, NT)]
        rkk = [(RK.k, g) for g in range(gt0, NT)]
        w12k = [(W12.k, g) for g in range(gt0, NT)]
        pc = PSE.next()
        MM(pc[:, 0:32], ones_f[:], Macc[:], True, True, [ones_f.k, Macc.k], [pc.k])
        cnt = sb("cnt", [128, 32])
        CP("dve", cnt[:], pc[:, 0:32], [pc.k], [cnt.k])
        cmp3 = sb("cmp3", [128, 32, NT])
        TT("dve", cmp3[:], cnt[:].unsqueeze(2).to_broadcast([128, 32, NT]), mc[:, 0:NT].unsqueeze(1).to_broadcast([128, 32, NT]),
           ALU.is_gt, [cnt.k, mc.k], [cmp3.k])
        nblk = sb("nblk", [128, 32])
        RED(nblk[:], cmp3[:], [cmp3.k], [nblk.k])
        ptr = PSE.next()
        TR(ptr[0:32, 0:128], nblk[:, 0:32], ident_f, [nblk.k, cmat.k], [ptr.k])
        nbT = sb("nbT", [32, 128])
        CP("act", nbT[:], ptr[0:32, 0:128], [ptr.k], [nbT.k])
        pp = PSE.next()
        MM(pp[:, 0:32], nbT[0:32, :], cmat[0:32, CM["triU"], 0:32], True, True, [nbT.k, cmat.k], [pp.k])
        pend = sb("pend", [128, 32])
        CP("dve", pend[:], pp[:, 0:32], [pp.k], [pend.k])
        pstart = sb("pstart", [128, 32])
        TT("dve", pstart[:], pend[:], nblk[:], ALU.subtract, [pend.k, nblk.k], [pstart.k])
        tmp3 = sb("tmp3", [128, NT, 32])
        psk = sb("psk", [128, NT])
        slotf = sb("slotf", [128, NT, 2])
        SL = sb("SL", [128, NT, 2], I32)
        for k, (MK, mkk) in enumerate(((MK1, mk1k), (MK2, mk2k))):
            TT("dve", tmp3[:, gt0:NT, :], MK[:, gt0:NT, :], pstart[:].unsqueeze(1).to_broadcast([128, ntl, 32]), ALU.mult,
               mkk + [pstart.k, tmp3.k], [tmp3.k])
            RED(psk[:, gt0:NT], tmp3[:, gt0:NT, :], [tmp3.k], [psk.k])
            STT("dve", slotf[:, gt0:NT, k], psk[:, gt0:NT], 128.0, RK[:, gt0:NT, k], ALU.mult, ALU.add, [psk.k] + rkk, [(slotf.k, k)])
        CP("dve", SL[:, gt0:NT, :], slotf[:, gt0:NT, :], [(slotf.k, 0), (slotf.k, 1)], [SL.k])
        cmpb = sb("cmpb", [128, NB, 32])
        TT("dve", cmpb[:], pend[:].unsqueeze(1).to_broadcast([128, NB, 32]), mc[:, 34:34 + NB].unsqueeze(2).to_broadcast([128, NB, 32]),
           ALU.is_le, [pend.k, mc.k], [cmpb.k])
        ebf = sb("ebf", [128, NB])
        RED(ebf[:], cmpb[:], [cmpb.k], [ebf.k])
        TS("dve", ebf[:], ebf[:], 31.0, None, ALU.min, None, [ebf.k], [ebf.k])
        TS("dve", ebf[:], ebf[:], 128.0, mc[:, 138:139], ALU.mult, ALU.add, [ebf.k, mc.k], [ebf.k])
        WIDX = sb("WIDX", [128, NB], I32)
        CP("dve", WIDX[:], ebf[:], [ebf.k], [WIDX.k])
        if "META" in dbg:
            d1 = nc.dram_tensor(f"dbg_SL{l}", [128, NT, 2], I32, kind="ExternalOutput").ap()
            d2 = nc.dram_tensor(f"dbg_WIDX{l}", [128, NB], I32, kind="ExternalOutput").ap()
            d3 = nc.dram_tensor(f"dbg_cnt{l}", [128, 32], F32, kind="ExternalOutput").ap()
            d4 = nc.dram_tensor(f"dbg_pend{l}", [128, 32], F32, kind="ExternalOutput").ap()
            P.dma("sp", d1[:, gt0:NT, :], SL[:, gt0:NT, :], [SL.k], ["d1"])
            P.dma("sp", d2, WIDX[:], [WIDX.k], ["d2"])
            P.dma("sp", d3, cnt[:], [cnt.k], ["d3"])
            P.dma("sp", d4, pend[:], [pend.k], ["d4"])
        if stop_after == ("E1", l):
            st["hi"] = SB_HI
            P.barrier()
            return
        idx_r = sbring("idx", 12, [128, 1], I32)
        idx_r.transient = True

        def IDX(col_ap, rk):
            it = idx_r.next()
            CP("pool", it[:, 0:1], col_ap, rk, [it.k])
            return it

        hrow_r = sbring("hrow", 3, [128, D], BF16)
        for gt in range(gt0, NT):
            hr = hrow_r.next()
            P.dma("sp", hr[:], H2TM[gt * 128:(gt + 1) * 128, :], (), [hr.k])
            for k in range(2):
                it = IDX(SL[:, gt, k:k + 1], [SL.k])
                P.idma(XBs, bass.IndirectOffsetOnAxis(it[:, 0:1].bitcast(U32), 0), hr[:], None, [hr.k, it.k], [hr.k, ("XB", gt, k)])
        P.barrier()
        if stop_after == ("E2", l):
            st["hi"] = SB_HI
            return
        base_blocks = st["off"]
        wt_r = sbring("wt", WT_RING, [128, 6144], BF16)
        wt_r.transient = True
        xtm_r = sbring("xtm", 3, [128, D], BF16)
        xT_r = sbring("xT", 2, [128, 8, 128], BF16)
        sg_r = sbring("ssg", 2, [128, 256], BF16)
        hid_r = sbring("shid", 2, [128, 256], BF16)
        yo_r = sbring("yo", 2, [128, D])
        for r_ in (xT_r, sg_r, hid_r):
            r_.transient = True

        def blockjob(b):
            wt, xtm = wt_r.next(), xtm_r.next()
            it = IDX(WIDX[:, b:b + 1], [WIDX.k])
            P.idma(wt[:], None, WBs, bass.IndirectOffsetOnAxis(it[:, 0:1].bitcast(U32), 0), [it.k], [wt.k])
            if XLOAD:
                P.dma("sp", xtm[:], XBs[b * 128:(b + 1) * 128, :], (), [xtm.k])
            yield
            yield
            if BLK_CUT == 0:
                return
            pti = PSE.next()
            pt = Buf(pti.t.bitcast(BF16), pti.k)
            for c in range(8):
                TR(pt[:, c * 128:(c + 1) * 128], xtm[:, c * 128:(c + 1) * 128], ident_b, [xtm.k, cmatb.k], [pt.k])
            xT = xT_r.next()
            CP("act", xT[:], pt[:, 0:D].rearrange("p (c n) -> p c n", c=8), [pt.k], [xT.k])
            if BLK_CUT == 1:
                return
            pg, pu = PSE.next(), PSE.next()
            for (pb_, off) in ((pg, 0), (pu, 2048)):
                for hc in range(2):
                    for kc in range(8):
                        MM(pb_[:, hc * 128:(hc + 1) * 128], wt[:, off + kc * 256 + hc * 128:off + kc * 256 + (hc + 1) * 128], xT[:, kc, :],
                           kc == 0, kc == 7, [wt.k, xT.k], [pb_.k])
            sg = sg_r.next()
            ACT(sg[:], pg[:, 0:256], AF.Silu, [pg.k], [sg.k])
            hid = hid_r.next()
            TT("dve", hid[:], pu[:, 0:256], sg[:], ALU.mult, [pu.k, sg.k], [hid.k])
            if BLK_CUT == 2:
                return
            yo = yo_r.next()
            for half in range(2):
                py = PSE.next()
                for hc in range(2):
                    MM(py[:, 0:512], hid[:, hc * 128:(hc + 1) * 128], wt[:, 4096 + hc * 1024 + half * 512:4096 + hc * 1024 + (half + 1) * 512],
                       hc == 0, hc == 1, [hid.k, wt.k], [py.k])
                if half == 0:
                    CP("act", yo[:, 0:512], py[:, 0:512], [py.k], [(yo.k, 0)])
                else:
                    CP("dve", yo[:, 512:1024], py[:, 0:512], [py.k], [(yo.k, 1)])
            P.dma("sp", YBs[b * 128:(b + 1) * 128, :], yo[:], [(yo.k, 0), (yo.k, 1)], [("YB", b), (yo.k, 0), (yo.k, 1)])

        run_jobs([blockjob(b) for b in range(NBLK_START, NB if NBLK_DBG is None else NBLK_DBG)])
        P.barrier()
        if stop_after == ("E3", l):
            st["hi"] = SB_HI
            return
        st["off"] = base_blocks
        ya_r = sbring("ya", 4, [128, D])
        yb_r = sbring("yb", 4, [128, D])
        xm_r = sbring("xm", 2, [128, 8, 512])
        blocks = blocks_for(need_ctx)

        def cjob(bi):
            t0, n = blocks[bi]
            wh = 1 if t0 == 0 else 0
            xm = xm_r.next()
            P.dma("sp", xm[:, :, 0:n], XM[:, t0:t0 + n].rearrange("(c p) n -> p c n", p=128), (), [xm.k])
            gath = []
            for tt in range(n // 128):
                gt = (t0 + tt * 128) // 128
                ya, yb = ya_r.next(), yb_r.next()
                ita = IDX(SL[:, gt, 0:1], [SL.k])
                P.idma(ya[:], None, YBs, bass.IndirectOffsetOnAxis(ita[:, 0:1].bitcast(U32), 0), [ita.k], [ya.k])
                itb = IDX(SL[:, gt, 1:2], [SL.k])
                P.idma(yb[:], None, YBs, bass.IndirectOffsetOnAxis(itb[:, 0:1].bitcast(U32), 0), [itb.k], [yb.k])
                gath.append((gt, ya, yb))
            for tt, (gt, ya, yb) in enumerate(gath):
                TS("dve", ya[:], ya[:], W12[:, gt, 0:1], None, ALU.mult, None, [ya.k] + w12k, [ya.k])
                STT("dve", ya[:], yb[:], W12[:, gt, 1:2], ya[:], ALU.mult, ALU.add, [ya.k, yb.k] + w12k, [ya.k])
                for q in range(2):
                    ptq = PSE.next()
                    for c4 in range(4):
                        c = q * 4 + c4
                        TR(ptq[:, c4 * 128:(c4 + 1) * 128], ya[:, c * 128:(c + 1) * 128], ident_f, [ya.k, cmat.k], [ptq.k])
                    for c4 in range(4):
                        c = q * 4 + c4
                        STT("dve", xm[:, c, tt * 128:(tt + 1) * 128], ptq[:, c4 * 128:(c4 + 1) * 128], modv[:, 5 * 8 + c, wh:wh + 1],
                            xm[:, c, tt * 128:(tt + 1) * 128], ALU.mult, ALU.add, [ptq.k, modv.k, xm.k], [xm.k])
            if last:
                P.dma("sp", yT[:, t0 - CTX:t0 - CTX + n].rearrange("(c p) n -> p c n", p=128), xm[:, :, 0:n], [xm.k], [("yT", t0), xm.k])
            else:
                P.dma("sp", XR[:, t0:t0 + n].rearrange("(c p) n -> p c n", p=128), xm[:, :, 0:n], [xm.k], [("XR", t0), xm.k])

        for bi in range(len(blocks)):
            cjob(bi)
        st["hi"] = SB_HI
        P.barrier()

    def finish():
        P.barrier()

    layers = []
    for l in range(depth):
        last = l == depth - 1
        Xin = xT if l == 0 else XR
        phase0(l)
        phaseA(l, Xin)
        if stop_after == ("A", l):
            break
        phaseB(l, not last)
        if stop_after == ("B", l):
            break
        phaseC(l, not last)
        if stop_after == ("C", l):
            break
        phaseD(l, Xin, not last)
        if stop_after == ("D", l):
            break
        phaseE(l, not last, last)
        if stop_after in (("E", l), ("E1", l), ("E2", l), ("E3", l)):
            break
    finish()
    with ExitStack() as stack:
        P.emit(stack)
    return nc, scr


def make_in_maps(inp, S, depth):
    B = inp["x"].shape[0]
    cmat, rope, sel, mconst = host_consts(S)
    smalls = np.stack([pack_smalls(inp, l) for l in range(depth)])
    rows = np.stack([pack_rows(inp, l) for l in range(depth)])
    router_w = np.ascontiguousarray(np.concatenate([inp["moe_group_w"], inp["moe_expert_w"]], axis=-1))
    shared = {
        "ada_w": inp["ada_w"], "w_in": inp["w_in"], "w_uq": inp["w_uq"], "w_ukv": inp["w_ukv"],
        "w_branch": inp["w_branch"], "w_out": inp["w_out"], "router_w": router_w,
        "moe_w_gate": inp["moe_w_gate"], "moe_w_up": inp["moe_w_up"], "moe_w_down": inp["moe_w_down"],
        "smalls": smalls, "rows": rows, "cmat": cmat, "rope": rope, "sel": sel, "mconst": mconst,
    }
    shared = {k: np.ascontiguousarray(v, dtype=np.float32) for k, v in shared.items()}
    maps = []
    for b in range(B):
        xt = np.ascontiguousarray(np.concatenate([inp["ctx"][b], inp["x"][b]], axis=0).T)
        cm = np.stack([inp["c"][b].reshape(8, 128).T, inp["c_ctx"].reshape(8, 128).T], axis=-1)
        m = dict(shared)
        m["xT"] = xt.astype(np.float32)
        m["cmod"] = np.ascontiguousarray(cm, dtype=np.float32)
        maps.append(m)
    return maps


def kernel(**inputs):
    inp = {k: np.asarray(v) for k, v in inputs.items()}
    B, S, _ = inp["x"].shape
    depth = inp["w_in"].shape[0]
    nc, _ = build_program(S, depth)
    maps = make_in_maps(inp, S, depth)
    res = run_bass_kernel_spmd(nc, maps, core_ids=list(range(B)))
    out = np.stack([np.ascontiguousarray(res.results[b]["yT"].T) for b in range(B)])
    return out.astype(np.float32)
```

```python
import math
from contextlib import ExitStack
import numpy as np
import concourse.bass as bass
import concourse.mybir as mybir
from concourse.bass_utils import run_bass_kernel_spmd

F32 = mybir.dt.float32
BF16 = mybir.dt.bfloat16
I32 = mybir.dt.int32
U32 = mybir.dt.uint32
AF = mybir.ActivationFunctionType
ALU = mybir.AluOpType
AX = mybir.AxisListType

EPOCH = 20000
NDMASEM = 8
D = 1024
CTX = 256
EPS = 1e-6
NEG = -30000.0
BLK_CUT = 9
WT_RING = 3
NBLK_DBG = None
NBLK_START = 0
XLOAD = True


class Prog:
    CE = ("pe", "dve", "act", "pool")
    QE = ("sp", "pool", "act")

    def __init__(self, nc):
        self.nc = nc
        self.ops = {e: [] for e in ("pe", "dve", "act", "pool", "sp")}
        self.cnt = {e: 0 for e in self.CE}
        self.dcnt = {q: 0 for q in self.QE}
        self.seen = {e: {} for e in self.ops}
        self.last_w = {}
        self.readers = {}
        self.latest = {}
        self.nsem = set()
        self.sems = {}

    def _tok_compute(self, eng):
        k = self.cnt[eng]
        self.cnt[eng] += 1
        return (("c", eng, k // EPOCH), (k % EPOCH) + 1)

    def _tok_dma(self, q):
        k = self.dcnt[q]
        self.dcnt[q] += 1
        return (("d", q, k % NDMASEM), 16 * (k // NDMASEM + 1))

    def _filter(self, eng, need):
        out = []
        seen = self.seen[eng]
        for sk, v in need.items():
            if sk[0] == "c":
                hi = max([s[2] for s in seen if s[0] == "c" and s[1] == sk[1]] + [-1])
                if hi > sk[2]:
                    continue
            if seen.get(sk, 0) >= v:
                continue
            seen[sk] = v
            out.append((sk, v))
        return out

    def _deps(self, eng, reads, writes):
        need = {}

        def add(tok):
            if tok is None:
                return
            sk, v = tok
            if sk[0] == "c" and sk[1] == eng and eng == "pe":
                return
            if need.get(sk, 0) < v:
                need[sk] = v

        for r in reads:
            add(self.last_w.get(r))
        for w in writes:
            add(self.last_w.get(w))
            for sk, v in self.readers.get(w, {}).items():
                add((sk, v))
        return self._filter(eng, need)

    def _commit(self, tok, reads, writes):
        sk, v = tok
        self.latest[sk] = max(self.latest.get(sk, 0), v)
        self.nsem.add(sk)
        for r in reads:
            d = self.readers.setdefault(r, {})
            if d.get(sk, 0) < v:
                d[sk] = v
        for w in writes:
            self.last_w[w] = tok
            self.readers[w] = {}

    def op(self, eng, fn, reads=(), writes=()):
        waits = self._deps(eng, reads, writes)
        tok = self._tok_compute(eng)
        self.ops[eng].append((waits, fn, tok))
        self._commit(tok, reads, writes)

    def dma(self, q, out, in_, reads=(), writes=()):
        waits = self._deps(q, reads, writes)
        tok = self._tok_dma(q)
        self.ops[q].append((waits, (lambda e, o=out, i=in_: e.dma_start(out=o, in_=i)), tok))
        self._commit(tok, reads, writes)

    def idma(self, out, out_off, in_, in_off, reads=(), writes=()):
        waits = self._deps("pool", reads, writes)
        tok = self._tok_dma("pool")
        self.ops["pool"].append((waits, (lambda e: e.indirect_dma_start(out=out, out_offset=out_off, in_=in_, in_offset=in_off)), tok))
        self._commit(tok, reads, writes)

    def barrier(self):
        need = {}
        for sk, v in self.latest.items():
            if sk[0] == "c":
                hi = max(s[2] for s in self.latest if s[0] == "c" and s[1] == sk[1])
                if sk[2] < hi:
                    continue
            need[sk] = v
        for eng in self.ops:
            w = self._filter(eng, {sk: v for sk, v in need.items() if not (sk[0] == "c" and sk[1] == eng)})
            if w:
                self.ops[eng].append((w, None, None))
        self.last_w = {}
        self.readers = {}

    def emit(self, stack):
        nc = self.nc
        for sk in sorted(self.nsem, key=str):
            self.sems[sk] = stack.enter_context(nc.semaphore("s_" + "_".join(str(x) for x in sk)))
        block = stack.enter_context(nc.Block())
        sems = self.sems

        def run(eobj, lst):
            for waits, fn, tok in lst:
                for sk, v in waits:
                    eobj.wait_ge(sems[sk], v)
                if fn is not None:
                    fn(eobj).then_inc(sems[tok[0]], 16 if tok[0][0] == "d" else 1)

        ops = self.ops

        @block.tensor
        def _(e):
            run(e, ops["pe"])

        @block.vector
        def _(e):
            run(e, ops["dve"])

        @block.scalar
        def _(e):
            run(e, ops["act"])

        @block.gpsimd
        def _(e):
            run(e, ops["pool"])

        @block.sync
        def _(e):
            run(e, ops["sp"])


class Buf:
    def __init__(self, t, k):
        self.t = t
        self.k = k

    def __getitem__(self, idx):
        return self.t[idx]


class Job:
    def __init__(self, gen):
        self.gen = gen
        self.done = False


class Sched:
    cur = None


class Ring:
    def __init__(self, bufs, transient=False):
        self.bufs = bufs
        self.i = 0
        self.transient = transient

    def next(self):
        b = self.bufs[self.i % len(self.bufs)]
        self.i += 1
        if self.transient:
            return b
        own = getattr(b, "owner", None)
        if own is not None and not own.done and own is not Sched.cur:
            raise RuntimeError(f"ring too small: buffer {b.k} still owned by a live job")
        if not isinstance(b, int):
            b.owner = Sched.cur
        return b


def run_jobs(gens):
    active = []
    pending = list(gens)
    while pending or active:
        order = []
        if pending:
            order.append(Job(pending.pop(0)))
        order += list(reversed(active))
        for j in order:
            Sched.cur = j
            try:
                next(j.gen)
                if j not in active:
                    active.append(j)
            except StopIteration:
                j.done = True
                if j in active:
                    active.remove(j)
        Sched.cur = None


SM = {}
_c = 0
for _n, _w in [("n1g", 8), ("n2g", 8), ("adab", 48), ("dqg", 1), ("dkg", 1), ("subg", 1), ("convw", 40), ("convb", 8),
               ("cqg", 3), ("ckvg", 2), ("mqg", 1), ("mkg", 1), ("mkrg", 1)]:
    SM[_n] = (_c, _w)
    _c += _w
NSM = _c
RW = {}
_c = 0
for _n, _w in [("dtb", 16), ("alog", 16), ("ssdD", 8), ("ssdg", 512), ("rb", 36), ("lam", 256)]:
    RW[_n] = (_c, _w)
    _c += _w
NRW = _c
CM = {}
for _i, _n in enumerate(["ident", "rdT", "rmT", "triU", "negTriU", "strictL", "negmF", "triL", "negTriL", "strictU", "negmB"]):
    CM[_n] = _i
NCM = len(CM)
NMC = 140


def _rot_matrix(dim):
    q = dim // 4
    R = np.zeros((dim, dim), np.float32)
    for i in range(q):
        R[i, q + i] = -1.0
        R[q + i, i] = 1.0
        R[2 * q + i, 3 * q + i] = -1.0
        R[3 * q + i, 2 * q + i] = 1.0
    return R


def _rope_tables(S, dim):
    q = dim // 4
    inv = (10000.0 ** (-np.arange(q, dtype=np.float32) / q)).astype(np.float32)
    rows = S // 64
    r = np.repeat(np.arange(rows, dtype=np.float32), 64)
    cc = np.tile(np.arange(64, dtype=np.float32), rows)
    ar = r[:, None] * inv
    ac = cc[:, None] * inv
    ang = np.concatenate([ar, ar, ac, ac], axis=-1)
    return np.cos(ang).astype(np.float32).T, np.sin(ang).astype(np.float32).T


def host_consts(S):
    T = CTX + S
    cm = np.zeros((NCM, 128, 128), np.float32)
    cm[CM["ident"]] = np.eye(128, dtype=np.float32)
    R64 = _rot_matrix(64)
    rd = np.zeros((128, 128), np.float32)
    rd[:64, :64] = R64
    rd[64:, 64:] = R64
    cm[CM["rdT"]] = rd.T
    rm = np.zeros((128, 128), np.float32)
    rm[64:96, 64:96] = _rot_matrix(32)
    cm[CM["rmT"]] = rm.T
    t = np.arange(128)
    triU = (t[:, None] <= t[None, :]).astype(np.float32)
    triL = (t[:, None] >= t[None, :]).astype(np.float32)
    cm[CM["triU"]] = triU
    cm[CM["negTriU"]] = -triU
    cm[CM["strictL"]] = (t[:, None] > t[None, :]).astype(np.float32)
    cm[CM["negmF"]] = NEG * (t[:, None] > t[None, :])
    cm[CM["triL"]] = triL
    cm[CM["negTriL"]] = -triL
    cm[CM["strictU"]] = (t[:, None] < t[None, :]).astype(np.float32)
    cm[CM["negmB"]] = NEG * (t[:, None] < t[None, :])
    cmat = np.ascontiguousarray(cm.transpose(1, 0, 2))
    rope = np.zeros((6, 128, T), np.float32)
    cd, sd = _rope_tables(S, 64)
    rope[0, :, :CTX] = 1.0
    rope[0, :64, CTX:] = cd
    rope[0, 64:, CTX:] = cd
    rope[1, :64, CTX:] = sd
    rope[1, 64:, CTX:] = sd
    cmm, smm = _rope_tables(S, 32)
    rope[2, :, :] = 1.0
    rope[2, 64:96, CTX:] = cmm
    rope[3, 64:96, CTX:] = smm
    rope[4, :, :CTX] = 1.0
    rope[4, :32, CTX:] = cmm
    rope[5, :32, CTX:] = smm
    sel = np.zeros((64, 32, 128), np.float32)
    for e in range(32):
        sel[e, e, :] = 1.0
        sel[32 + e, e, :] = 1.0
    mc = np.zeros((128, NMC), np.float32)
    mc[:, 0:34] = 128.0 * np.arange(34, dtype=np.float32)[None, :]
    mc[:, 34:34 + 104] = np.arange(104, dtype=np.float32)[None, :]
    mc[:, 138] = np.arange(128, dtype=np.float32)
    return cmat, rope, sel, mc


def pack_smalls(inp, l):
    s = np.zeros((128, NSM), np.float32)

    def put(name, arr):
        c0, w = SM[name]
        s[: arr.shape[0], c0:c0 + w] = arr.reshape(arr.shape[0], w)

    put("n1g", inp["norm1_g"][l].reshape(8, 128).T)
    put("n2g", inp["norm2_g"][l].reshape(8, 128).T)
    put("adab", inp["ada_b"][l].reshape(48, 128).T)
    put("dqg", np.tile(inp["diff_q_g"][l], 2)[:, None])
    put("dkg", np.tile(inp["diff_k_g"][l], 2)[:, None])
    put("subg", inp["diff_subln_g"][l][:, None])
    put("convw", inp["ssd_conv_w"][l].T.reshape(8, 128, 5).transpose(1, 0, 2).reshape(128, 40))
    put("convb", inp["ssd_conv_b"][l].reshape(8, 128).T)
    put("cqg", inp["mla_cq_g"][l].reshape(3, 128).T)
    put("ckvg", inp["mla_ckv_g"][l].reshape(2, 128).T)
    put("mqg", inp["mla_q_g"][l][:, None])
    put("mkg", inp["mla_k_g"][l][:64][:, None])
    put("mkrg", inp["mla_k_g"][l][64:][:, None])
    return s


def pack_rows(inp, l):
    r = np.zeros((NRW,), np.float32)

    def put(name, arr):
        c0, w = RW[name]
        r[c0:c0 + w] = arr.reshape(-1)

    put("dtb", inp["ssd_dt_bias"][l])
    put("alog", inp["ssd_A_log"][l])
    put("ssdD", inp["ssd_D"][l])
    put("ssdg", inp["ssd_norm_g"][l])
    put("rb", np.concatenate([inp["moe_group_b"][l], inp["moe_expert_b"][l]]))
    put("lam", inp["diff_lambda"][l])
    return r


def build_program(S, depth, dbg=(), stop_after=None):
    T = CTX + S
    NT = T // 128
    nc = bass.Bass("TRN2", target_bir_lowering=False)
    P = Prog(nc)
    dram_in = {}

    def din(name, shape, dt=F32):
        dram_in[name] = nc.dram_tensor(name, list(shape), dt, kind="ExternalInput").ap()
        return dram_in[name]

    xT = din("xT", [D, T])
    cmod_d = din("cmod", [128, 8, 2])
    ada_w = din("ada_w", [depth, D, 6 * D])
    w_in = din("w_in", [depth, D, 6832])
    w_uq = din("w_uq", [depth, 384, 768])
    w_ukv = din("w_ukv", [depth, 256, 1024])
    w_branch = din("w_branch", [depth, 3, 512, D])
    w_out = din("w_out", [depth, D, D])
    router_w = din("router_w", [depth, D, 36])
    moe_wg = din("moe_w_gate", [depth, 32, D, 256])
    moe_wu = din("moe_w_up", [depth, 32, D, 256])
    moe_wd = din("moe_w_down", [depth, 32, 256, D])
    smalls_d = din("smalls", [depth, 128, NSM])
    rows_d = din("rows", [depth, NRW])
    cmat_d = din("cmat", [128, NCM, 128])
    rope_d = din("rope", [6, 128, T])
    sel_d = din("sel", [64, 32, 128])
    mconst_d = din("mconst", [128, NMC])
    yT = nc.dram_tensor("yT", [D, S], F32, kind="ExternalOutput").ap()

    scr = {}

    def dscr(name, shape, dt):
        kind = "ExternalOutput" if name in dbg else "Internal"
        scr[name] = nc.dram_tensor("scr_" + name, list(shape), dt, kind=kind).ap()
        return scr[name]

    NBMAX = 2 * NT + 32
    WBs = dscr("WB", [32 * 128, 6144], BF16)
    XBs = dscr("XB", [NBMAX * 128, D], BF16)
    YBs = dscr("YB", [NBMAX * 128, D], F32)
    XM = dscr("XM", [D, T], F32)
    XR = dscr("XR", [D, T], F32)
    Hs = dscr("H", [D, T], BF16)
    DQ = dscr("DQ", [4, 128, T], BF16)
    DK = dscr("DK", [4, 128, T], BF16)
    DV = dscr("DV", [T, 512], BF16)
    Zs = dscr("Z", [T, 512], BF16)
    XBC = dscr("XBC", [D, T], BF16)
    DTs = dscr("DT", [T, 16], F32)
    MQ = dscr("MQ", [8, 96, T], BF16)
    MK = dscr("MK", [8, 96, T], BF16)
    MV = dscr("MV", [T, 512], BF16)
    DOs = dscr("DO", [512, T], BF16)
    MOs = dscr("MO", [512, T], BF16)
    SOs = dscr("SO", [512, T], BF16)
    YF = dscr("YF", [T, 512], F32)
    XSs = dscr("XS", [T, 512], BF16)
    BTs = dscr("BT", [T, 256], BF16)
    H2s = dscr("H2", [D, T], BF16)
    CWT = dscr("CWT", [64, T], BF16)
    H2TM = dscr("H2TM", [T, D], BF16)

    SB_LO, SB_HI = 16640, 229376 - 64
    st = {"off": SB_LO, "n": 0}

    def sb(name, shape, dt=F32):
        nbytes = int(np.prod(shape[1:])) * (2 if dt == BF16 else 4)
        nbytes = (nbytes + 63) // 64 * 64
        off = st["off"]
        assert off + nbytes <= st.get("hi", SB_HI), f"SBUF overflow allocating {name}: {off}+{nbytes}"
        st["off"] = off + nbytes
        st["n"] += 1
        t = nc.alloc_sbuf_tensor_at(f"{name}_{st['n']}", list(shape), dt, offset=off)
        return Buf(t, f"{name}_{st['n']}")

    META_BYTES = 5632
    SB_META = SB_HI - META_BYTES

    def meta_bufs(tag):
        off = SB_META
        out = []
        for nm, shape, dt in (("MK1", [128, NT, 32], BF16), ("MK2", [128, NT, 32], BF16), ("W12", [128, NT, 2], F32),
                              ("RK", [128, NT, 2], F32), ("Macc", [128, 32], F32)):
            nb = (int(np.prod(shape[1:])) * (2 if dt == BF16 else 4) + 63) // 64 * 64
            st["n"] += 1
            out.append(Buf(nc.alloc_sbuf_tensor_at(f"{nm}_{tag}_{st['n']}", list(shape), dt, offset=off), f"{nm}"))
            off += nb
        assert off <= SB_HI
        return out

    def sbring(name, n, shape, dt=F32):
        return Ring([sb(f"{name}{i}", shape, dt) for i in range(n)])

    pbanks = [Buf(nc.alloc_psum_tensor(f"pb{i}", [128, 512], F32), f"pb{i}") for i in range(8)]

    def MM(out, lhsT, rhs, start, stop, r, w):
        P.op("pe", lambda e: e.matmul(out, lhsT=lhsT, rhs=rhs, start=start, stop=stop), r, w)

    def TR(out, in_, ident, r, w):
        P.op("pe", lambda e: e.transpose(out, in_, ident), r, w)

    def ACT(out, in_, func, r, w, **kw):
        P.op("act", lambda e: e.activation(out=out, in_=in_, func=func, **kw), r, w)

    def TT(eng, out, in0, in1, op, r, w):
        P.op(eng, lambda e: e.tensor_tensor(out=out, in0=in0, in1=in1, op=op), r, w)

    def TS(eng, out, in0, s1, s2, op0, op1, r, w):
        if s2 is None:
            P.op(eng, lambda e: e.tensor_scalar(out=out, in0=in0, scalar1=s1, scalar2=None, op0=op0), r, w)
        else:
            P.op(eng, lambda e: e.tensor_scalar(out=out, in0=in0, scalar1=s1, scalar2=s2, op0=op0, op1=op1), r, w)

    def STT(eng, out, in0, scalar, in1, op0, op1, r, w):
        P.op(eng, lambda e: e.scalar_tensor_tensor(out=out, in0=in0, scalar=scalar, in1=in1, op0=op0, op1=op1), r, w)

    def CP(eng, out, in_, r, w):
        if eng == "act":
            P.op("act", lambda e: e.copy(out=out, in_=in_), r, w)
        else:
            P.op(eng, lambda e: e.tensor_copy(out=out, in_=in_), r, w)

    def RECIP(out, in_, r, w):
        P.op("dve", lambda e: e.reciprocal(out=out, in_=in_), r, w)

    def MEMSET(eng, ap, val, w):
        P.op(eng, lambda e: e.memset(ap, val), (), w)

    cmat = sb("cmat", [128, NCM, 128])
    P.dma("sp", cmat[:], cmat_d, (), [cmat.k])
    cmatb = sb("cmatb", [128, 3, 128], BF16)
    P.dma("pool", cmatb[:], cmat_d[:, 0:3, :], (), [cmatb.k])
    ident_f = cmat[:, CM["ident"], :]
    ident_b = cmatb[:, 0, :]
    rdT_b = cmatb[:, 1, :]
    rmT_b = cmatb[:, 2, :]
    ones_b = sb("ones_b", [128, 128], BF16)
    MEMSET("dve", ones_b[:], 1.0, [ones_b.k])
    ones_f = sb("ones_f", [128, 128])
    MEMSET("dve", ones_f[:], 1.0, [ones_f.k])
    mean128_b = sb("mean128", [128, 128], BF16)
    MEMSET("dve", mean128_b[:], 1.0 / 128, [mean128_b.k])
    blk64_b = sb("blk64", [128, 128], BF16)
    MEMSET("dve", blk64_b[:], 0.0, [blk64_b.k])
    MEMSET("dve", blk64_b[0:64, 0:64], 1.0 / 64, [blk64_b.k])
    MEMSET("dve", blk64_b[64:128, 64:128], 1.0 / 64, [blk64_b.k])
    blk96_b = sb("blk96", [128, 128], BF16)
    MEMSET("dve", blk96_b[:], 0.0, [blk96_b.k])
    MEMSET("dve", blk96_b[0:64, 0:64], 1.0 / 64, [blk96_b.k])
    MEMSET("dve", blk96_b[64:96, 64:96], 1.0 / 32, [blk96_b.k])
    blk32_b = sb("blk32", [32, 32], BF16)
    MEMSET("dve", blk32_b[:], 1.0 / 32, [blk32_b.k])
    rkT_b = sb("rkT", [32, 32], BF16)
    P.dma("pool", rkT_b[:], cmat_d[64:96, CM["rmT"], 64:96], (), [rkT_b.k])
    sel65 = sb("sel65", [65, 64])
    MEMSET("dve", sel65[:], 0.0, [sel65.k])
    MEMSET("dve", sel65[64:65, :], 1.0, [sel65.k])
    cmod = sb("cmod", [128, 8, 2])
    P.dma("sp", cmod[:], cmod_d, (), [cmod.k])
    silu_c = sb("silu_c", [128, 8, 2])
    ACT(silu_c[:], cmod[:], AF.Silu, [cmod.k], [silu_c.k])
    modv = sb("modv", [128, 48, 2])
    gs1 = sb("gs1", [128, 8, 2])
    gs2 = sb("gs2", [128, 8, 2])
    smalls = sb("smalls", [128, NSM])
    rows = sb("rows", [128, NRW])
    neglam = sb("neglam", [128, 1])
    gsub = sb("gsub", [128, 1])
    lamt = sb("lamt", [128, 4])
    SB_PERSIST = st["off"]

    def smc(name, i=0, n=1, p0=0, p1=128):
        c0, _ = SM[name]
        return smalls[p0:p1, c0 + i:c0 + i + n]

    def blocks_for(include_ctx=True):
        b = [(0, CTX)] if include_ctx else []
        return b + [(CTX + 512 * i, 512) for i in range(S // 512)]

    PS = Ring(pbanks)

    def phase0(l):
        st["off"] = SB_PERSIST
        P.dma("sp", smalls[:], smalls_d[l], (), [smalls.k])
        P.dma("sp", rows[:], rows_d[l:l + 1, :].partition_broadcast(128), (), [rows.k])
        wa = sbring("wa", 2, [128, 8, 768])
        pb = PS.next()
        for j in range(8):
            w = wa.next()
            P.dma("sp", w[:], ada_w[l, :, j * 768:(j + 1) * 768].rearrange("(c p) n -> p c n", p=128), (), [w.k])
            for cc in range(6):
                q = j * 6 + cc
                for k in range(8):
                    MM(pb[:, 2 * q:2 * q + 2], w[:, k, cc * 128:(cc + 1) * 128], silu_c[:, k, :], k == 0, k == 7,
                       [w.k, silu_c.k], [pb.k])
        c0, _ = SM["adab"]
        TT("dve", modv[:], pb[:, 0:96].rearrange("p (q w) -> p q w", w=2),
           smalls[:, c0:c0 + 48].unsqueeze(2).to_broadcast([128, 48, 2]), ALU.add, [pb.k, smalls.k], [modv.k])
        for gs, nm, m in ((gs1, "n1g", 1), (gs2, "n2g", 4)):
            c0, _ = SM[nm]
            TS("dve", gs[:], modv[:, m * 8:(m + 1) * 8, :], 1.0, None, ALU.add, None, [modv.k], [gs.k])
            TT("dve", gs[:], gs[:], smalls[:, c0:c0 + 8].unsqueeze(2).to_broadcast([128, 8, 2]), ALU.mult,
               [gs.k, smalls.k], [gs.k])
        lam_init = 0.8 - 0.6 * math.exp(-0.3 * l)
        c0, _ = RW["lam"]
        prod = sb("lamprod", [128, 128])
        TT("dve", prod[:].rearrange("p (a d) -> p a d", a=2), rows[:, c0:c0 + 256].rearrange("p (a b d) -> p a b d", a=2, b=2)[:, :, 0, :],
           rows[:, c0:c0 + 256].rearrange("p (a b d) -> p a b d", a=2, b=2)[:, :, 1, :], ALU.mult, [rows.k], [prod.k])
        P.op("dve", lambda e: e.tensor_reduce(out=lamt[:, 0:2], in_=prod[:].rearrange("p (a d) -> p a d", a=2), axis=AX.X, op=ALU.add),
             [prod.k], [lamt.k])
        ACT(lamt[:, 2:4], lamt[:, 0:2], AF.Exp, [lamt.k], [lamt.k])
        TT("dve", neglam[:], lamt[:, 3:4], lamt[:, 2:3], ALU.subtract, [lamt.k], [neglam.k])
        TS("dve", neglam[:], neglam[:], -lam_init, None, ALU.add, None, [neglam.k], [neglam.k])
        c0, _ = SM["subg"]
        TS("dve", gsub[:], smalls[:, c0:c0 + 1], 1.0 - lam_init, None, ALU.mult, None, [smalls.k], [gsub.k])
        P.barrier()

    def rms_rstd(ss_ps_ap, n, scale, npart, ring_std, r, w_extra=()):
        sd = ring_std.next()
        if st.get("act_rstd"):
            ACT(sd[0:npart, 0:n], ss_ps_ap, AF.Ln, r, [sd.k], scale=scale, bias=EPS)
            ACT(sd[0:npart, 0:n], sd[0:npart, 0:n], AF.Exp, [sd.k], [sd.k], scale=-0.5)
            return sd
        ACT(sd[0:npart, 0:n], ss_ps_ap, AF.Sqrt, r, [sd.k], scale=scale, bias=EPS)
        RECIP(sd[0:npart, 0:n], sd[0:npart, 0:n], [sd.k], [sd.k])
        return sd

    def phaseA(l, Xin):
        st["off"] = SB_PERSIST
        NCA = 3760
        win = sb("win", [128, 8, NCA], BF16)
        for c in range(8):
            P.dma("pool", win[:, c, :], w_in[l, c * 128:(c + 1) * 128, 0:NCA], (), [(win.k, c)])
        wuq = sb("wuq", [128, 3, 768], BF16)
        P.dma("pool", wuq[:], w_uq[l].rearrange("(c p) n -> p c n", p=128), (), [wuq.k])
        wukv = sb("wukv", [128, 2, 1024], BF16)
        P.dma("pool", wukv[:], w_ukv[l].rearrange("(c p) n -> p c n", p=128), (), [wukv.k])
        xb_r = sbring("xb", 1, [128, 8, 512])
        sq_r = sbring("sq", 1, [128, 8, 512], BF16)
        hT_r = sbring("hT", 2, [128, 8, 512], BF16)
        std_r = sbring("std", 4, [128, 512])
        rope_r = sbring("rope", 2, [128, 6, 512])
        f32_r = sbring("tf", 4, [128, 512])
        xq_r = sbring("xq", 4, [128, 512])
        b16_r = sbring("tb", 13, [128, 512], BF16)
        cqf = sb("cqf", [128, 3, 512])
        cqn = sb("cqn", [128, 3, 512], BF16)
        ckvf = sb("ckvf", [128, 2, 512])
        ckvn = sb("ckvn", [128, 2, 512], BF16)
        sqcq = sb("sqcq", [128, 3, 512], BF16)
        sqckv = sb("sqckv", [128, 2, 512], BF16)
        PSA = Ring(pbanks, transient=True)
        blocks = blocks_for(True)
        B = {}

        def prep(bi):
            t0, n = blocks[bi]
            wh = 1 if t0 == 0 else 0
            xb, rp, hT, sq = xb_r.next(), rope_r.next(), hT_r.next(), sq_r.next()
            B[bi] = (hT, rp)
            P.dma("sp", xb[:, :, 0:n], Xin[:, t0:t0 + n].rearrange("(c p) n -> p c n", p=128), (), [xb.k])
            P.dma("sp", rp[:, :, 0:n], rope_d[:, :, t0:t0 + n].rearrange("a p n -> p a n"), (), [rp.k])
            ACT(sq[:, :, 0:n], xb[:, :, 0:n], AF.Square, [xb.k], [sq.k])
            pb = PSA.next()
            for c in range(8):
                MM(pb[:, 0:n], ones_b[:], sq[:, c, 0:n], c == 0, c == 7, [ones_b.k, sq.k], [pb.k])
            rs = rms_rstd(pb[:, 0:n], n, 1.0 / D, 128, std_r, [pb.k])
            yield
            TT("dve", xb[:, :, 0:n], xb[:, :, 0:n], rs[:, 0:n].unsqueeze(1).to_broadcast([128, 8, n]), ALU.mult,
               [xb.k, rs.k], [xb.k])
            for c in range(8):
                ACT(hT[:, c, 0:n], xb[:, c, 0:n], AF.Identity, [xb.k, gs1.k, modv.k], [(hT.k, c)],
                    scale=gs1[:, c, wh:wh + 1], bias=modv[:, 0 * 8 + c, wh:wh + 1])
            P.dma("sp", Hs[:, t0:t0 + n].rearrange("(c p) n -> p c n", p=128), hT[:, :, 0:n], [(hT.k, c) for c in range(8)], [("H", t0)])

        def hk(hT):
            return [(hT.k, c) for c in range(8)]

        def proj_fm(bi, col0, M, wt=None, nk=8, src=None, srck=None):
            t0, n = blocks[bi]
            hT, rp = B[bi]
            wt = win if wt is None else wt
            if src is None:
                src, srck = hT, hk(hT)
            pbx = PSA.next()
            for k in range(nk):
                MM(pbx[0:M, 0:n], wt[:, k, col0:col0 + M], src[:, k, 0:n], k == 0, k == nk - 1,
                   [(win.k, k) if wt is win else wt.k] + srck, [pbx.k])
            return pbx

        def norm_rope(bi, pbx, M, blk_ap, blk_k, gain_ap, rT_ap, rT_k, ci, si, dst, dkey):
            t0, n = blocks[bi]
            hT, rp = B[bi]
            s2 = b16_r.next()
            ACT(s2[0:M, 0:n], pbx[0:M, 0:n], AF.Square, [pbx.k], [s2.k])
            xq = xq_r.next()
            CP("act", xq[0:M, 0:n], pbx[0:M, 0:n], [pbx.k], [xq.k])
            yield
            pss = PSA.next()
            MM(pss[0:M, 0:n], blk_ap, s2[0:M, 0:n], True, True, [blk_k, s2.k], [pss.k])
            r2 = rms_rstd(pss[0:M, 0:n], n, 1.0, M, std_r, [pss.k])
            qn = b16_r.next()
            STT("dve", qn[0:M, 0:n], xq[0:M, 0:n], gain_ap, r2[0:M, 0:n], ALU.mult, ALU.mult,
                [xq.k, r2.k, smalls.k], [qn.k])
            if rT_ap is None:
                P.dma("sp", dst, qn[0:M, 0:n], [qn.k], [dkey])
                return
            yield
            yield
            prot = PSA.next()
            MM(prot[0:M, 0:n], rT_ap, qn[0:M, 0:n], True, True, [rT_k, qn.k], [prot.k])
            t1 = f32_r.next()
            TT("pool", t1[0:M, 0:n], qn[0:M, 0:n], rp[0:M, ci, 0:n], ALU.mult, [qn.k, rp.k], [t1.k])
            t2 = f32_r.next()
            TT("dve", t2[0:M, 0:n], prot[0:M, 0:n], rp[0:M, si, 0:n], ALU.mult, [prot.k, rp.k], [t2.k])
            o = b16_r.next()
            TT("pool", o[0:M, 0:n], t1[0:M, 0:n], t2[0:M, 0:n], ALU.add, [t1.k, t2.k], [o.k])
            for d_ in (dst if isinstance(dst, list) else [dst]):
                P.dma("sp", d_, o[0:M, 0:n], [o.k], [dkey])

        def job_dqk(bi, h, isk):
            t0, n = blocks[bi]
            pbx = proj_fm(bi, (512 if isk else 0) + h * 128, 128)
            dst = (DK if isk else DQ)[h, :, t0:t0 + n]
            yield from norm_rope(bi, pbx, 128, blk64_b[:], blk64_b.k, smc("dkg" if isk else "dqg"), rdT_b, cmatb.k, 0, 1,
                                 dst, ("DQK", isk, h, t0))

        def job_tm(bi, tt, col0, N, dst, dt_):
            t0, n = blocks[bi]
            hT, rp = B[bi]
            pbx = PSA.next()
            for k in range(8):
                MM(pbx[:, 0:N], hT[:, k, tt * 128:(tt + 1) * 128], win[:, k, col0:col0 + N], k == 0, k == 7,
                   [(hT.k, k), (win.k, k)], [pbx.k])
            o = (b16_r if dt_ == BF16 else f32_r).next()
            CP("act", o[:, 0:N], pbx[:, 0:N], [pbx.k], [o.k])
            P.dma("sp", dst[t0 + tt * 128:t0 + (tt + 1) * 128, :], o[:, 0:N], [o.k], [("tm", col0, t0, tt)])
            return
            yield

        def job_xbc(bi, c):
            t0, n = blocks[bi]
            pbx = proj_fm(bi, 2048 + c * 128, 128)
            o = b16_r.next()
            CP("act", o[:, 0:n], pbx[:, 0:n], [pbx.k], [o.k])
            P.dma("sp", XBC[c * 128:(c + 1) * 128, t0:t0 + n], o[:, 0:n], [o.k], [("XBC", c, t0)])
            return
            yield

        def job_lat(bi, col0, nch, xf, xnb, s3, gname, ndim):
            t0, n = blocks[bi]
            for c in range(nch):
                pbx = proj_fm(bi, col0 + c * 128, 128)
                CP("act", xf[:, c, 0:n], pbx[:, 0:n], [pbx.k], [(xf.k, c)])
                ACT(s3[:, c, 0:n], pbx[:, 0:n], AF.Square, [pbx.k], [(s3.k, c)])
            yield
            pss = PSA.next()
            for c in range(nch):
                MM(pss[:, 0:n], ones_b[:], s3[:, c, 0:n], c == 0, c == nch - 1, [ones_b.k, (s3.k, c)], [pss.k])
            r3 = rms_rstd(pss[:, 0:n], n, 1.0 / ndim, 128, std_r, [pss.k])
            for c in range(nch):
                STT("dve", xnb[:, c, 0:n], xf[:, c, 0:n], smc(gname, c), r3[:, 0:n], ALU.mult, ALU.mult,
                    [(xf.k, c), r3.k, smalls.k], [(xnb.k, c)])

        def job_mq(bi, h):
            t0, n = blocks[bi]
            pbx = proj_fm(bi, h * 96, 96, wt=wuq, nk=3, src=cqn, srck=[(cqn.k, c) for c in range(3)])
            yield from norm_rope(bi, pbx, 96, blk96_b[0:96, 0:96], blk96_b.k, smc("mqg", p1=96), rmT_b[0:96, 0:96], cmatb.k,
                                 2, 3, MQ[h, :, t0:t0 + n], ("MQ", h, t0))

        def job_mk(bi, h):
            t0, n = blocks[bi]
            pbx = proj_fm(bi, h * 128, 64, wt=wukv, nk=2, src=ckvn, srck=[(ckvn.k, c) for c in range(2)])
            yield from norm_rope(bi, pbx, 64, blk64_b[0:64, 0:64], blk64_b.k, smc("mkg", p1=64), None, None, None, None,
                                 MK[h, 0:64, t0:t0 + n], ("MKn", h, t0))

        def job_mv(bi, tt):
            t0, n = blocks[bi]
            pbx = PSA.next()
            for k in range(2):
                MM(pbx[:, 0:512].rearrange("p (h d) -> p h d", h=8), ckvn[:, k, tt * 128:(tt + 1) * 128],
                   wukv[:, k, :].rearrange("p (h d) -> p h d", h=8)[:, :, 64:128], k == 0, k == 1, [(ckvn.k, k), wukv.k], [pbx.k])
            o = b16_r.next()
            CP("act", o[:, 0:512], pbx[:, 0:512], [pbx.k], [o.k])
            P.dma("sp", MV[t0 + tt * 128:t0 + (tt + 1) * 128, :], o[:, 0:512], [o.k], [("MV", t0, tt)])
            return
            yield

        def job_kr(bi):
            t0, n = blocks[bi]
            pbx = proj_fm(bi, 3728, 32)
            yield from norm_rope(bi, pbx, 32, blk32_b[:], blk32_b.k, smc("mkrg", p1=32), rkT_b[:], rkT_b.k, 4, 5,
                                 [MK[h, 64:96, t0:t0 + n] for h in range(8)], ("MKr", t0))

        st["act_rstd"] = True
        run_jobs([prep(0)])
        gens = []
        for bi, (t0, n) in enumerate(blocks):
            jl = [job_lat(bi, 3088, 3, cqf, cqn, sqcq, "cqg", 384), job_lat(bi, 3472, 2, ckvf, ckvn, sqckv, "ckvg", 256)]
            jl += [job_dqk(bi, h, False) for h in range(4)]
            jl += [job_dqk(bi, h, True) for h in range(4)]
            for tt in range(n // 128):
                jl += [job_tm(bi, tt, 1024, 512, DV, BF16), job_tm(bi, tt, 1536, 512, Zs, BF16), job_tm(bi, tt, 3072, 16, DTs, F32)]
            jl += [job_xbc(bi, c) for c in range(8)]
            if bi + 1 < len(blocks):
                jl.append(prep(bi + 1))
            jl += [job_mq(bi, h) for h in range(8)]
            jl += [job_mk(bi, h) for h in range(8)]
            jl += [job_mv(bi, tt) for tt in range(n // 128)]
            jl.append(job_kr(bi))
            gens += jl
        run_jobs(gens)
        st["act_rstd"] = False
        P.barrier()

    def phaseB(l, need_ctx):
        st["off"] = SB_PERSIST
        kt_r = sbring("kt", 2, [128, 2, T], BF16)
        for b in kt_r.bufs:
            MEMSET("pool", b[:], 0.0, [b.k])
        qt_r = sbring("qt", 2, [128, T], BF16)
        vd_r = sbring("vd", 2, [128, NT, 128], BF16)
        vm_r = sbring("vm", 2, [128, NT, 65], BF16)
        for b in vm_r.bufs:
            MEMSET("pool", b[:, :, 64:65], 1.0, [b.k])
        pT_r = sbring("pT", 8, [128, 512], BF16)
        f32_r = sbring("bf", 6, [128, 512])
        b16_r = sbring("bb", 3, [128, 512], BF16)
        wst_r = sbring("wst", 3, [128, 6144], BF16)
        for e in range(32):
            w = wst_r.next()
            P.dma("pool", w[:, 0:2048].rearrange("p (c n) -> p c n", c=8), moe_wg[l, e].rearrange("(c p) n -> p c n", p=128), (), [(w.k, 0)])
            P.dma("pool", w[:, 2048:4096].rearrange("p (c n) -> p c n", c=8), moe_wu[l, e].rearrange("(c p) n -> p c n", p=128), (), [(w.k, 1)])
            P.dma("pool", w[:, 4096:6144].rearrange("p (c n) -> p c n", c=2), moe_wd[l, e].rearrange("(c p) n -> p c n", p=128), (), [(w.k, 2)])
            P.dma("pool", WBs[e * 128:(e + 1) * 128, :], w[:], [(w.k, 0), (w.k, 1), (w.k, 2)], ["WB", (w.k, 0), (w.k, 1), (w.k, 2)])
        SC = Ring(pbanks[0:4])
        OA = Ring(pbanks[4:6])
        SA = Ring(pbanks[6:8])
        qblocks = [(CTX + 512 * i, 512, NT) for i in range(S // 512)]
        if need_ctx:
            qblocks = [(0, CTX, CTX // 128)] + qblocks

        PD = 4

        def run_head(kt, qt, vv, is_diff, h):
            groups = []
            for (q0, nq, nkt) in qblocks:
                for comp in range(2 if is_diff else 1):
                    groups.append((q0, nq, nkt, comp))
            items = []
            for gi, (q0, nq, nkt, comp) in enumerate(groups):
                for ki in range(nkt):
                    items.append((gi, ki))
            gstate = {}
            pTs = {}
            oc_hold = {}

            def issue_sc(idx):
                gi, ki = items[idx]
                q0, nq, nkt, comp = groups[gi]
                sc = SC.next()
                scale = 0.125 if is_diff else mscale
                MM(sc[:, 0:nq], kt[:, comp, ki * 128:(ki + 1) * 128], qt[:, q0:q0 + nq], True, True, [kt.k, qt.k], [sc.k])
                pT = pT_r.next()
                ACT(pT[:, 0:nq], sc[:, 0:nq], AF.Exp, [sc.k], [pT.k], scale=scale)
                pTs[idx] = pT

            def issue_av(idx):
                gi, ki = items[idx]
                q0, nq, nkt, comp = groups[gi]
                if ki == 0:
                    gstate[gi] = (OA.next(), SA.next() if is_diff else None)
                O, Sm = gstate[gi]
                pT = pTs.pop(idx)
                if is_diff:
                    MM(O[:, 0:nq], vv[:, ki, :], pT[:, 0:nq], ki == 0, ki == nkt - 1, [vv.k, pT.k], [O.k])
                    MM(Sm[:, 0:nq], ones_b[:], pT[:, 0:nq], ki == 0, ki == nkt - 1, [ones_b.k, pT.k], [Sm.k])
                else:
                    MM(O[0:65, 0:nq], vv[:, ki, :], pT[:, 0:nq], ki == 0, ki == nkt - 1, [vv.k, pT.k], [O.k])
                if ki != nkt - 1:
                    return
                del gstate[gi]
                if is_diff:
                    rc = f32_r.next()
                    RECIP(rc[:, 0:nq], Sm[:, 0:nq], [Sm.k], [rc.k])
                    o_ = oc_r.next()
                    TT("dve", o_[:, 0:nq], O[:, 0:nq], rc[:, 0:nq], ALU.mult, [O.k, rc.k], [o_.k])
                    if comp == 0:
                        oc_hold[q0] = o_
                        return
                    o0 = oc_hold.pop(q0)
                    o = f32_r.next()
                    STT("dve", o[:, 0:nq], o_[:, 0:nq], neglam[:, 0:1], o0[:, 0:nq], ALU.mult, ALU.add,
                        [o0.k, o_.k, neglam.k], [o.k])
                    s2 = b16_r.next()
                    ACT(s2[:, 0:nq], o[:, 0:nq], AF.Square, [o.k], [s2.k])
                    pss = SC.next()
                    MM(pss[:, 0:nq], mean128_b[:], s2[:, 0:nq], True, True, [mean128_b.k, s2.k], [pss.k])
                    r2 = f32_r.next()
                    ACT(r2[:, 0:nq], pss[:, 0:nq], AF.Sqrt, [pss.k], [r2.k], scale=1.0, bias=EPS)
                    RECIP(r2[:, 0:nq], r2[:, 0:nq], [r2.k], [r2.k])
                    ob = b16_r.next()
                    STT("dve", ob[:, 0:nq], o[:, 0:nq], gsub[:, 0:1], r2[:, 0:nq], ALU.mult, ALU.mult, [o.k, gsub.k, r2.k], [ob.k])
                    P.dma("sp", DOs[h * 128:(h + 1) * 128, q0:q0 + nq], ob[:, 0:nq], [ob.k], [("DO", h, q0)])
                else:
                    oa = f32_r.next()
                    CP("act", oa[0:65, 0:nq], O[0:65, 0:nq], [O.k], [oa.k])
                    pbc = SC.next()
                    MM(pbc[0:64, 0:nq], sel65[:], oa[0:65, 0:nq], True, True, [sel65.k, oa.k], [pbc.k])
                    rc = f32_r.next()
                    RECIP(rc[0:64, 0:nq], pbc[0:64, 0:nq], [pbc.k], [rc.k])
                    ob = b16_r.next()
                    TT("dve", ob[0:64, 0:nq], oa[0:64, 0:nq], rc[0:64, 0:nq], ALU.mult, [oa.k, rc.k], [ob.k])
                    P.dma("sp", MOs[h * 64:(h + 1) * 64, q0:q0 + nq], ob[0:64, 0:nq], [ob.k], [("MO", h, q0)])

            N = len(items)
            for idx in range(N + PD):
                if idx < N:
                    issue_sc(idx)
                if idx >= PD:
                    issue_av(idx - PD)

        mscale = 96.0 ** -0.5
        oc_r = sbring("oc", 3, [128, 512])
        for h in range(4):
            kt, qt, vv = kt_r.next(), qt_r.next(), vd_r.next()
            P.dma("sp", kt[0:64, 0, :], DK[h, 0:64, :], (), [kt.k])
            P.dma("sp", kt[64:128, 1, :], DK[h, 64:128, :], (), [kt.k])
            P.dma("sp", qt[:], DQ[h], (), [qt.k])
            P.dma("sp", vv[:], DV[:, h * 128:(h + 1) * 128].rearrange("(t p) d -> p t d", p=128), (), [vv.k])
            run_head(kt, qt, vv, True, h)
        for h in range(8):
            kt, qt, vv = kt_r.next(), qt_r.next(), vm_r.next()
            P.dma("sp", kt[0:96, 0, :], MK[h], (), [kt.k])
            P.dma("sp", qt[0:96, :], MQ[h], (), [qt.k])
            P.dma("sp", vv[:, :, 0:64], MV[:, h * 64:(h + 1) * 64].rearrange("(t p) d -> p t d", p=128), (), [vv.k])
            run_head(kt, qt, vv, False, h)
        P.barrier()


    pbanks_b = [Buf(pb.t.bitcast(BF16), pb.k) for pb in pbanks]

    def phaseC(l, need_ctx):
        st["off"] = SB_PERSIST
        U = sb("U", [128, 8, T], BF16)
        base_after_U = st["off"]
        W = T + 8
        WO = W - 4
        uin_r = sbring("uin", 2, [128, W], BF16)
        for b in uin_r.bufs:
            MEMSET("pool", b[:, 0:2], 0.0, [b.k])
            MEMSET("pool", b[:, 258:262], 0.0, [b.k])
            MEMSET("pool", b[:, 262 + S:264 + S], 0.0, [b.k])
        cw0, _ = SM["convw"]
        cb0, _ = SM["convb"]
        dg = sb("dg", [128, 40, 128], BF16)
        for i in range(40):
            TS("dve" if i % 2 == 0 else "pool", dg[:, i, :], ident_f, smalls[:, cw0 + i:cw0 + i + 1], None, ALU.mult, None,
               [cmat.k, smalls.k], [(dg.k, i)])
        PSC = Ring(pbanks, transient=True)
        cblocks = [(0, CTX, 0)] + [(260 + 512 * i, 512, CTX + 512 * i) for i in range(S // 512)]
        for c in range(8):
            uin = uin_r.next()
            P.dma("sp", uin[:, 2:258], XBC[c * 128:(c + 1) * 128, 0:CTX], (), [uin.k])
            P.dma("sp", uin[:, 262:262 + S], XBC[c * 128:(c + 1) * 128, CTX:T], (), [uin.k])
            for (j0, w, u0) in cblocks:
                pc = PSC.next()
                for k in range(5):
                    MM(pc[:, 0:w], dg[:, c * 5 + k, :], uin[:, j0 + k:j0 + k + w], k == 0, k == 4, [(dg.k, c * 5 + k), uin.k], [pc.k])
                ACT(U[:, c, u0:u0 + w], pc[:, 0:w], AF.Silu, [pc.k, smalls.k], [U.k], bias=smalls[:, cb0 + c:cb0 + c + 1])
        P.barrier()
        st["off"] = base_after_U
        dtr = sb("dtr", [128, NT, 16])
        dtt = sb("dtt", [128, NT, 16])
        dtu = sb("dtu", [128, NT, 16])
        dtA = sb("dtA", [128, NT, 16])
        Arow = sb("Arow", [128, 16])
        Drow = sb("Drow", [128, 8])
        P.dma("sp", dtr[:], DTs.rearrange("(t p) d -> p t d", p=128), (), [dtr.k])
        c0, _ = RW["dtb"]
        TT("dve", dtr[:], dtr[:], rows[:, c0:c0 + 16].unsqueeze(1).to_broadcast([128, NT, 16]), ALU.add, [dtr.k, rows.k], [dtr.k])
        ACT(dtt[:], dtr[:], AF.Abs, [dtr.k], [dtt.k])
        ACT(dtt[:], dtt[:], AF.Exp, [dtt.k], [dtt.k], scale=-1.0)
        ACT(dtt[:], dtt[:], AF.Ln, [dtt.k], [dtt.k], bias=1.0)
        TS("dve", dtu[:], dtr[:], 0.0, None, ALU.max, None, [dtr.k], [dtu.k])
        TT("dve", dtu[:], dtu[:], dtt[:], ALU.add, [dtu.k, dtt.k], [dtu.k])
        c0, _ = RW["alog"]
        ACT(Arow[:], rows[:, c0:c0 + 16], AF.Exp, [rows.k], [Arow.k])
        TS("dve", Arow[:], Arow[:], -1.0, None, ALU.mult, None, [Arow.k], [Arow.k])
        TT("dve", dtA[:], dtu[:], Arow[:].unsqueeze(1).to_broadcast([128, NT, 16]), ALU.mult, [dtu.k, Arow.k], [dtA.k])
        c0, _ = RW["ssdD"]
        CP("dve", Drow[:], rows[:, c0:c0 + 8], [rows.k], [Drow.k])
        g0, _ = RW["ssdg"]

        hs = [sb(f"hs{d}", [128, 512]) for d in range(2)]
        hsb = [sb(f"hsb{d}", [128, 512], BF16) for d in range(2)]
        for d in range(2):
            MEMSET("dve", hs[d][:], 0.0, [hs[d].k])
            MEMSET("pool", hsb[d][:], 0.0, [hsb[d].k])
        xs_r = sbring("xs", 4, [128, 512], BF16)
        bt_r = sbring("bt", 4, [128, 256], BF16)
        cbT_r = sbring("cbT", 4, [128, 2, 128], BF16)
        ex_r = sbring("ex", 4, [128, 24])
        xdt_r = sbring("xdt", 4, [128, 512], BF16)
        xw_r = sbring("xw", 4, [128, 512], BF16)
        E_r = sbring("E", 4, [128, 128], BF16)
        E_r.transient = True
        mix_r = sbring("mix", 4, [128, 8, 128], BF16)
        y_r = sbring("y", 4, [128, 512])
        yf_r = sbring("yf", 4, [128, 512])
        z_r = sbring("z", 4, [128, 512], BF16)
        zg_r = sbring("zg", 4, [128, 512])
        ob_r = sbring("ob", 4, [128, 512], BF16)
        oT_r = sbring("oT", 4, [128, 4, 128], BF16)
        sm_r = sbring("sm", 6, [128, 2])
        tmp_r = sbring("tmp", 4, [128, 512])
        SEGR = Ring(pbanks[0:3], transient=True)
        MISC = Ring([3, 4])
        YD, YO, STP = pbanks[5], pbanks[6], pbanks[7]
        cmf = lambda nm: cmat[:, CM[nm], :]

        def chunk(ck, d, finalize):
            cs = slice(ck * 128, (ck + 1) * 128)
            hd = d * 8
            tri, ntri, strict, negm = (("triU", "negTriU", "strictL", "negmF") if d == 0 else ("triL", "negTriL", "strictU", "negmB"))
            mi = MISC.next()
            pt = pbanks_b[mi]
            for c in range(4):
                TR(pt[:, c * 128:(c + 1) * 128], U[:, c, cs], ident_b, [U.k, cmatb.k], [pt.k])
            xs = xs_r.next()
            CP("act", xs[:], pt[:, 0:512], [pt.k], [xs.k])
            mi = MISC.next()
            pt2 = pbanks_b[mi]
            for g in range(2):
                TR(pt2[:, g * 128:(g + 1) * 128], U[:, 4 + g, cs], ident_b, [U.k, cmatb.k], [pt2.k])
            bt = bt_r.next()
            CP("act", bt[:], pt2[:, 0:256], [pt2.k], [bt.k])
            mi = MISC.next()
            pc = pbanks[mi]
            for g in range(2):
                MM(pc[:, g * 128:(g + 1) * 128], U[:, 4 + g, cs], U[:, 6 + g, cs], True, True, [U.k], [pc.k])
            cbT = cbT_r.next()
            CP("dve", cbT[:], pc[:, 0:256].rearrange("p (g i) -> p g i", g=2), [pc.k], [cbT.k])
            mi = MISC.next()
            pa = pbanks[mi]
            dcol = dtA[:, ck, hd:hd + 8]
            MM(pa[:, 0:8], cmf(tri), dcol, True, True, [cmat.k, dtA.k], [pa.k])
            MM(pa[:, 8:16], cmf(strict), dcol, True, True, [cmat.k, dtA.k], [pa.k])
            MM(pa[:, 16:24], ones_f[:], dcol, True, True, [ones_f.k, dtA.k], [pa.k])
            ex = ex_r.next()
            ACT(ex[:], pa[:, 0:24], AF.Exp, [pa.k], [ex.k])
            xdt = xdt_r.next()
            TT("dve", xdt[:].rearrange("p (h q) -> p h q", h=8), xs[:].rearrange("p (h q) -> p h q", h=8),
               dtu[:, ck, hd:hd + 8].unsqueeze(2).to_broadcast([128, 8, 64]), ALU.mult, [xs.k, dtu.k], [xdt.k])
            xw = xw_r.next()
            TT("pool", xw[:].rearrange("p (h q) -> p h q", h=8), xdt[:].rearrange("p (h q) -> p h q", h=8),
               ex[:, 8:16].unsqueeze(2).to_broadcast([128, 8, 64]), ALU.mult, [xdt.k, ex.k], [xw.k])
            mixj = mix_r.next()
            for h in range(8):
                g = h // 4
                sg = SEGR.next()
                dbc = dtA[:, ck, hd + h:hd + h + 1].to_broadcast([128, 128])
                MM(sg[:, 0:128], dbc, cmf(tri), True, False, [dtA.k, cmat.k], [sg.k])
                MM(sg[:, 0:128], cmf(ntri), dbc, False, False, [dtA.k, cmat.k], [sg.k])
                MM(sg[:, 0:128], ident_f, cmf(negm), False, True, [cmat.k], [sg.k])
                E = E_r.next()
                ACT(E[:], sg[:, 0:128], AF.Exp, [sg.k], [E.k])
                TT("dve" if h % 2 == 0 else "pool", mixj[:, h, :], E[:], cbT[:, g, :], ALU.mult, [E.k, cbT.k], [(mixj.k, h)])
            yield
            for h in range(8):
                MM(YD[:, h * 64:(h + 1) * 64], mixj[:, h, :], xdt[:, h * 64:(h + 1) * 64], True, True, [(mixj.k, h), xdt.k], [YD.k])
            for g in range(2):
                MM(YO[:, g * 256:(g + 1) * 256], U[:, 6 + g, cs], hsb[d][:, g * 256:(g + 1) * 256], True, True, [U.k, hsb[d].k], [YO.k])
            y = y_r.next()
            TT("dve", y[:].rearrange("p (h q) -> p h q", h=8), YO[:, 0:512].rearrange("p (h q) -> p h q", h=8),
               ex[:, 0:8].unsqueeze(2).to_broadcast([128, 8, 64]), ALU.mult, [YO.k, ex.k], [y.k])
            TT("dve", y[:], y[:], YD[:, 0:512], ALU.add, [y.k, YD.k], [y.k])
            for g in range(2):
                MM(STP[:, g * 256:(g + 1) * 256], bt[:, g * 128:(g + 1) * 128], xw[:, g * 256:(g + 1) * 256], True, True, [bt.k, xw.k], [STP.k])
            TT("dve", hs[d][:].rearrange("p (h q) -> p h q", h=8), hs[d][:].rearrange("p (h q) -> p h q", h=8),
               ex[:, 16:24].unsqueeze(2).to_broadcast([128, 8, 64]), ALU.mult, [hs[d].k, ex.k], [hs[d].k])
            TT("dve", hs[d][:], hs[d][:], STP[:, 0:512], ALU.add, [hs[d].k, STP.k], [hs[d].k])
            CP("pool", hsb[d][:], hs[d][:], [hs[d].k], [hsb[d].k])
            rows_ck = slice(ck * 128, (ck + 1) * 128)
            if d == 0:
                tmp = tmp_r.next()
                TT("pool", tmp[:].rearrange("p (h q) -> p h q", h=8), xs[:].rearrange("p (h q) -> p h q", h=8),
                   Drow[:].unsqueeze(2).to_broadcast([128, 8, 64]), ALU.mult, [xs.k, Drow.k], [tmp.k])
                TT("pool", y[:], y[:], tmp[:], ALU.add, [y.k, tmp.k], [y.k])
                P.dma("sp", YF[rows_ck, :], y[:], [y.k], [("YF", ck)])
                return
            if not finalize:
                return
            yf = yf_r.next()
            P.dma("sp", yf[:], YF[rows_ck, :], [("YF", ck)], [yf.k])
            z = z_r.next()
            P.dma("sp", z[:], Zs[rows_ck, :], (), [z.k])
            zg = zg_r.next()
            ACT(zg[:], z[:], AF.Silu, [z.k], [zg.k])
            TT("pool", y[:], y[:], yf[:], ALU.add, [y.k, yf.k], [y.k])
            TT("dve", y[:], y[:], zg[:], ALU.mult, [y.k, zg.k], [y.k])
            sm = sm_r.next()
            MEMSET("dve", sm[:], 0.0, [sm.k])
            ACT(zg[:], y[:], AF.Square, [y.k, zg.k], [zg.k, sm.k], accum_out=sm[:, 0:1])
            ACT(sm[:, 1:2], sm[:, 0:1], AF.Sqrt, [sm.k], [sm.k], scale=1.0 / 512, bias=EPS)
            RECIP(sm[:, 1:2], sm[:, 1:2], [sm.k], [sm.k])
            ob = ob_r.next()
            STT("dve", ob[:], y[:], sm[:, 1:2], rows[:, g0:g0 + 512], ALU.mult, ALU.mult, [y.k, sm.k, rows.k], [ob.k])
            yield
            mi = MISC.next()
            po = pbanks_b[mi]
            for c in range(4):
                TR(po[:, c * 128:(c + 1) * 128], ob[:, c * 128:(c + 1) * 128], ident_b, [ob.k, cmatb.k], [po.k])
            oT = oT_r.next()
            CP("act", oT[:], po[:, 0:512].rearrange("p (c i) -> p c i", c=4), [po.k], [oT.k])
            P.dma("sp", SOs[:, rows_ck].rearrange("(c p) i -> p c i", p=128), oT[:], [oT.k], [("SO", ck)])

        nctx = CTX // 128
        order = list(range(nctx - 1, -1, -1)) + list(range(NT - 1, nctx - 1, -1))
        run_jobs([chunk(ck, 0, False) for ck in range(NT)])
        run_jobs([chunk(ck, 1, need_ctx or ck >= nctx) for ck in order])
        P.barrier()

    def phaseD(l, Xin, need_ctx):
        st["off"] = SB_PERSIST
        st["hi"] = SB_META
        wg = sb("wg", [128, 8, 3072], BF16)
        for c in range(8):
            P.dma("pool", wg[:, c, :], w_in[l, c * 128:(c + 1) * 128, 3760:6832], (), [(wg.k, c)])
        wb = sb("wb", [128, 3, 4, 1024], BF16)
        for k in range(3):
            P.dma("pool", wb[:, k], w_branch[l, k].rearrange("(c p) n -> p c n", p=128), (), [(wb.k, k)])
        wo = sb("wo", [128, 8, 1024], BF16)
        P.dma("pool", wo[:], w_out[l].rearrange("(c p) n -> p c n", p=128), (), [wo.k])
        wr = sb("wr", [128, 8, 36])
        P.dma("sp", wr[:], router_w[l].rearrange("(c p) n -> p c n", p=128), (), [wr.k])
        hT_r = sbring("dhT", 1, [128, 8, 512], BF16)
        o3_r = sbring("o3", 1, [128, 3, 4, 512], BF16)
        xb_r = sbring("dxb", 1, [128, 8, 512])
        acc = sb("dacc", [128, 8, 512])
        accb = sb("daccb", [128, 8, 512], BF16)
        h2f = sb("dh2f", [128, 8, 512])
        h2b = sb("dh2b", [128, 8, 512], BF16)
        g_r = sbring("dg", 3, [128, 512], BF16)
        gm_r = sbring("dgm", 2, [128, 512])
        std_r = sbring("dstd", 1, [128, 512])
        lg_r = sbring("lg", 2, [128, 36])
        sc_r = sbring("rsc", 2, [128, 16])
        m_r = sbring("rm", 2, [128, 4, 32])
        htm_r = sbring("htm", 2, [128, D], BF16)
        htm_r.transient = True
        rt_r = sbring("rt", 2, [128, 2, 32])
        rt_r.transient = True
        MK1, MK2, W12, RK, Macc = meta_bufs(f"d{l}")
        MEMSET("dve", Macc[:], 0.0, [Macc.k])
        for r_ in (g_r, gm_r, std_r, lg_r, sc_r, m_r):
            r_.transient = True
        PSD = Ring(pbanks, transient=True)
        rb0, _ = RW["rb"]
        BIG = 30000.0
        blocks = blocks_for(need_ctx)
        BD = {}
        ack = [(acc.k, c) for c in range(8)]
        abk = [(accb.k, c) for c in range(8)]
        hfk = [(h2f.k, c) for c in range(8)]

        def load_ho(bi):
            t0, n = blocks[bi]
            hT, o3 = hT_r.next(), o3_r.next()
            BD[bi] = [hT, o3, None]
            P.dma("sp", hT[:, :, 0:n], Hs[:, t0:t0 + n].rearrange("(c p) n -> p c n", p=128), (), [hT.k])
            for k, src in enumerate((DOs, SOs, MOs)):
                P.dma("sp", o3[:, k, :, 0:n], src[:, t0:t0 + n].rearrange("(c p) n -> p c n", p=128), (), [(o3.k, k)])
            return
            yield

        def load_x(bi):
            t0, n = blocks[bi]
            xb = xb_r.next()
            BD[bi][2] = xb
            P.dma("sp", xb[:, :, 0:n], Xin[:, t0:t0 + n].rearrange("(c p) n -> p c n", p=128), (), [xb.k])
            return
            yield

        def gjob(bi, c, k):
            t0, n = blocks[bi]
            hT, o3, _ = BD[bi]
            pg = PSD.next()
            for kc in range(8):
                MM(pg[:, 0:n], wg[:, kc, k * 1024 + c * 128:k * 1024 + (c + 1) * 128], hT[:, kc, 0:n], kc == 0, kc == 7,
                   [(wg.k, kc), hT.k], [pg.k])
            g = g_r.next()
            ACT(g[:, 0:n], pg[:, 0:n], AF.Sigmoid, [pg.k], [g.k])
            pm = PSD.next()
            for kc in range(4):
                MM(pm[:, 0:n], wb[:, k, kc, c * 128:(c + 1) * 128], o3[:, k, kc, 0:n], kc == 0, kc == 3,
                   [(wb.k, k), (o3.k, k)], [pm.k])
            ak = (acc.k, c)
            if k == 0:
                TT("dve", acc[:, c, 0:n], pm[:, 0:n], g[:, 0:n], ALU.mult, [pm.k, g.k], [ak])
            else:
                gm = gm_r.next()
                TT("dve", gm[:, 0:n], pm[:, 0:n], g[:, 0:n], ALU.mult, [pm.k, g.k], [gm.k])
                if k == 1:
                    TT("pool", acc[:, c, 0:n], acc[:, c, 0:n], gm[:, 0:n], ALU.add, [ak, gm.k], [ak])
                else:
                    TT("pool", accb[:, c, 0:n], acc[:, c, 0:n], gm[:, 0:n], ALU.add, [ak, gm.k], [(accb.k, c)])
            return
            yield

        def tail(bi):
            t0, n = blocks[bi]
            wh = 1 if t0 == 0 else 0
            xb = BD[bi][2]
            for c in range(8):
                py = PSD.next()
                for kc in range(8):
                    MM(py[:, 0:n], wo[:, kc, c * 128:(c + 1) * 128], accb[:, kc, 0:n], kc == 0, kc == 7,
                       [wo.k, (accb.k, kc)], [py.k])
                STT("dve", xb[:, c, 0:n], py[:, 0:n], modv[:, 2 * 8 + c, wh:wh + 1], xb[:, c, 0:n], ALU.mult, ALU.add,
                    [py.k, modv.k, xb.k], [xb.k])
            P.dma("sp", XM[:, t0:t0 + n].rearrange("(c p) n -> p c n", p=128), xb[:, :, 0:n], [xb.k], [("XM", t0)])
            ACT(accb[:, :, 0:n], xb[:, :, 0:n], AF.Square, [xb.k], abk)
            pb = PSD.next()
            for c in range(8):
                MM(pb[:, 0:n], ones_b[:], accb[:, c, 0:n], c == 0, c == 7, [ones_b.k] + abk, [pb.k])
            rs = rms_rstd(pb[:, 0:n], n, 1.0 / D, 128, std_r, [pb.k])
            TT("dve", h2f[:, :, 0:n], xb[:, :, 0:n], rs[:, 0:n].unsqueeze(1).to_broadcast([128, 8, n]), ALU.mult,
               [xb.k, rs.k], hfk)
            for c in range(8):
                ACT(h2f[:, c, 0:n], h2f[:, c, 0:n], AF.Identity, [(h2f.k, c), gs2.k, modv.k], [(h2f.k, c)],
                    scale=gs2[:, c, wh:wh + 1], bias=modv[:, 3 * 8 + c, wh:wh + 1])
            CP("pool", h2b[:, :, 0:n], h2f[:, :, 0:n], hfk, [(h2b.k, c) for c in range(8)])
            for _ in range(7):
                yield
            for tt in range(n // 128):
                pti = PSD.next()
                pt = Buf(pti.t.bitcast(BF16), pti.k)
                for c in range(8):
                    TR(pt[:, c * 128:(c + 1) * 128], h2b[:, c, tt * 128:(tt + 1) * 128], ident_b, [(h2b.k, c), cmatb.k], [pt.k])
                htm = htm_r.next()
                CP("act", htm[:], pt[:, 0:D], [pt.k], [htm.k])
                P.dma("sp", H2TM[t0 + tt * 128:t0 + (tt + 1) * 128, :], htm[:], [htm.k], [("H2TM", t0, tt)])
            for tt in range(n // 128):
                pr = PSD.next()
                for kc in range(8):
                    MM(pr[:, 0:36], h2f[:, kc, tt * 128:(tt + 1) * 128], wr[:, kc, :], kc == 0, kc == 7, [(h2f.k, kc), wr.k], [pr.k])
                lg, sc, m = lg_r.next(), sc_r.next(), m_r.next()
                TT("dve", lg[:], pr[:, 0:36], rows[:, rb0:rb0 + 36], ALU.add, [pr.k, rows.k], [lg.k])
                MEMSET("dve", sc[:], 0.0, [sc.k])
                RED = lambda o, i, r, w: P.op("dve", lambda e: e.tensor_reduce(out=o, in_=i, axis=AX.X, op=ALU.max), r, w)
                RED(sc[:, 0:1], lg[:, 0:4], [lg.k], [sc.k])
                TS("dve", m[:, 0, 0:4], lg[:, 0:4], sc[:, 0:1], None, ALU.is_equal, None, [lg.k, sc.k], [m.k])
                TS("dve", sc[:, 1:2], sc[:, 0:1], -1.0, None, ALU.mult, None, [sc.k], [sc.k])
                ACT(m[:, 0, 8:12], lg[:, 0:4], AF.Exp, [lg.k, sc.k, m.k], [m.k, sc.k], bias=sc[:, 1:2], accum_out=sc[:, 2:3])
                RECIP(sc[:, 3:4], sc[:, 2:3], [sc.k], [sc.k])
                TS("dve", m[:, 0, 4:8], m[:, 0, 0:4], -1.0, BIG, ALU.add, ALU.mult, [m.k], [m.k])
                TT("dve", m[:, 1, :].rearrange("p (g e) -> p g e", g=4), lg[:, 4:36].rearrange("p (g e) -> p g e", g=4),
                   m[:, 0, 4:8].unsqueeze(2).to_broadcast([128, 4, 8]), ALU.add, [lg.k, m.k], [m.k])
                RED(sc[:, 4:5], m[:, 1, :], [m.k], [sc.k])
                TS("dve", m[:, 2, :], m[:, 1, :], sc[:, 4:5], None, ALU.is_equal, None, [m.k, sc.k], [m.k])
                STT("dve", m[:, 1, :], m[:, 2, :], -BIG, m[:, 1, :], ALU.mult, ALU.add, [m.k], [m.k])
                RED(sc[:, 5:6], m[:, 1, :], [m.k], [sc.k])
                TS("dve", m[:, 3, :], m[:, 1, :], sc[:, 5:6], None, ALU.is_equal, None, [m.k, sc.k], [m.k])
                TT("dve", sc[:, 6:7], sc[:, 4:5], sc[:, 5:6], ALU.subtract, [sc.k], [sc.k])
                ACT(sc[:, 7:8], sc[:, 6:7], AF.Sigmoid, [sc.k], [sc.k])
                TT("dve", sc[:, 8:9], sc[:, 7:8], sc[:, 3:4], ALU.mult, [sc.k], [sc.k])
                TT("dve", sc[:, 9:10], sc[:, 3:4], sc[:, 8:9], ALU.subtract, [sc.k], [sc.k])
                gt = (t0 + tt * 128) // 128
                CP("dve", MK1[:, gt, :], m[:, 2, :], [m.k], [(MK1.k, gt)])
                CP("pool", MK2[:, gt, :], m[:, 3, :], [m.k], [(MK2.k, gt)])
                CP("dve", W12[:, gt, :], sc[:, 8:10], [sc.k], [(W12.k, gt)])
                TT("dve", m[:, 0, :], m[:, 2, :], m[:, 3, :], ALU.add, [m.k], [m.k])
                pk = PSD.next()
                MM(pk[:, 0:32], cmat[:, CM["strictU"], :], m[:, 0, :], True, False, [cmat.k, m.k], [pk.k])
                MM(pk[:, 0:32], ones_f[:], Macc[:], False, True, [ones_f.k, Macc.k], [pk.k])
                rt = rt_r.next()
                TT("dve", rt[:, 0, :], pk[:, 0:32], m[:, 2, :], ALU.mult, [pk.k, m.k], [rt.k])
                TT("dve", rt[:, 1, :], pk[:, 0:32], m[:, 3, :], ALU.mult, [pk.k, m.k], [rt.k])
                P.op("dve", lambda e, o=RK[:, gt, :], i=rt[:]: e.tensor_reduce(out=o, in_=i, axis=AX.X, op=ALU.add), [rt.k], [(RK.k, gt)])
                TT("dve", Macc[:], Macc[:], m[:, 0, :], ALU.add, [Macc.k, m.k], [Macc.k])

        gens = []
        for bi in range(len(blocks)):
            gens.append(load_ho(bi))
            gj = [gjob(bi, c, k) for c in range(8) for k in range(3)]
            gens += gj[:4] + [load_x(bi)] + gj[4:]
            gens.append(tail(bi))
        run_jobs(gens)
        st["hi"] = SB_HI
        P.barrier()

    def phaseE(l, need_ctx, last):
        st["off"] = SB_PERSIST
        st["hi"] = SB_META
        MK1, MK2, W12, RK, Macc = meta_bufs(f"e{l}")
        gt0 = 0 if need_ctx else CTX // 128
        ntl = NT - gt0
        NB = 2 * ntl + 32
        PSE = Ring(pbanks, transient=True)
        mc = sb("mc", [128, NMC])
        P.dma("sp", mc[:], mconst_d, (), [mc.k])
        RED = lambda o, i, r, w_, op=ALU.add: P.op("dve", lambda e_: e_.tensor_reduce(out=o, in_=i, axis=AX.X, op=op), r, w_)
        mk1k = [(MK1.k, g) for g in range(gt0, NT)]
        mk2k = [(MK2.k, g) for g in range(gt0, NT)]
        rkk = [(RK.k, g) for g in range(gt0, NT)]
        w12k = [(W12.k, g) for g in range(gt0, NT)]
        pc = PSE.next()
        MM(pc[:, 0:32], ones_f[:], Macc[:], True, True, [ones_f.k, Macc.k], [pc.k])
        cnt = sb("cnt", [128, 32])
        CP("dve", cnt[:], pc[:, 0:32], [pc.k], [cnt.k])
        cmp3 = sb("cmp3", [128, 32, NT])
        TT("dve", cmp3[:], cnt[:].unsqueeze(2).to_broadcast([128, 32, NT]), mc[:, 0:NT].unsqueeze(1).to_broadcast([128, 32, NT]),
           ALU.is_gt, [cnt.k, mc.k], [cmp3.k])
        nblk = sb("nblk", [128, 32])
        RED(nblk[:], cmp3[:], [cmp3.k], [nblk.k])
        ptr = PSE.next()
        TR(ptr[0:32, 0:128], nblk[:, 0:32], ident_f, [nblk.k, cmat.k], [ptr.k])
        nbT = sb("nbT", [32, 128])
        CP("act", nbT[:], ptr[0:32, 0:128], [ptr.k], [nbT.k])
        pp = PSE.next()
        MM(pp[:, 0:32], nbT[0:32, :], cmat[0:32, CM["triU"], 0:32], True, True, [nbT.k, cmat.k], [pp.k])
        pend = sb("pend", [128, 32])
        CP("dve", pend[:], pp[:, 0:32], [pp.k], [pend.k])
        pstart = sb("pstart", [128, 32])
        TT("dve", pstart[:], pend[:], nblk[:], ALU.subtract, [pend.k, nblk.k], [pstart.k])
        tmp3 = sb("tmp3", [128, NT, 32])
        psk = sb("psk", [128, NT])
        slotf = sb("slotf", [128, NT, 2])
        SL = sb("SL", [128, NT, 2], I32)
        for k, (MK, mkk) in enumerate(((MK1, mk1k), (MK2, mk2k))):
            TT("dve", tmp3[:, gt0:NT, :], MK[:, gt0:NT, :], pstart[:].unsqueeze(1).to_broadcast([128, ntl, 32]), ALU.mult,
               mkk + [pstart.k, tmp3.k], [tmp3.k])
            RED(psk[:, gt0:NT], tmp3[:, gt0:NT, :], [tmp3.k], [psk.k])
            STT("dve", slotf[:, gt0:NT, k], psk[:, gt0:NT], 128.0, RK[:, gt0:NT, k], ALU.mult, ALU.add, [psk.k] + rkk, [(slotf.k, k)])
        CP("dve", SL[:, gt0:NT, :], slotf[:, gt0:NT, :], [(slotf.k, 0), (slotf.k, 1)], [SL.k])
        cmpb = sb("cmpb", [128, NB, 32])
        TT("dve", cmpb[:], pend[:].unsqueeze(1).to_broadcast([128, NB, 32]), mc[:, 34:34 + NB].unsqueeze(2).to_broadcast([128, NB, 32]),
           ALU.is_le, [pend.k, mc.k], [cmpb.k])
        ebf = sb("ebf", [128, NB])
        RED(ebf[:], cmpb[:], [cmpb.k], [ebf.k])
        TS("dve", ebf[:], ebf[:], 31.0, None, ALU.min, None, [ebf.k], [ebf.k])
        TS("dve", ebf[:], ebf[:], 128.0, mc[:, 138:139], ALU.mult, ALU.add, [ebf.k, mc.k], [ebf.k])
        WIDX = sb("WIDX", [128, NB], I32)
        CP("dve", WIDX[:], ebf[:], [ebf.k], [WIDX.k])
        if "META" in dbg:
            d1 = nc.dram_tensor(f"dbg_SL{l}", [128, NT, 2], I32, kind="ExternalOutput").ap()
            d2 = nc.dram_tensor(f"dbg_WIDX{l}", [128, NB], I32, kind="ExternalOutput").ap()
            d3 = nc.dram_tensor(f"dbg_cnt{l}", [128, 32], F32, kind="ExternalOutput").ap()
            d4 = nc.dram_tensor(f"dbg_pend{l}", [128, 32], F32, kind="ExternalOutput").ap()
            P.dma("sp", d1[:, gt0:NT, :], SL[:, gt0:NT, :], [SL.k], ["d1"])
            P.dma("sp", d2, WIDX[:], [WIDX.k], ["d2"])
            P.dma("sp", d3, cnt[:], [cnt.k], ["d3"])
            P.dma("sp", d4, pend[:], [pend.k], ["d4"])
        if stop_after == ("E1", l):
            st["hi"] = SB_HI
            P.barrier()
            return
        idx_r = sbring("idx", 12, [128, 1], I32)
        idx_r.transient = True

        def IDX(col_ap, rk):
            it = idx_r.next()
            CP("pool", it[:, 0:1], col_ap, rk, [it.k])
            return it

        hrow_r = sbring("hrow", 3, [128, D], BF16)
        for gt in range(gt0, NT):
            hr = hrow_r.next()
            P.dma("sp", hr[:], H2TM[gt * 128:(gt + 1) * 128, :], (), [hr.k])
            for k in range(2):
                it = IDX(SL[:, gt, k:k + 1], [SL.k])
                P.idma(XBs, bass.IndirectOffsetOnAxis(it[:, 0:1].bitcast(U32), 0), hr[:], None, [hr.k, it.k], [hr.k, ("XB", gt, k)])
        P.barrier()
        if stop_after == ("E2", l):
            st["hi"] = SB_HI
            return
        base_blocks = st["off"]
        wt_r = sbring("wt", WT_RING, [128, 6144], BF16)
        wt_r.transient = True
        xtm_r = sbring("xtm", 3, [128, D], BF16)
        xT_r = sbring("xT", 2, [128, 8, 128], BF16)
        sg_r = sbring("ssg", 2, [128, 256], BF16)
        hid_r = sbring("shid", 2, [128, 256], BF16)
        yo_r = sbring("yo", 2, [128, D])
        for r_ in (xT_r, sg_r, hid_r):
            r_.transient = True

        def blockjob(b):
            wt, xtm = wt_r.next(), xtm_r.next()
            it = IDX(WIDX[:, b:b + 1], [WIDX.k])
            P.idma(wt[:], None, WBs, bass.IndirectOffsetOnAxis(it[:, 0:1].bitcast(U32), 0), [it.k], [wt.k])
            if XLOAD:
                P.dma("sp", xtm[:], XBs[b * 128:(b + 1) * 128, :], (), [xtm.k])
            yield
            yield
            if BLK_CUT == 0:
                return
            pti = PSE.next()
            pt = Buf(pti.t.bitcast(BF16), pti.k)
            for c in range(8):
                TR(pt[:, c * 128:(c + 1) * 128], xtm[:, c * 128:(c + 1) * 128], ident_b, [xtm.k, cmatb.k], [pt.k])
            xT = xT_r.next()
            CP("act", xT[:], pt[:, 0:D].rearrange("p (c n) -> p c n", c=8), [pt.k], [xT.k])
            if BLK_CUT == 1:
                return
            pg, pu = PSE.next(), PSE.next()
            for (pb_, off) in ((pg, 0), (pu, 2048)):
                for hc in range(2):
                    for kc in range(8):
                        MM(pb_[:, hc * 128:(hc + 1) * 128], wt[:, off + kc * 256 + hc * 128:off + kc * 256 + (hc + 1) * 128], xT[:, kc, :],
                           kc == 0, kc == 7, [wt.k, xT.k], [pb_.k])
            sg = sg_r.next()
            ACT(sg[:], pg[:, 0:256], AF.Silu, [pg.k], [sg.k])
            hid = hid_r.next()
            TT("dve", hid[:], pu[:, 0:256], sg[:], ALU.mult, [pu.k, sg.k], [hid.k])
            if BLK_CUT == 2:
                return
            yo = yo_r.next()
            for half in range(2):
                py = PSE.next()
                for hc in range(2):
                    MM(py[:, 0:512], hid[:, hc * 128:(hc + 1) * 128], wt[:, 4096 + hc * 1024 + half * 512:4096 + hc * 1024 + (half + 1) * 512],
                       hc == 0, hc == 1, [hid.k, wt.k], [py.k])
                if half == 0:
                    CP("act", yo[:, 0:512], py[:, 0:512], [py.k], [(yo.k, 0)])
                else:
                    CP("dve", yo[:, 512:1024], py[:, 0:512], [py.k], [(yo.k, 1)])
            P.dma("sp", YBs[b * 128:(b + 1) * 128, :], yo[:], [(yo.k, 0), (yo.k, 1)], [("YB", b), (yo.k, 0), (yo.k, 1)])

        run_jobs([blockjob(b) for b in range(NBLK_START, NB if NBLK_DBG is None else NBLK_DBG)])
        P.barrier()
        if stop_after == ("E3", l):
            st["hi"] = SB_HI
            return
        st["off"] = base_blocks
        ya_r = sbring("ya", 4, [128, D])
        yb_r = sbring("yb", 4, [128, D])
        xm_r = sbring("xm", 2, [128, 8, 512])
        blocks = blocks_for(need_ctx)

        def cjob(bi):
            t0, n = blocks[bi]
            wh = 1 if t0 == 0 else 0
            xm = xm_r.next()
            P.dma("sp", xm[:, :, 0:n], XM[:, t0:t0 + n].rearrange("(c p) n -> p c n", p=128), (), [xm.k])
            gath = []
            for tt in range(n // 128):
                gt = (t0 + tt * 128) // 128
                ya, yb = ya_r.next(), yb_r.next()
                ita = IDX(SL[:, gt, 0:1], [SL.k])
                P.idma(ya[:], None, YBs, bass.IndirectOffsetOnAxis(ita[:, 0:1].bitcast(U32), 0), [ita.k], [ya.k])
                itb = IDX(SL[:, gt, 1:2], [SL.k])
                P.idma(yb[:], None, YBs, bass.IndirectOffsetOnAxis(itb[:, 0:1].bitcast(U32), 0), [itb.k], [yb.k])
                gath.append((gt, ya, yb))
            for tt, (gt, ya, yb) in enumerate(gath):
                TS("dve", ya[:], ya[:], W12[:, gt, 0:1], None, ALU.mult, None, [ya.k] + w12k, [ya.k])
                STT("dve", ya[:], yb[:], W12[:, gt, 1:2], ya[:], ALU.mult, ALU.add, [ya.k, yb.k] + w12k, [ya.k])
                for q in range(2):
                    ptq = PSE.next()
                    for c4 in range(4):
                        c = q * 4 + c4
                        TR(ptq[:, c4 * 128:(c4 + 1) * 128], ya[:, c * 128:(c + 1) * 128], ident_f, [ya.k, cmat.k], [ptq.k])
                    for c4 in range(4):
                        c = q * 4 + c4
                        STT("dve", xm[:, c, tt * 128:(tt + 1) * 128], ptq[:, c4 * 128:(c4 + 1) * 128], modv[:, 5 * 8 + c, wh:wh + 1],
                            xm[:, c, tt * 128:(tt + 1) * 128], ALU.mult, ALU.add, [ptq.k, modv.k, xm.k], [xm.k])
            if last:
                P.dma("sp", yT[:, t0 - CTX:t0 - CTX + n].rearrange("(c p) n -> p c n", p=128), xm[:, :, 0:n], [xm.k], [("yT", t0), xm.k])
            else:
                P.dma("sp", XR[:, t0:t0 + n].rearrange("(c p) n -> p c n", p=128), xm[:, :, 0:n], [xm.k], [("XR", t0), xm.k])

        for bi in range(len(blocks)):
            cjob(bi)
        st["hi"] = SB_HI
        P.barrier()

    def finish():
        P.barrier()

    layers = []
    for l in range(depth):
        last = l == depth - 1
        Xin = xT if l == 0 else XR
        phase0(l)
        phaseA(l, Xin)
        if stop_after == ("A", l):
            break
        phaseB(l, not last)
        if stop_after == ("B", l):
            break
        phaseC(l, not last)
        if stop_after == ("C", l):
            break
        phaseD(l, Xin, not last)
        if stop_after == ("D", l):
            break
        phaseE(l, not last, last)
        if stop_after in (("E", l), ("E1", l), ("E2", l), ("E3", l)):
            break
    finish()
    with ExitStack() as stack:
        P.emit(stack)
    return nc, scr


def make_in_maps(inp, S, depth):
    B = inp["x"].shape[0]
    cmat, rope, sel, mconst = host_consts(S)
    smalls = np.stack([pack_smalls(inp, l) for l in range(depth)])
    rows = np.stack([pack_rows(inp, l) for l in range(depth)])
    router_w = np.ascontiguousarray(np.concatenate([inp["moe_group_w"], inp["moe_expert_w"]], axis=-1))
    shared = {
        "ada_w": inp["ada_w"], "w_in": inp["w_in"], "w_uq": inp["w_uq"], "w_ukv": inp["w_ukv"],
        "w_branch": inp["w_branch"], "w_out": inp["w_out"], "router_w": router_w,
        "moe_w_gate": inp["moe_w_gate"], "moe_w_up": inp["moe_w_up"], "moe_w_down": inp["moe_w_down"],
        "smalls": smalls, "rows": rows, "cmat": cmat, "rope": rope, "sel": sel, "mconst": mconst,
    }
    shared = {k: np.ascontiguousarray(v, dtype=np.float32) for k, v in shared.items()}
    maps = []
    for b in range(B):
        xt = np.ascontiguousarray(np.concatenate([inp["ctx"][b], inp["x"][b]], axis=0).T)
        cm = np.stack([inp["c"][b].reshape(8, 128).T, inp["c_ctx"].reshape(8, 128).T], axis=-1)
        m = dict(shared)
        m["xT"] = xt.astype(np.float32)
        m["cmod"] = np.ascontiguousarray(cm, dtype=np.float32)
        maps.append(m)
    return maps


def kernel(**inputs):
    inp = {k: np.asarray(v) for k, v in inputs.items()}
    B, S, _ = inp["x"].shape
    depth = inp["w_in"].shape[0]
    nc, _ = build_program(S, depth)
    maps = make_in_maps(inp, S, depth)
    res = run_bass_kernel_spmd(nc, maps, core_ids=list(range(B)))
    out = np.stack([np.ascontiguousarray(res.results[b]["yT"].T) for b in range(B)])
    return out.astype(np.float32)
```
